# Optimizing a Trainium2 kernel written in Bass

```python
import jax, jax.numpy as jnp
from jax import lax
import numpy as np

D_MODEL = 4096
BATCH = 2
SEQ = 4096
DEPTH = 2

ROPE_THETA = 10000.0
NORM_EPS = 1e-6
NEG_INF = -1e30

A_HEADS = 16
A_HEAD_DIM = 128
MOBA_BLOCK = 256
MOBA_TOPK = 3
MOBA_Q_CHUNK = 16
B_HEADS = 32
B_KV_HEADS = 4
B_HEAD_DIM = 64
SWA_WINDOW = 128
C_HEADS = 16
C_Q_RANK = 1024
C_KV_RANK = 512
C_NOPE_DIM = 128
C_ROPE_DIM = 64
C_V_DIM = 128
C_Q_BLOCK = 128
N_GROUPS = 8
EXPERTS_PER_GROUP = 4
N_EXPERTS = N_GROUPS * EXPERTS_PER_GROUP
EXPERT_TOPK = 2
D_FF_EXPERT = 768
MOE_BLOCK = 128

A_WIDTH = A_HEADS * A_HEAD_DIM
B_WIDTH = B_HEADS * B_HEAD_DIM
B_KV_WIDTH = B_KV_HEADS * B_HEAD_DIM
C_WIDTH = C_HEADS * C_V_DIM
IN_SIZES = (A_WIDTH, A_WIDTH, A_WIDTH, B_WIDTH, B_KV_WIDTH, B_KV_WIDTH,
            C_Q_RANK, C_KV_RANK, C_ROPE_DIM, D_MODEL, D_MODEL, D_MODEL)
N_IN = sum(IN_SIZES)

kernel_name = 'hybrid_moba_swa_mla_hier_moe'


def rmsnorm(x, g):
    xf = x.astype(jnp.float32)
    y = xf * lax.rsqrt(jnp.mean(xf * xf, axis=-1, keepdims=True) + NORM_EPS)
    return (y * g.astype(jnp.float32)).astype(x.dtype)


def rope_tables(positions, dim):
    inv_freq = ROPE_THETA ** (-jnp.arange(0, dim, 2, dtype=jnp.float32) / dim)
    ang = positions.astype(jnp.float32)[:, None, :, None] * inv_freq
    return jnp.cos(ang), jnp.sin(ang)


def apply_rope(x, cos, sin):
    x1, x2 = jnp.split(x, 2, axis=-1)
    c = cos.astype(x.dtype)
    s = sin.astype(x.dtype)
    return jnp.concatenate([x1 * c - x2 * s, x1 * s + x2 * c], axis=-1)


def split_heads(t, n_heads):
    b, s, _ = t.shape
    return t.reshape(b, s, n_heads, -1).transpose(0, 2, 1, 3)


def merge_heads(t):
    b, h, s, d = t.shape
    return t.transpose(0, 2, 1, 3).reshape(b, s, h * d)


def moba_attention(q, k, v):
    b, h, s, d = q.shape
    n_blk = -(-s // MOBA_BLOCK)
    s_pad = n_blk * MOBA_BLOCK
    pad = ((0, 0), (0, 0), (0, s_pad - s), (0, 0))
    kb = jnp.pad(k, pad).reshape(b, h, n_blk, MOBA_BLOCK, d)
    vb = jnp.pad(v, pad).reshape(b, h, n_blk, MOBA_BLOCK, d)
    k_mean = jnp.mean(kb.astype(jnp.float32), axis=3)
    topk = min(MOBA_TOPK, n_blk)
    scale = d ** -0.5
    n_chunks = s // MOBA_Q_CHUNK
    q_chunks = q.reshape(b, h, n_chunks, MOBA_Q_CHUNK, d).transpose(2, 0, 1, 3, 4)
    bi = jnp.arange(b)[:, None, None, None]
    hi = jnp.arange(h)[None, :, None, None]
    blk_ids = jnp.arange(n_blk)
    key_off = jnp.arange(MOBA_BLOCK)

    def chunk(args):
        qc, c = args
        q_pos = c * MOBA_Q_CHUNK + jnp.arange(MOBA_Q_CHUNK)
        own = (c * MOBA_Q_CHUNK) // MOBA_BLOCK
        gate = jnp.einsum('bhqd,bhnd->bhqn', qc.astype(jnp.float32), k_mean)
        gate = jnp.where(blk_ids < own, gate, NEG_INF)
        _, idx = lax.top_k(gate, topk)
        valid = idx < own
        k_sel = kb[bi, hi, idx]
        v_sel = vb[bi, hi, idx]
        s_sel = jnp.einsum('bhqd,bhqnkd->bhqnk', qc, k_sel,
                           preferred_element_type=jnp.float32) * scale
        s_sel = jnp.where(valid[..., None], s_sel, NEG_INF)
        k_own = lax.dynamic_index_in_dim(kb, own, axis=2, keepdims=False)
        v_own = lax.dynamic_index_in_dim(vb, own, axis=2, keepdims=False)
        s_own = jnp.einsum('bhqd,bhkd->bhqk', qc, k_own,
                           preferred_element_type=jnp.float32) * scale
        s_own = jnp.where((own * MOBA_BLOCK + key_off)[None, :] <= q_pos[:, None], s_own, NEG_INF)
        logits = jnp.concatenate(
            [s_sel.reshape(b, h, MOBA_Q_CHUNK, topk * MOBA_BLOCK), s_own], axis=-1)
        p = jax.nn.softmax(logits, axis=-1).astype(v.dtype)
        p_sel = p[..., :topk * MOBA_BLOCK].reshape(b, h, MOBA_Q_CHUNK, topk, MOBA_BLOCK)
        p_own = p[..., topk * MOBA_BLOCK:]
        return (jnp.einsum('bhqnk,bhqnkd->bhqd', p_sel, v_sel)
                + jnp.einsum('bhqk,bhkd->bhqd', p_own, v_own))

    out = lax.map(chunk, (q_chunks, jnp.arange(n_chunks)))
    return out.transpose(1, 2, 0, 3, 4).reshape(b, h, s, d)


def swa_sink_attention(q, k, v, sinks):
    b, hq, s, d = q.shape
    g = k.shape[1]
    r = hq // g
    w = SWA_WINDOW
    nb = s // w
    qb = q.reshape(b, g, r, nb, w, d)
    kb = k.reshape(b, g, nb, w, d)
    vb = v.reshape(b, g, nb, w, d)
    blk_pad = ((0, 0), (0, 0), (1, 0), (0, 0), (0, 0))
    kk = jnp.concatenate([jnp.pad(kb, blk_pad)[:, :, :-1], kb], axis=3)
    vv = jnp.concatenate([jnp.pad(vb, blk_pad)[:, :, :-1], vb], axis=3)
    sc = jnp.einsum('bgrnqd,bgnkd->bgrnqk', qb, kk,
                    preferred_element_type=jnp.float32) * (d ** -0.5)
    rel = (w + jnp.arange(w))[:, None] - jnp.arange(2 * w)[None, :]
    band = (rel >= 0) & (rel < w)
    key_exists = (jnp.arange(nb)[:, None] * w - w + jnp.arange(2 * w)[None, :]) >= 0
    mask = band[None, :, :] & key_exists[:, None, :]
    sc = jnp.where(mask, sc, NEG_INF)
    sink = jnp.broadcast_to(sinks.astype(jnp.float32).reshape(1, g, r, 1, 1, 1),
                            sc.shape[:-1] + (1,))
    p = jax.nn.softmax(jnp.concatenate([sc, sink], axis=-1), axis=-1)[..., :-1]
    out = jnp.einsum('bgrnqk,bgnkd->bgrnqd', p.astype(v.dtype), vv)
    return out.reshape(b, hq, s, d)


def mla_attention(c_q, c_kv, k_pe_in, q_norm_g, kv_norm_g, wq_b, wkv_b, cos, sin):
    b, s, _ = c_q.shape
    q = split_heads(rmsnorm(c_q, q_norm_g) @ wq_b, C_HEADS)
    q_nope = q[..., :C_NOPE_DIM]
    q_pe = apply_rope(q[..., C_NOPE_DIM:], cos, sin)
    kv = split_heads(rmsnorm(c_kv, kv_norm_g) @ wkv_b, C_HEADS)
    k_nope = kv[..., :C_NOPE_DIM]
    v = kv[..., C_NOPE_DIM:]
    k_pe = apply_rope(k_pe_in[:, None], cos, sin)[:, 0]
    scale = (C_NOPE_DIM + C_ROPE_DIM) ** -0.5
    nq = s // C_Q_BLOCK
    qn_b = q_nope.reshape(b, C_HEADS, nq, C_Q_BLOCK, C_NOPE_DIM).transpose(2, 0, 1, 3, 4)
    qp_b = q_pe.reshape(b, C_HEADS, nq, C_Q_BLOCK, C_ROPE_DIM).transpose(2, 0, 1, 3, 4)
    key_pos = jnp.arange(s)

    def block(args):
        qn, qp, i = args
        sc = (jnp.einsum('bhqd,bhkd->bhqk', qn, k_nope, preferred_element_type=jnp.float32)
              + jnp.einsum('bhqd,bkd->bhqk', qp, k_pe, preferred_element_type=jnp.float32)) * scale
        q_pos = i * C_Q_BLOCK + jnp.arange(C_Q_BLOCK)
        sc = jnp.where(key_pos[None, :] <= q_pos[:, None], sc, NEG_INF)
        p = jax.nn.softmax(sc, axis=-1).astype(v.dtype)
        return jnp.einsum('bhqk,bhkd->bhqd', p, v)

    out = lax.map(block, (qn_b, qp_b, jnp.arange(nq)))
    return out.transpose(1, 2, 0, 3, 4).reshape(b, C_HEADS, s, C_V_DIM)


def mixer_layer(hn, cos_a, sin_a, cos_b, sin_b, cos_c, sin_c, w_in, q_norm_g, kv_norm_g,
                wq_b, wkv_b, sinks, w_out_a, w_out_b, w_out_c, w_o):
    z = hn @ w_in
    offsets = [int(o) for o in np.cumsum(IN_SIZES)[:-1]]
    qa, ka, va, qb, kb, vb, cq, ckv, kpe, ga, gb, gc = jnp.split(z, offsets, axis=-1)
    oa = moba_attention(apply_rope(split_heads(qa, A_HEADS), cos_a, sin_a),
                        apply_rope(split_heads(ka, A_HEADS), cos_a, sin_a),
                        split_heads(va, A_HEADS))
    ob = swa_sink_attention(apply_rope(split_heads(qb, B_HEADS), cos_b, sin_b),
                            apply_rope(split_heads(kb, B_KV_HEADS), cos_b, sin_b),
                            split_heads(vb, B_KV_HEADS), sinks)
    oc = mla_attention(cq, ckv, kpe, q_norm_g, kv_norm_g, wq_b, wkv_b, cos_c, sin_c)
    y = (jax.nn.sigmoid(ga) * (merge_heads(oa) @ w_out_a)
         + jax.nn.sigmoid(gb) * (merge_heads(ob) @ w_out_b)
         + jax.nn.sigmoid(gc) * (merge_heads(oc) @ w_out_c))
    return y @ w_o


def hier_moe(hn, w_group, b_group, w_expert, b_expert, w_gate, w_up, w_down):
    b, s, d = hn.shape
    t = b * s
    xt = hn.reshape(t, d)
    tok = jnp.arange(t)
    g_logits = (xt @ w_group).astype(jnp.float32) + b_group.astype(jnp.float32)
    g_prob = jax.nn.softmax(g_logits, axis=-1)
    g_sel = jnp.argmax(g_logits, axis=-1)
    p_g = g_prob[tok, g_sel][:, None]
    e_logits = ((xt @ w_expert).astype(jnp.float32)
                + b_expert.astype(jnp.float32)).reshape(t, N_GROUPS, EXPERTS_PER_GROUP)
    e_prob = jax.nn.softmax(e_logits[tok, g_sel], axis=-1)
    top_p, top_local = lax.top_k(e_prob, EXPERT_TOPK)
    weights = p_g * top_p / jnp.sum(top_p, axis=-1, keepdims=True)
    expert_id = g_sel[:, None] * EXPERTS_PER_GROUP + top_local
    tk = t * EXPERT_TOPK
    flat_e = expert_id.reshape(tk).astype(jnp.int32)
    flat_tok = jnp.repeat(jnp.arange(t, dtype=jnp.int32), EXPERT_TOPK)
    flat_w = weights.reshape(tk)
    order = jnp.argsort(flat_e)
    e_sorted = flat_e[order]
    counts = jnp.bincount(flat_e, length=N_EXPERTS)
    padded = (counts + MOE_BLOCK - 1) // MOE_BLOCK * MOE_BLOCK
    pad_end = jnp.cumsum(padded)
    pad_start = pad_end - padded
    start = jnp.cumsum(counts) - counts
    dest = pad_start[e_sorted] + jnp.arange(tk) - start[e_sorted]
    n_blocks = -(-tk // MOE_BLOCK) + N_EXPERTS
    n_buf = n_blocks * MOE_BLOCK
    buf_tok = jnp.zeros((n_buf,), jnp.int32).at[dest].set(flat_tok[order])
    buf_w = jnp.zeros((n_buf,), jnp.float32).at[dest].set(flat_w[order])
    block_expert = jnp.minimum(
        jnp.searchsorted(pad_end, jnp.arange(n_blocks) * MOE_BLOCK, side='right'), N_EXPERTS - 1)
    xb = xt[buf_tok].reshape(n_blocks, MOE_BLOCK, d)

    def expert_block(args):
        xblk, e = args
        return (jax.nn.silu(xblk @ w_gate[e]) * (xblk @ w_up[e])) @ w_down[e]

    yb = lax.map(expert_block, (xb, block_expert)).reshape(n_buf, d)
    out = jnp.zeros((t, d), hn.dtype).at[buf_tok].add(yb * buf_w[:, None].astype(hn.dtype))
    return out.reshape(b, s, d)


def setup_inputs(seed: int = 0) -> dict:
    key = jax.random.key(seed)
    ks = jax.random.split(key, 24)
    f32 = jnp.float32
    L, D = DEPTH, D_MODEL

    def normal(k, shape, scale):
        return jax.random.normal(k, shape, f32) * scale

    def gain(k, shape):
        return 1.0 + 0.02 * jax.random.normal(k, shape, f32)

    offset = jax.random.randint(ks[1], (BATCH, 1), 0, 1024, dtype=jnp.int32)
    positions = offset + jnp.arange(SEQ, dtype=jnp.int32)[None, :]
    return {
        'x': normal(ks[0], (BATCH, SEQ, D), 1.0),
        'positions': positions,
        'attn_norm_g': gain(ks[2], (L, D)),
        'w_in': normal(ks[3], (L, D, N_IN), D ** -0.5),
        'q_norm_g': gain(ks[4], (L, C_Q_RANK)),
        'kv_norm_g': gain(ks[5], (L, C_KV_RANK)),
        'wq_b': normal(ks[6], (L, C_Q_RANK, C_HEADS * (C_NOPE_DIM + C_ROPE_DIM)), C_Q_RANK ** -0.5),
        'wkv_b': normal(ks[7], (L, C_KV_RANK, C_HEADS * (C_NOPE_DIM + C_V_DIM)), C_KV_RANK ** -0.5),
        'sinks': normal(ks[8], (L, B_HEADS), 0.5),
        'w_out_a': normal(ks[9], (L, A_WIDTH, D), A_WIDTH ** -0.5),
        'w_out_b': normal(ks[10], (L, B_WIDTH, D), B_WIDTH ** -0.5),
        'w_out_c': normal(ks[11], (L, C_WIDTH, D), C_WIDTH ** -0.5),
        'w_o': normal(ks[12], (L, D, D), D ** -0.5),
        'ffn_norm_g': gain(ks[13], (L, D)),
        'w_group': normal(ks[14], (L, D, N_GROUPS), D ** -0.5),
        'b_group': normal(ks[15], (L, N_GROUPS), 0.01),
        'w_expert': normal(ks[16], (L, D, N_EXPERTS), D ** -0.5),
        'b_expert': normal(ks[17], (L, N_EXPERTS), 0.01),
        'w_gate': normal(ks[18], (L, N_EXPERTS, D, D_FF_EXPERT), D ** -0.5),
        'w_up': normal(ks[19], (L, N_EXPERTS, D, D_FF_EXPERT), D ** -0.5),
        'w_down': normal(ks[20], (L, N_EXPERTS, D_FF_EXPERT, D), D_FF_EXPERT ** -0.5),
        'final_norm_g': gain(ks[21], (D,)),
    }


def reference(x, positions, attn_norm_g, w_in, q_norm_g, kv_norm_g, wq_b, wkv_b, sinks,
              w_out_a, w_out_b, w_out_c, w_o, ffn_norm_g, w_group, b_group, w_expert,
              b_expert, w_gate, w_up, w_down, final_norm_g):
    cos_a, sin_a = rope_tables(positions, A_HEAD_DIM)
    cos_b, sin_b = rope_tables(positions, B_HEAD_DIM)
    cos_c, sin_c = rope_tables(positions, C_ROPE_DIM)
    h = x
    for l in range(DEPTH):
        h = h + mixer_layer(rmsnorm(h, attn_norm_g[l]), cos_a, sin_a, cos_b, sin_b, cos_c, sin_c,
                            w_in[l], q_norm_g[l], kv_norm_g[l], wq_b[l], wkv_b[l], sinks[l],
                            w_out_a[l], w_out_b[l], w_out_c[l], w_o[l])
        h = h + hier_moe(rmsnorm(h, ffn_norm_g[l]), w_group[l], b_group[l], w_expert[l],
                         b_expert[l], w_gate[l], w_up[l], w_down[l])
    return rmsnorm(h, final_norm_g)
```

```python
import numpy as np
import contextlib
import concourse.bass as bass
import concourse.mybir as mybir
from concourse.bass_utils import run_bass_kernel_spmd

F32 = mybir.dt.float32
BF16 = mybir.dt.bfloat16
I32 = mybir.dt.int32
ALU = mybir.AluOpType
AF = mybir.ActivationFunctionType
AX = mybir.AxisListType

ENGS = ("pe", "act", "dve", "pool", "sp")
NDSEM = {"sp": 24, "pool": 24, "act": 8}


class Op:
    __slots__ = ("eng", "fn", "deps", "dma", "sig", "sigval", "dslot", "dval", "out")

    def __init__(self, eng, fn, dma):
        self.eng = eng
        self.fn = fn
        self.dma = dma
        self.deps = []
        self.sig = False
        self.sigval = 0
        self.dslot = 0
        self.dval = 0
        self.out = False


class Buf:
    __slots__ = ("name", "w", "rs", "rd")

    def __init__(self, name=""):
        self.name = name
        self.w = None
        self.rs = {}
        self.rd = []


class Sched:
    def __init__(self):
        self.ops = {e: [] for e in ENGS}
        self.ndma = {e: 0 for e in ENGS}

    def add(self, eng, fn, r=(), w=(), dma=False, out=False):
        op = Op(eng, fn, dma)
        op.out = out
        deps = {}
        def adddep(d):
            if d is None or d is op:
                return
            if d.eng == "pe" and eng == "pe" and not d.dma and not dma:
                return
            deps[id(d)] = d
        for b in r:
            adddep(b.w)
        for b in w:
            adddep(b.w)
            for d in b.rs.values():
                adddep(d)
            for d in b.rd:
                adddep(d)
        op.deps = list(deps.values())
        for b in r:
            if dma:
                b.rd.append(op)
            else:
                b.rs[eng] = op
        for b in w:
            b.w = op
            b.rs = {}
            b.rd = []
        if dma:
            K = NDSEM[eng]
            i = self.ndma[eng]
            self.ndma[eng] = i + 1
            op.dslot = i % K
            op.dval = 16 * (i // K + 1)
        self.ops[eng].append(op)
        return op

    def barrier(self):
        lasts = []
        for e in ENGS:
            comp = [o for o in self.ops[e] if not o.dma and o.fn is not None]
            if comp:
                lasts.append(comp[-1])
            lasts.extend(o for o in self.ops[e][-64:] if o.dma)
        for e in ENGS:
            op = Op(e, None, False)
            op.deps = [d for d in lasts]
            self.ops[e].append(op)

    def emit(self, nc, block, csem, dsem):
        for e in ENGS:
            for op in self.ops[e]:
                for d in op.deps:
                    d.sig = True
        for e in ENGS:
            cnt = 0
            for op in self.ops[e]:
                if not op.dma and op.sig and op.fn is not None:
                    cnt += 1
                    op.sigval = cnt
        sched = self

        def run(e, engine):
            waited = {}
            def wait(key, sem, val):
                if waited.get(key, 0) >= val:
                    return
                engine.wait_ge(sem, val)
                waited[key] = val
            outs = []
            for op in sched.ops[e]:
                for d in op.deps:
                    if d.dma:
                        wait(("d", d.eng, d.dslot), dsem[d.eng][d.dslot], d.dval)
                    else:
                        wait(("c", d.eng), csem[d.eng], d.sigval)
                if op.fn is None:
                    continue
                if op.dma:
                    if op.dval > 16:
                        wait(("d", e, op.dslot), dsem[e][op.dslot], op.dval - 16)
                    ins = op.fn(engine)
                    ins.then_inc(dsem[e][op.dslot], 16)
                    if op.out:
                        outs.append(op)
                else:
                    ins = op.fn(engine)
                    if op.sig:
                        ins.then_inc(csem[e], 1)
            for op in outs:
                wait(("d", e, op.dslot), dsem[e][op.dslot], op.dval)

        @block.tensor
        def _(eng):
            run("pe", eng)

        @block.scalar
        def _(eng):
            run("act", eng)

        @block.vector
        def _(eng):
            run("dve", eng)

        @block.gpsimd
        def _(eng):
            run("pool", eng)

        @block.sync
        def _(eng):
            run("sp", eng)


class Arena:
    def __init__(self, handle_f32, nbytes):
        self.h = handle_f32
        self.nbytes = nbytes
        self.off = 0
        self.marks = []

    def alloc(self, nelem, dtype):
        esz = 2 if dtype == BF16 else 4
        nb = (nelem * esz + 63) // 64 * 64
        assert self.off + nb <= self.nbytes, ("SBUF arena overflow", self.off, nb, self.nbytes)
        a = self.h[:, self.off // 4:(self.off + nb) // 4]
        self.off += nb
        if dtype == BF16:
            a = a.bitcast(BF16)
        elif dtype == I32:
            a = a.bitcast(I32)
        return a[:, 0:nelem]

    def mark(self):
        self.marks.append(self.off)

    def release(self):
        self.off = self.marks.pop()


class Ctx:
    pass


def build_program(body, arena_bytes=180 * 1024):
    nc = bass.Bass("TRN2", target_bir_lowering=False)
    cx = Ctx()
    cx.nc = nc
    cx.s = Sched()
    with contextlib.ExitStack() as es:
        arena_h = es.enter_context(nc.sbuf_tensor("arena", [128, arena_bytes // 4], F32))
        cx.arena = Arena(arena_h, arena_bytes)
        cx.psum = []
        cx.psb = []
        for i in range(8):
            p = es.enter_context(nc.psum_tensor(f"ps{i}", [128, 512], F32))
            cx.psum.append(p)
            cx.psb.append(Buf(f"ps{i}"))
        csem = {e: es.enter_context(nc.semaphore(f"c_{e}")) for e in ENGS}
        dsem = {e: [es.enter_context(nc.semaphore(f"d_{e}{i}")) for i in range(n)] for e, n in NDSEM.items()}
        body(cx)
        block = es.enter_context(nc.Block())
        cx.s.emit(nc, block, csem, dsem)
    return nc


import math
import numpy as np
import ml_dtypes


class Cfg:
    def __init__(self, **kw):
        self.D = 4096; self.S = 4096; self.DEPTH = 2
        self.AH = 16; self.AD = 128; self.MBLK = 256; self.TOPK = 3
        self.BH = 32; self.BKV = 4; self.BD = 64; self.WIN = 128
        self.CH = 16; self.CQ = 1024; self.CKV = 512; self.CN = 128; self.CR = 64; self.CV = 128
        self.NG = 8; self.EPG = 4; self.FF = 768
        self.SB = 1024
        self.MG = 1024
        self.CAP = 128
        self.EPS = 1e-6; self.THETA = 10000.0
        for k, v in kw.items():
            setattr(self, k, v)
        c = self
        c.AW = c.AH * c.AD; c.BW = c.BH * c.BD; c.BKW = c.BKV * c.BD; c.CW = c.CH * c.CV
        sizes = (c.AW, c.AW, c.AW, c.BW, c.BKW, c.BKW, c.CQ, c.CKV, c.CR, c.D, c.D, c.D)
        c.OFF = [0] + list(np.cumsum(sizes))
        c.NIN = int(c.OFF[-1])
        c.NE = c.NG * c.EPG
        c.NT = c.S // 128
        c.NBLK = c.S // c.MBLK


def dma(cx, q, out_ap, in_ap, r=(), w=(), out=False, slow=False):
    if slow:
        return cx.s.add(q, lambda e: e.dma_start(out=out_ap, in_=in_ap, allow_slow_non_contiguous=True),
                        r=r, w=w, dma=True, out=out)
    return cx.s.add(q, lambda e: e.dma_start(out=out_ap, in_=in_ap), r=r, w=w, dma=True, out=out)


def I(cx, eng, meth, *args, r=(), w=(), **kw):
    return cx.s.add(eng, lambda e: getattr(e, meth)(*args, **kw), r=r, w=w)


class Tile:
    def __init__(self, cx, shape, dtype, name=""):
        n = int(np.prod(shape[1:]))
        self.ap = cx.arena.alloc(n, dtype)
        self.p = shape[0]
        if len(shape) == 3:
            self.v = self.ap[0:shape[0], :].rearrange("p (a b) -> p a b", b=shape[2])
        else:
            self.v = self.ap[0:shape[0], :]
        self.b = Buf(name)


def psum_f32(cx, i):
    return cx.psum[i]


def psum_bf16(cx, i):
    return cx.psum[i][:, :].bitcast(BF16)


class Consts:
    pass


def const_inputs(c):
    bf = ml_dtypes.bfloat16
    d = {}
    d["c_ident"] = np.eye(128, dtype=np.float32).astype(bf)
    def rot(dim, reps):
        P = np.zeros((128, 128), np.float32)
        h = dim // 2
        for r in range(reps):
            o = r * dim
            for i in range(dim):
                if i < h:
                    P[o + i + h, o + i] = -1.0
                else:
                    P[o + i - h, o + i] = 1.0
        return P.astype(bf)
    d["c_rot128"] = rot(128, 1)
    d["c_rot64"] = rot(64, 2)
    NEG = -30000.0
    k = np.arange(128)[:, None]
    q = np.arange(512)[None, :]
    m = np.zeros((128, 4, 512), np.float32)
    for j in range(4):
        m[:, j, :] = np.where(j * 128 + k <= q, 0.0, NEG)
    d["c_cmask"] = m.reshape(128, 2048).astype(bf)
    qq = np.arange(128)[None, :]
    md = np.where(k <= qq, 0.0, NEG)
    mp = np.where(k > qq, 0.0, NEG)
    d["c_smask"] = np.concatenate([np.tile(md, (1, 4)), np.tile(mp, (1, 4))], axis=1).astype(bf)
    ng = np.full((128, 128), NEG, np.float32)
    d["c_smask2"] = np.concatenate([mp, md, mp, md, ng, md, ng, md], axis=1).astype(bf)
    E = np.zeros((16, c.S), np.float32)
    for n in range(c.NBLK):
        E[n, n * c.MBLK:(n + 1) * c.MBLK] = 1.0
    d["c_eblk"] = E.astype(bf)
    past = np.zeros((128, c.NT, 16), np.float32)
    gm = np.zeros((128, c.NT, 16), np.float32)
    for t in range(c.NT):
        own = (t * 128) // c.MBLK
        past[:, t, :own] = 1.0
        gm[:, t, own:] = -1e30
    d["c_past"] = past.reshape(128, c.NT * 16)
    d["c_gmask"] = gm.reshape(128, c.NT * 16)
    def fr(dim):
        p = np.arange(128)
        i = (p % dim) % (dim // 2)
        return (-(2.0 * i) / dim).astype(np.float32).reshape(128, 1)
    d["c_fexp"] = np.concatenate([fr(128), fr(64)], axis=1)
    d["c_iota"] = np.tile(np.arange(128, dtype=np.float32)[None, :], (128, 1))
    tri = (np.arange(128)[:, None] < np.arange(128)[None, :]).astype(np.float32)
    d["c_tri"] = tri.astype(bf)
    d["c_ones"] = np.ones((128, 128), np.float32).astype(bf)
    d["c_identf"] = np.eye(128, dtype=np.float32)
    return d


CONST_DT = {"c_ident": BF16, "c_rot128": BF16, "c_rot64": BF16, "c_cmask": BF16, "c_smask": BF16,
            "c_eblk": BF16, "c_smask2": BF16, "c_past": F32, "c_gmask": F32, "c_fexp": F32, "c_iota": F32,
            "c_tri": BF16, "c_ones": BF16, "c_identf": F32}


def load_consts(cx, c, cin):
    K = Consts()
    def ld(name, shape, dt):
        t = Tile(cx, shape, dt, name)
        dma(cx, "sp", t.v, cin[name], w=[t.b])
        return t
    K.ident = ld("c_ident", [128, 128], BF16)
    K.rot128 = ld("c_rot128", [128, 128], BF16)
    K.rot64 = ld("c_rot64", [128, 128], BF16)
    K.cmask = ld("c_cmask", [128, 2048], BF16)
    K.smask = ld("c_smask", [128, 1024], BF16)
    K.ones = ld("c_ones", [128, 128], BF16)
    K.fexp = ld("c_fexp", [128, 2], F32)
    return K


def build_rope(cx, c, K, pos_dram, which, cosT, sinT):
    S = c.S
    cx.arena.mark()
    pi = Tile(cx, [128, S], I32, "pos_i")
    pf = Tile(cx, [128, S], F32, "pos_f")
    invf = Tile(cx, [128, 1], F32, "invf")
    tmp = Tile(cx, [128, S], F32, "rtmp")
    dma(cx, "sp", pi.v, pos_dram.partition_broadcast(128), w=[pi.b])
    I(cx, "dve", "tensor_copy", pf.v, pi.v, r=[pi.b], w=[pf.b])
    col = K.fexp.v[:, which:which + 1]
    I(cx, "act", "activation", out=invf.v, in_=col, func=AF.Exp, scale=math.log(c.THETA),
      r=[K.fexp.b], w=[invf.b])
    TWO_PI = 2.0 * math.pi
    ki = Tile(cx, [128, S], I32, "rk_i")
    for (dst, shift) in ((sinT, 0.0), (cosT, 0.5 * math.pi)):
        I(cx, "dve", "tensor_scalar", tmp.v, pf.v, invf.v[:, 0:1], shift, ALU.mult, ALU.add,
          r=[pf.b, invf.b], w=[tmp.b])
        I(cx, "dve", "tensor_scalar", pi.v.bitcast(F32), tmp.v, 1.0 / TWO_PI, 0.0, ALU.mult, ALU.add,
          r=[tmp.b], w=[pi.b])
        I(cx, "dve", "tensor_copy", ki.v, pi.v.bitcast(F32), r=[pi.b], w=[ki.b])
        I(cx, "dve", "tensor_copy", pi.v.bitcast(F32), ki.v, r=[ki.b], w=[pi.b])
        I(cx, "dve", "scalar_tensor_tensor", tmp.v, pi.v.bitcast(F32), -TWO_PI, tmp.v, ALU.mult, ALU.add,
          r=[pi.b, tmp.b], w=[tmp.b])
        I(cx, "act", "activation", out=dst.v, in_=tmp.v, func=AF.Sin, r=[tmp.b], w=[dst.b])
    cx.s.barrier()
    cx.arena.release()


def phase_rmsnorm_T(cx, c, K, h_d, g_d, hnT_d, hn_tok_d=None, hn_f32T_d=None):
    D, S = c.D, c.S
    NC = D // 128
    cx.arena.mark()
    gb = Tile(cx, [128, D], F32, "g_bc")
    dma(cx, "sp", gb.v, g_d.partition_broadcast(128), w=[gb.b])
    xt = [Tile(cx, [128, D], F32, f"x{i}") for i in range(2)]
    junk = Tile(cx, [128, D], BF16, "junk")
    xn = [Tile(cx, [128, D], BF16, f"xn{i}") for i in range(2)]
    ss = [Tile(cx, [128, 2], F32, f"ss{i}") for i in range(2)]
    TB = 4 if S >= 512 else S // 128
    hT = [Tile(cx, [128, NC, TB * 128], BF16, f"hT{i}") for i in range(2)]
    nblk = S // (128 * TB)
    pb = 0
    for blk in range(nblk):
        ht = hT[blk % 2]
        for sub in range(TB):
            t = blk * TB + sub
            x = xt[t % 2]; n = xn[t % 2]; s2 = ss[t % 2]
            dma(cx, "sp", x.v, h_d[t * 128:(t + 1) * 128, :], w=[x.b])
            I(cx, "act", "activation", out=junk.v, in_=x.v, func=AF.Square, accum_out=s2.v[:, 0:1],
              r=[x.b], w=[junk.b, s2.b])
            I(cx, "dve", "tensor_scalar", s2.v[:, 1:2], s2.v[:, 0:1], 1.0 / D, c.EPS, ALU.mult, ALU.add,
              r=[s2.b], w=[s2.b])
            I(cx, "act", "activation", out=s2.v[:, 1:2], in_=s2.v[:, 1:2], func=AF.Sqrt, r=[s2.b], w=[s2.b])
            I(cx, "dve", "reciprocal", s2.v[:, 1:2], s2.v[:, 1:2], r=[s2.b], w=[s2.b])
            I(cx, "dve", "scalar_tensor_tensor", n.v, x.v, s2.v[:, 1:2], gb.v, ALU.mult, ALU.mult,
              r=[x.b, s2.b, gb.b], w=[n.b])
            if hn_tok_d is not None:
                dma(cx, "sp", hn_tok_d[t * 128:(t + 1) * 128, :], n.v, r=[n.b])
            for g8 in range(0, NC, 8):
                bank = pb % 2
                pb += 1
                pt = psum_bf16(cx, bank)
                nn = min(8, NC - g8)
                for j in range(nn):
                    cc = g8 + j
                    I(cx, "pe", "transpose", pt[:, j * 128:(j + 1) * 128], n.v[:, cc * 128:(cc + 1) * 128],
                      K.ident.v, r=[n.b, K.ident.b], w=[cx.psb[bank]])
                eng = "act" if (pb % 2) else "dve"
                src = pt[:, 0:nn * 128].rearrange("p (a b) -> p a b", b=128)
                dstv = ht.v[:, g8:g8 + nn, sub * 128:(sub + 1) * 128]
                if eng == "act":
                    I(cx, "act", "copy", dstv, src, r=[cx.psb[bank]], w=[ht.b])
                else:
                    I(cx, "dve", "tensor_copy", dstv, src, r=[cx.psb[bank]], w=[ht.b])
        dst = hnT_d[:, blk * TB * 128:(blk + 1) * TB * 128].rearrange("(a p) s -> p a s", p=128)
        dma(cx, "sp", dst, ht.v, r=[ht.b])
    cx.s.barrier()
    cx.arena.release()


def phase_proj(cx, c, xT_d, Kdim, W_d, col_tiles, mode, epi, SB=None, PW=256, banks=(2, 3), prep=None):
    S = c.S
    SB = SB or min(c.SB, S)
    KC = Kdim // 128
    cx.arena.mark()
    xb = Tile(cx, [128, KC, SB], BF16, "xblk")
    KQ = 8 if KC >= 8 else KC
    NP = KC // KQ
    stg = [Tile(cx, [128, KQ, PW], F32, f"wstg{i}") for i in range(3)]
    wp = [Tile(cx, [128, KC, PW], BF16, f"wp{i}") for i in range(2)]
    panels = []
    cur = []
    for ct in col_tiles:
        if cur and (ct[0] != cur[-1][0] + cur[-1][1] or (ct[0] + ct[1] - cur[0][0]) > PW):
            panels.append(cur); cur = []
        cur.append(ct)
    if cur:
        panels.append(cur)
    nstg = 0
    npan = 0
    nbank = 0
    TS = 512 if SB >= 512 else SB
    for s0 in range(0, S, SB):
        dma(cx, "sp", xb.v, xT_d[:, s0:s0 + SB].rearrange("(a p) s -> p a s", p=128), w=[xb.b])
        if prep is not None:
            prep(cx, xb, SB)
        for pan in panels:
            w = wp[npan % 2]
            npan += 1
            c0 = pan[0][0]
            pw = pan[-1][0] + pan[-1][1] - c0
            for q in range(NP):
                st = stg[nstg % 3]
                nstg += 1
                src = W_d[q * KQ * 128:(q + 1) * KQ * 128, c0:c0 + pw].rearrange("(a p) n -> p a n", p=128)
                dma(cx, "act" if (nstg % 2) else "sp", st.v[:, :, 0:pw], src, w=[st.b])
                I(cx, "pool", "tensor_copy", w.v[:, q * KQ:(q + 1) * KQ, 0:pw], st.v[:, :, 0:pw],
                  r=[st.b], w=[w.b])
            for (cs, cw, tag) in pan:
                lo = cs - c0
                if mode == "T":
                    for ts in range(0, SB, TS):
                        bank = banks[nbank % len(banks)]
                        nbank += 1
                        ps = cx.psum[bank][0:cw, 0:TS]
                        for kc in range(KC):
                            I(cx, "pe", "matmul", ps, w.v[:, kc, lo:lo + cw], xb.v[:, kc, ts:ts + TS],
                              start=(kc == 0), stop=(kc == KC - 1), r=[w.b, xb.b], w=[cx.psb[bank]])
                        epi(cx, tag, s0 + ts, TS, ps, cx.psb[bank])
                else:
                    for ts in range(0, SB, 128):
                        bank = banks[nbank % len(banks)]
                        nbank += 1
                        ps = cx.psum[bank][:, 0:cw]
                        for kc in range(KC):
                            I(cx, "pe", "matmul", ps, xb.v[:, kc, ts:ts + 128], w.v[:, kc, lo:lo + cw],
                              start=(kc == 0), stop=(kc == KC - 1), r=[w.b, xb.b], w=[cx.psb[bank]])
                        epi(cx, tag, s0 + ts, 128, ps, cx.psb[bank])
    cx.s.barrier()
    cx.arena.release()


def dram(cx, name, shape, dtype):
    kind = "ExternalOutput" if name in cx.debug else "Internal"
    return cx.nc.dram_tensor(name, list(shape), dtype, kind=kind).ap()


class Rot:
    def __init__(self, cx, n, shape, dtype, name):
        self.t = [Tile(cx, shape, dtype, f"{name}{i}") for i in range(n)]
        self.i = 0

    def next(self):
        t = self.t[self.i % len(self.t)]
        self.i += 1
        return t


def make_epilogues(cx, c, K, R):
    E = Consts()
    E.xs = Rot(cx, 2, [128, 512], BF16, "e_xs")
    E.t1 = Rot(cx, 2, [128, 512], F32, "e_t1")
    E.t2 = Rot(cx, 2, [128, 512], F32, "e_t2")
    E.cs = Rot(cx, 2, [128, 1024], F32, "e_cs")
    E.ob = Rot(cx, 3, [128, 512], BF16, "e_ob")
    E.rotbank = 4
    E.trbank = 5
    E.n = 0
    return E


def epi_copy(cx, E, ps, psb, cw, ns, dst_ap, func=None, scale_ap=None, scale_buf=None, mul_ap=None, mul_buf=None):
    o = E.ob.next()
    E.n += 1
    ov = o.v[0:cw, 0:ns]
    if func is not None:
        I(cx, "act", "activation", out=ov, in_=ps, func=func, r=[psb], w=[o.b])
    elif scale_ap is not None:
        I(cx, "dve", "tensor_scalar", ov, ps, scale_ap, None, ALU.mult, r=[psb, scale_buf], w=[o.b])
    elif mul_ap is not None:
        I(cx, "dve", "tensor_tensor", ov, ps, mul_ap, ALU.mult, r=[psb, mul_buf], w=[o.b])
    elif E.n % 2:
        I(cx, "act", "copy", ov, ps, r=[psb], w=[o.b])
    else:
        I(cx, "dve", "tensor_copy", ov, ps, r=[psb], w=[o.b])
    dma(cx, "sp", dst_ap, ov, r=[o.b])


def epi_rope(cx, c, K, E, ps, psb, cw, ns, s0, dst_ap, rot, cos_d, sin_d, mul_ap=None, mul_buf=None):
    xs = E.xs.next(); t1 = E.t1.next(); t2 = E.t2.next(); cs = E.cs.next(); o = E.ob.next()
    dma(cx, "act", cs.v[0:cw, 0:ns], cos_d[0:cw, s0:s0 + ns], w=[cs.b])
    dma(cx, "act", cs.v[0:cw, 512:512 + ns], sin_d[0:cw, s0:s0 + ns], w=[cs.b])
    xv = xs.v[0:cw, 0:ns]
    if mul_ap is not None:
        I(cx, "dve", "tensor_tensor", xv, ps, mul_ap, ALU.mult, r=[psb, mul_buf], w=[xs.b])
    else:
        I(cx, "act", "copy", xv, ps, r=[psb], w=[xs.b])
    rb = E.rotbank
    rp = cx.psum[rb][0:cw, 0:ns]
    I(cx, "pe", "matmul", rp, rot.v[0:cw, 0:cw], xv, start=True, stop=True, r=[rot.b, xs.b], w=[cx.psb[rb]])
    I(cx, "dve", "tensor_tensor", t1.v[0:cw, 0:ns], xv, cs.v[0:cw, 0:ns], ALU.mult, r=[xs.b, cs.b], w=[t1.b])
    I(cx, "dve", "tensor_tensor", t2.v[0:cw, 0:ns], rp, cs.v[0:cw, 512:512 + ns], ALU.mult,
      r=[cx.psb[rb], cs.b], w=[t2.b])
    I(cx, "pool", "tensor_tensor", o.v[0:cw, 0:ns], t1.v[0:cw, 0:ns], t2.v[0:cw, 0:ns], ALU.add,
      r=[t1.b, t2.b], w=[o.b])
    dma(cx, "sp", dst_ap, o.v[0:cw, 0:ns], r=[o.b])


def epi_T2N(cx, c, K, E, ps, psb, cw, ns, s0, dst_rows_fn, mul_ap=None, mul_buf=None):
    xs = E.xs.next(); o = E.ob.next()
    xv = xs.v[0:cw, 0:ns]
    if mul_ap is not None:
        I(cx, "dve", "tensor_tensor", xv, ps, mul_ap, ALU.mult, r=[psb, mul_buf], w=[xs.b])
    else:
        I(cx, "act", "copy", xv, ps, r=[psb], w=[xs.b])
    tb = E.trbank
    pt = psum_bf16(cx, tb)
    nt = ns // 128
    for i in range(nt):
        I(cx, "pe", "transpose", pt[:, i * 128:i * 128 + cw], xs.v[0:cw, i * 128:(i + 1) * 128],
          K.ident.v[0:cw, 0:cw], r=[xs.b, K.ident.b], w=[cx.psb[tb]])
    ov = o.v[:, 0:nt * cw].rearrange("p (a b) -> p a b", b=cw)
    src = pt[:, 0:nt * 128].rearrange("p (a b) -> p a b", b=128)[:, :, 0:cw]
    I(cx, "act", "copy", ov, src, r=[cx.psb[tb]], w=[o.b])
    dma(cx, "sp", dst_rows_fn(s0, ns), ov, r=[o.b])


def phase_A(cx, c, K, E, R, T, hnT_d, w_in_d):
    O = c.OFF
    tilesT = []
    for h in range(c.AH):
        tilesT.append((O[0] + 128 * h, 128, ("ropeA", T["qaT"], h)))
    for h in range(c.AH):
        tilesT.append((O[1] + 128 * h, 128, ("ropeA", T["kaT"], h)))
    for j in range(c.BW // 128):
        tilesT.append((O[3] + 128 * j, 128, ("ropeB", T["qbT"], j)))
    for j in range(c.BKW // 128):
        tilesT.append((O[4] + 128 * j, 128, ("ropeB", T["kbT"], j)))
    for j in range(c.CQ // 128):
        tilesT.append((O[6] + 128 * j, 128, ("copy", T["cqT"], j)))
    for j in range(c.CKV // 128):
        tilesT.append((O[7] + 128 * j, 128, ("copy", T["ckvT"], j)))
    tilesT.append((O[8], c.CR, ("ropeK", T["kpeT"], 0)))
    for i in range(3):
        for j in range(c.D // 128):
            tilesT.append((O[9 + i] + 128 * j, 128, ("sig", T["gT"], i * (c.D // 128) + j)))

    def epiT(cx, tag, s0, ns, ps, psb):
        kind, dst, j = tag
        if kind == "ropeA":
            epi_rope(cx, c, K, E, ps, psb, 128, ns, s0, dst[j * 128:(j + 1) * 128, s0:s0 + ns], K.rot128,
                     R["cosA"], R["sinA"])
        elif kind == "ropeB":
            epi_rope(cx, c, K, E, ps, psb, 128, ns, s0, dst[j * 128:(j + 1) * 128, s0:s0 + ns], K.rot64,
                     R["cosB"], R["sinB"])
        elif kind == "ropeK":
            epi_rope(cx, c, K, E, ps, psb, 64, ns, s0, dst[0:64, s0:s0 + ns], K.rot64, R["cosB"], R["sinB"])
        elif kind == "copy":
            epi_copy(cx, E, ps, psb, 128, ns, dst[j * 128:(j + 1) * 128, s0:s0 + ns])
        elif kind == "sig":
            epi_copy(cx, E, ps, psb, 128, ns, dst[j * 128:(j + 1) * 128, s0:s0 + ns], func=AF.Sigmoid)
    phase_proj(cx, c, hnT_d, c.D, w_in_d, tilesT, "T", epiT)

    tilesN = []
    for j in range(c.AW // 256):
        tilesN.append((O[2] + 256 * j, 256, (T["va"], 256 * j)))
    for j in range(max(1, c.BKW // 256)):
        wdt = min(256, c.BKW)
        tilesN.append((O[5] + wdt * j, wdt, (T["vb"], wdt * j)))

    def epiN(cx, tag, s0, ns, ps, psb):
        dst, c0 = tag
        cw = ps.shape[1]
        epi_copy(cx, E, ps, psb, 128, cw, dst[s0:s0 + ns, c0:c0 + cw])
    phase_proj(cx, c, hnT_d, c.D, w_in_d, tilesN, "N", epiN)


def phase_mla_up(cx, c, K, E, R, T, qg_d, kvg_d, wq_d, wkv_d):
    for (xT_d, KD, g_d, W_d, which) in ((T["cqT"], c.CQ, qg_d, wq_d, "q"), (T["ckvT"], c.CKV, kvg_d, wkv_d, "kv")):
        KC = KD // 128
        SB = min(c.SB, c.S)
        cx.arena.mark()
        gcol = Tile(cx, [128, KC], F32, "gcol")
        dma(cx, "sp", gcol.v, g_d.rearrange("(a p) -> p a", p=128), w=[gcol.b], slow=True)
        rstd = Tile(cx, [128, SB], F32, "rstd_bc")
        sq = Rot(cx, 2, [128, 512], BF16, "sq")

        def prep(cx, xb, SBn, KC=KC, KD=KD, gcol=gcol, rstd=rstd, sq=sq):
            bank = 6
            for ts in range(0, SBn, 512):
                n = min(512, SBn - ts)
                ps = cx.psum[bank][:, 0:n]
                for kc in range(KC):
                    s = sq.next()
                    I(cx, "act", "activation", out=s.v[:, 0:n], in_=xb.v[:, kc, ts:ts + n], func=AF.Square,
                      r=[xb.b], w=[s.b])
                    I(cx, "pe", "matmul", ps, K.ones.v, s.v[:, 0:n], start=(kc == 0), stop=(kc == KC - 1),
                      r=[K.ones.b, s.b], w=[cx.psb[bank]])
                rv = rstd.v[:, ts:ts + n]
                I(cx, "dve", "tensor_scalar", rv, ps, 1.0 / KD, c.EPS, ALU.mult, ALU.add,
                  r=[cx.psb[bank]], w=[rstd.b])
                I(cx, "act", "activation", out=rv, in_=rv, func=AF.Sqrt, r=[rstd.b], w=[rstd.b])
                I(cx, "dve", "reciprocal", rv, rv, r=[rstd.b], w=[rstd.b])
            for kc in range(KC):
                I(cx, "dve", "tensor_scalar", xb.v[:, kc, :], xb.v[:, kc, :], gcol.v[:, kc:kc + 1], None, ALU.mult,
                  r=[xb.b, gcol.b], w=[xb.b])

        if which == "q":
            tiles = []
            for h in range(c.CH):
                tiles.append((192 * h, 128, ("qn", h)))
                tiles.append((192 * h + 128, 64, ("qpe", h)))

            def epi(cx, tag, s0, ns, ps, psb, rstd=rstd, SB=SB):
                kind, h = tag
                lo = s0 % SB
                if kind == "qn":
                    epi_copy(cx, E, ps, psb, 128, ns, T["qnT"][h * 128:(h + 1) * 128, s0:s0 + ns],
                             mul_ap=rstd.v[:, lo:lo + ns], mul_buf=rstd.b)
                else:
                    epi_rope(cx, c, K, E, ps, psb, 64, ns, s0, T["qpeT"][h * 64:(h + 1) * 64, s0:s0 + ns], K.rot64,
                             R["cosB"], R["sinB"], mul_ap=rstd.v[0:64, lo:lo + ns], mul_buf=rstd.b)
        else:
            tiles = []
            for h in range(c.CH):
                tiles.append((256 * h, 128, ("kn", h)))
                tiles.append((256 * h + 128, 128, ("v", h)))

            def epi(cx, tag, s0, ns, ps, psb, rstd=rstd, SB=SB):
                kind, h = tag
                lo = s0 % SB
                if kind == "kn":
                    epi_copy(cx, E, ps, psb, 128, ns, T["knT"][h * 128:(h + 1) * 128, s0:s0 + ns],
                             mul_ap=rstd.v[:, lo:lo + ns], mul_buf=rstd.b)
                else:
                    def rows(s0, ns, h=h):
                        return T["vc"][s0:s0 + ns, h * 128:(h + 1) * 128].rearrange("(a p) d -> p a d", p=128)
                    epi_T2N(cx, c, K, E, ps, psb, 128, ns, s0, rows, mul_ap=rstd.v[:, lo:lo + ns], mul_buf=rstd.b)
        phase_proj(cx, c, xT_d, KD, W_d, tiles, "T", epi, prep=prep)
        cx.arena.release()


def phase_attn_causal(cx, c, K, H, terms, v_d, dv, scale, oT_d, moba=False, cin=None, mla_shared_k=False):
    S, NT = c.S, c.NT
    NG = S // 512
    cx.arena.mark()
    nterm = len(terms)
    QT = [[Tile(cx, [kd, S], BF16, f"QT{p}") for (_, _, kd) in terms] for p in range(2)]
    KT = [[Tile(cx, [kd, S], BF16, f"KT{p}") for (_, _, kd) in terms] for p in range(2)]
    V = [Tile(cx, [128, NT, dv + 1], BF16, f"V{p}") for p in range(2)]
    for p in range(2):
        I(cx, "pool", "memset", V[p].v[:, :, dv:dv + 1], 1.0, w=[V[p].b])
    PT = Rot(cx, 3, [128, 512], BF16, "PT")
    osb = Rot(cx, 2, [128, 4, dv], BF16, "osb")
    oT = Rot(cx, 2, [128, 512], BF16, "oT")
    rc = Rot(cx, 4, [128, 1], F32, "rc")
    if moba:
        eblk = Tile(cx, [16, S], BF16, "eblk")
        dma(cx, "sp", eblk.v, cin["c_eblk"], w=[eblk.b])
        past = Tile(cx, [128, NT, 16], F32, "past")
        dma(cx, "sp", past.v, cin["c_past"].rearrange("p (a b) -> p a b", b=16), w=[past.b])
        gmask = Tile(cx, [128, NT, 16], F32, "gmask")
        dma(cx, "sp", gmask.v, cin["c_gmask"].rearrange("p (a b) -> p a b", b=16), w=[gmask.b])
        BT = [Tile(cx, [16, S], BF16, f"BT{p}") for p in range(2)]
        kmf = Tile(cx, [128, 16], F32, "kmf")
        kmb = Tile(cx, [128, 16], BF16, "kmb")
        I(cx, "pool", "memset", kmf.v, 0.0, w=[kmf.b])
        g1 = Tile(cx, [128, NT, 16], F32, "g1")
        g2 = Tile(cx, [128, NT, 16], F32, "g2")
        eq = Tile(cx, [128, NT, 16], F32, "eq")
        mx = Tile(cx, [128, NT], F32, "mx")
        bb = Tile(cx, [128, NT, 16], BF16, "bb")
    sstep = 0
    for h in range(H):
        p = h % 2
        for i, (q_d, k_d, kd) in enumerate(terms):
            dma(cx, "sp", QT[p][i].v, q_d[h * kd:(h + 1) * kd, :], w=[QT[p][i].b])
            if mla_shared_k and i == 1:
                if h < 2:
                    dma(cx, "act", KT[p][i].v, k_d[0:kd, :], w=[KT[p][i].b])
            else:
                dma(cx, "act", KT[p][i].v, k_d[h * kd:(h + 1) * kd, :], w=[KT[p][i].b])
        dma(cx, "sp", V[p].v[:, :, 0:dv], v_d[:, h * dv:(h + 1) * dv].rearrange("(t p) d -> p t d", p=128),
            w=[V[p].b])
        if moba:
            kt0 = KT[p][0]; qt0 = QT[p][0]; bt = BT[p]
            NB = c.NBLK
            I(cx, "dve", "tensor_reduce", kmf.v[:, 0:NB], kt0.v.rearrange("p (n k) -> p n k", k=c.MBLK), AX.X, ALU.add,
              r=[kt0.b], w=[kmf.b])
            I(cx, "dve", "tensor_copy", kmb.v, kmf.v, r=[kmf.b], w=[kmb.b])
            gb_ = 7
            gp = cx.psum[gb_][:, 0:NT * 16]
            for t in range(NT):
                I(cx, "pe", "matmul", gp[:, t * 16:(t + 1) * 16], qt0.v[:, t * 128:(t + 1) * 128], kmb.v,
                  start=True, stop=True, r=[qt0.b, kmb.b], w=[cx.psb[gb_]])
            gp3 = gp.rearrange("p (a b) -> p a b", b=16)
            I(cx, "dve", "tensor_tensor", g1.v, gp3, gmask.v, ALU.add, r=[cx.psb[gb_], gmask.b], w=[g1.b])
            cur = g1
            for it in range(3):
                I(cx, "dve", "tensor_reduce", mx.v, cur.v, AX.X, ALU.max, r=[cur.b], w=[mx.b])
                if it < 2:
                    mb = mx.v.unsqueeze(2).broadcast_to([128, NT, 16])
                    I(cx, "dve", "tensor_tensor", eq.v, cur.v, mb, ALU.is_equal, r=[cur.b, mx.b], w=[eq.b])
                    I(cx, "dve", "scalar_tensor_tensor", g2.v, eq.v, -1e30, cur.v, ALU.mult, ALU.add,
                      r=[eq.b, cur.b], w=[g2.b])
                    cur = g2
            mb = mx.v.unsqueeze(2).broadcast_to([128, NT, 16])
            I(cx, "dve", "tensor_tensor", eq.v, g1.v, mb, ALU.is_ge, r=[g1.b, mx.b], w=[eq.b])
            I(cx, "dve", "tensor_tensor", eq.v, eq.v, past.v, ALU.mult, r=[eq.b, past.b], w=[eq.b])
            I(cx, "dve", "tensor_tensor", eq.v, eq.v, past.v, ALU.subtract, r=[eq.b, past.b], w=[eq.b])
            I(cx, "dve", "tensor_scalar", bb.v, eq.v, 30000.0, None, ALU.mult, r=[eq.b], w=[bb.b])
            tb = 7
            pt = psum_bf16(cx, tb)
            for t0 in range(0, NT, 8):
                n8 = min(8, NT - t0)
                for j in range(n8):
                    I(cx, "pe", "transpose", pt[0:16, j * 128:(j + 1) * 128], bb.v[:, t0 + j, :], K.ident.v,
                      r=[bb.b, K.ident.b], w=[cx.psb[tb]])
                I(cx, "act", "copy", bt.v[:, t0 * 128:(t0 + n8) * 128], pt[0:16, 0:n8 * 128], r=[cx.psb[tb]], w=[bt.b])
        for G in range(NG):
            accb = (2, 3, 4, 5)
            def acc(i):
                return cx.psum[accb[i]][:, 0:dv + 1]
            nk = 4 * G + 4
            for kt in range(nk):
                j = kt - 4 * G
                jq = max(j, 0)
                q0 = jq * 128
                N = 512 - q0
                sb_ = sstep % 2
                sstep += 1
                ps = cx.psum[sb_][:, 0:N]
                ops = []
                for i in range(nterm):
                    ops.append((KT[p][i].v[:, kt * 128:(kt + 1) * 128], QT[p][i].v[:, G * 512 + q0:(G + 1) * 512],
                                [KT[p][i].b, QT[p][i].b]))
                if moba:
                    ops.append((eblk.v[:, kt * 128:(kt + 1) * 128], BT[p].v[:, G * 512 + q0:(G + 1) * 512],
                                [eblk.b, BT[p].b]))
                if j >= 0:
                    ops.append((K.ident.v, K.cmask.v[:, j * 512 + q0:(j + 1) * 512], [K.ident.b, K.cmask.b]))
                for oi, (l, r_, bufs) in enumerate(ops):
                    I(cx, "pe", "matmul", ps, l, r_, start=(oi == 0), stop=(oi == len(ops) - 1),
                      r=bufs, w=[cx.psb[sb_]])
                ptile = PT.next()
                I(cx, "act", "activation", out=ptile.v[:, 0:N], in_=ps, func=AF.Exp, scale=scale,
                  r=[cx.psb[sb_]], w=[ptile.b])
                for i in range(jq, 4):
                    I(cx, "pe", "matmul", acc(i), ptile.v[:, i * 128 - q0:i * 128 - q0 + 128], V[p].v[:, kt, :],
                      start=(kt == 0), stop=(kt == 4 * G + i), r=[ptile.b, V[p].b], w=[cx.psb[accb[i]]])
            ob = osb.next()
            for i in range(4):
                r1 = rc.next()
                a = acc(i)
                I(cx, "dve", "reciprocal", r1.v, a[:, dv:dv + 1], r=[cx.psb[accb[i]]], w=[r1.b])
                I(cx, "dve", "tensor_scalar", ob.v[:, i, :], a[:, 0:dv], r1.v[:, 0:1], None, ALU.mult,
                  r=[cx.psb[accb[i]], r1.b], w=[ob.b])
            tb = 6
            pt = psum_bf16(cx, tb)
            for i in range(4):
                I(cx, "pe", "transpose", pt[0:dv, i * 128:(i + 1) * 128], ob.v[:, i, :], K.ident.v,
                  r=[ob.b, K.ident.b], w=[cx.psb[tb]])
            ot = oT.next()
            I(cx, "act", "copy", ot.v[0:dv, :], pt[0:dv, 0:512], r=[cx.psb[tb]], w=[ot.b])
            dma(cx, "sp", oT_d[h * dv:(h + 1) * dv, G * 512:(G + 1) * 512], ot.v[0:dv, :], r=[ot.b])
    cx.s.barrier()
    cx.arena.release()


def phase_swa(cx, c, K, qT_d, kT_d, v_d, sinks_d, oT_d, cin):
    S, NT = c.S, c.NT
    d = c.BD
    r = c.BH // c.BKV
    scale = d ** -0.5
    cx.arena.mark()
    es = Tile(cx, [128, c.BH], F32, "esink")
    dma(cx, "sp", es.v, sinks_d.partition_broadcast(128), w=[es.b])
    I(cx, "act", "activation", out=es.v, in_=es.v, func=AF.Exp, r=[es.b], w=[es.b])
    sm = Tile(cx, [128, 1024], BF16, "smask2")
    dma(cx, "sp", sm.v, cin["c_smask2"], w=[sm.b])
    KTt = [Tile(cx, [64, S], BF16, f"sK{p}") for p in range(2)]
    Vt = [Tile(cx, [128, NT, d + 1], BF16, f"sV{p}") for p in range(2)]
    for p in range(2):
        I(cx, "pool", "memset", Vt[p].v[:, :, d:d + 1], 1.0, w=[Vt[p].b])
    Q2 = [Tile(cx, [64, 2, S], BF16, f"sQ{p}") for p in range(2)]
    PT = Rot(cx, 3, [128, 512], BF16, "sPT")
    ob2 = Rot(cx, 2, [128, 128], BF16, "sob")
    oTt = Rot(cx, 2, [128, 512], BF16, "soT")
    den = Rot(cx, 4, [128, 1], F32, "sden")
    step = 0
    npair = 0
    for g in range(c.BKV):
        kp = g % 2
        dma(cx, "sp", KTt[kp].v, kT_d[g * 64:(g + 1) * 64, :], w=[KTt[kp].b])
        dma(cx, "act", Vt[kp].v[:, :, 0:d], v_d[:, g * 64:(g + 1) * 64].rearrange("(t p) d -> p t d", p=128),
            w=[Vt[kp].b])
        for hp in range(r // 2):
            h0 = g * r + 2 * hp
            q2 = Q2[npair % 2]
            npair += 1
            dma(cx, "sp", q2.v, qT_d[h0 * 64:(h0 + 2) * 64, :].rearrange("(h d) s -> d h s", d=64), w=[q2.b])
            ot = None
            for t in range(NT):
                sb_ = step % 2
                step += 1
                ps = cx.psum[sb_][:, 0:512]
                mv = sm.v[:, 0:512] if t > 0 else sm.v[:, 512:1024]
                I(cx, "pe", "matmul", ps, K.ident.v, mv, start=True, stop=False, r=[K.ident.b, sm.b], w=[cx.psb[sb_]])
                mms = []
                for hh in range(2):
                    for kk in range(2):
                        kt = t - 1 + kk
                        if kt < 0:
                            continue
                        mms.append((hh, kk, kt))
                for mi, (hh, kk, kt) in enumerate(mms):
                    col = (hh * 2 + kk) * 128
                    I(cx, "pe", "matmul", ps[:, col:col + 128], KTt[kp].v[:, kt * 128:(kt + 1) * 128],
                      q2.v[:, hh, t * 128:(t + 1) * 128], start=False, stop=(mi == len(mms) - 1),
                      r=[KTt[kp].b, q2.b], w=[cx.psb[sb_]])
                pt_ = PT.next()
                I(cx, "act", "activation", out=pt_.v, in_=ps, func=AF.Exp, scale=scale, r=[cx.psb[sb_]], w=[pt_.b])
                ab = 2 + (step % 2)
                o2 = ob2.next()
                for hh in range(2):
                    a = cx.psum[ab][:, hh * 128:hh * 128 + d + 1]
                    kks = [kk for kk in range(2) if t - 1 + kk >= 0]
                    for ki, kk in enumerate(kks):
                        col = (hh * 2 + kk) * 128
                        I(cx, "pe", "matmul", a, pt_.v[:, col:col + 128], Vt[kp].v[:, t - 1 + kk, :],
                          start=(ki == 0), stop=(ki == len(kks) - 1), r=[pt_.b, Vt[kp].b], w=[cx.psb[ab]])
                    dn = den.next()
                    I(cx, "dve", "tensor_tensor", dn.v, a[:, d:d + 1], es.v[:, h0 + hh:h0 + hh + 1], ALU.add,
                      r=[cx.psb[ab], es.b], w=[dn.b])
                    I(cx, "dve", "reciprocal", dn.v, dn.v, r=[dn.b], w=[dn.b])
                    I(cx, "dve", "tensor_scalar", o2.v[:, hh * 64:(hh + 1) * 64], a[:, 0:d], dn.v[:, 0:1], None, ALU.mult,
                      r=[cx.psb[ab], dn.b], w=[o2.b])
                tb = 4
                ptb = psum_bf16(cx, tb)
                if t % 4 == 0:
                    ot = oTt.next()
                I(cx, "pe", "transpose", ptb[:, 0:128], o2.v, K.ident.v, r=[o2.b, K.ident.b], w=[cx.psb[tb]])
                I(cx, "act", "copy", ot.v[:, (t % 4) * 128:(t % 4 + 1) * 128], ptb[:, 0:128], r=[cx.psb[tb]], w=[ot.b])
                if t % 4 == 3 or t == NT - 1:
                    n = (t % 4 + 1) * 128
                    t0 = t - t % 4
                    dma(cx, "sp", oT_d[h0 * 64:(h0 + 2) * 64, t0 * 128:t0 * 128 + n], ot.v[:, 0:n], r=[ot.b])
    cx.s.barrier()
    cx.arena.release()


def phase_out_gate(cx, c, K, oT_list, W_list, gT_d, yT_d):
    S, D = c.S, c.D
    SB = 512 if S >= 512 else S
    PW = 256
    cx.arena.mark()
    KCs = [o.shape[0] // 128 for o in oT_list]
    xb = [Tile(cx, [128, KCs[i], SB], BF16, f"ox{i}") for i in range(3)]
    wp = [[Tile(cx, [128, KCs[i], PW], BF16, f"ow{i}_{p}") for p in range(2)] for i in range(3)]
    stg = Rot(cx, 3, [128, 8, PW], F32, "ostg")
    gt = [Rot(cx, 2, [128, SB], BF16, f"og{i}") for i in range(3)]
    tt = [Rot(cx, 2, [128, SB], F32, f"ot{i}") for i in range(3)]
    yo = Rot(cx, 2, [128, SB], BF16, "oy")
    npan = 0
    nb = 0
    for s0 in range(0, S, SB):
        for i in range(3):
            dma(cx, "sp", xb[i].v, oT_list[i][:, s0:s0 + SB].rearrange("(a p) s -> p a s", p=128), w=[xb[i].b])
        for c0 in range(0, D, PW):
            pp = npan % 2
            npan += 1
            for i in range(3):
                w = wp[i][pp]
                KQ = min(8, KCs[i])
                for q in range(KCs[i] // KQ):
                    st = stg.next()
                    src = W_list[i][q * KQ * 128:(q + 1) * KQ * 128, c0:c0 + PW].rearrange("(a p) n -> p a n", p=128)
                    dma(cx, "act" if q % 2 else "sp", st.v[:, 0:KQ, :], src, w=[st.b])
                    I(cx, "pool", "tensor_copy", w.v[:, q * KQ:(q + 1) * KQ, :], st.v[:, 0:KQ, :], r=[st.b], w=[w.b])
            for lo in range(0, PW, 128):
                col = c0 + lo
                tts = []
                for i in range(3):
                    bank = (nb % 2) * 3 + i
                    ps = cx.psum[bank][:, 0:SB]
                    w = wp[i][pp]
                    for kc in range(KCs[i]):
                        I(cx, "pe", "matmul", ps, w.v[:, kc, lo:lo + 128], xb[i].v[:, kc, :],
                          start=(kc == 0), stop=(kc == KCs[i] - 1), r=[w.b, xb[i].b], w=[cx.psb[bank]])
                    g = gt[i].next()
                    dma(cx, "act", g.v, gT_d[i * D + col:i * D + col + 128, s0:s0 + SB], w=[g.b])
                    t = tt[i].next()
                    I(cx, "dve", "tensor_tensor", t.v, ps, g.v, ALU.mult, r=[cx.psb[bank], g.b], w=[t.b])
                    tts.append(t)
                nb += 1
                I(cx, "pool", "tensor_tensor", tts[0].v, tts[0].v, tts[1].v, ALU.add, r=[tts[0].b, tts[1].b], w=[tts[0].b])
                y = yo.next()
                I(cx, "pool", "tensor_tensor", y.v, tts[0].v, tts[2].v, ALU.add, r=[tts[0].b, tts[2].b], w=[y.b])
                dma(cx, "sp", yT_d[col:col + 128, s0:s0 + SB], y.v, r=[y.b])
    cx.s.barrier()
    cx.arena.release()


def phase_resid_proj(cx, c, K, xT_d, Kdim, W_d, h_d):
    cx.arena.mark()
    ht = Rot(cx, 3, [128, 256], F32, "rh")
    tiles = [(256 * j, 256, 256 * j) for j in range(c.D // 256)]

    def epi(cx, tag, s0, ns, ps, psb):
        t = ht.next()
        dma(cx, "act", t.v, h_d[s0:s0 + 128, tag:tag + 256], w=[t.b])
        I(cx, "dve", "tensor_tensor", t.v, ps, t.v, ALU.add, r=[psb, t.b], w=[t.b])
        dma(cx, "sp", h_d[s0:s0 + 128, tag:tag + 256], t.v, r=[t.b])
    phase_proj(cx, c, xT_d, Kdim, W_d, tiles, "N", epi)
    cx.arena.release()


def phase_moe(cx, c, K, T, cin, h_d, g_d, wg_d, bg_d, we_d, be_d, wgate_d, wup_d, wdown_d):
    S, D, NT, NE = c.S, c.D, c.NT, c.NE
    NC = D // 128
    NGRP = max(1, S // c.MG)
    TPG = NT // NGRP
    NSL = NGRP * c.CAP
    FC = c.FF // 128
    hn2_d = T["hn2"]
    cx.arena.mark()
    A_f = Tile(cx, [128, NT, NE], F32, "A_f")
    A_b = Tile(cx, [128, NT, NE], BF16, "A_b")
    Wt = Tile(cx, [128, NT, NE], F32, "Wt")
    pos = Tile(cx, [128, NT, NE], F32, "pos")
    iota = Tile(cx, [128, 128], F32, "iota")
    dma(cx, "sp", iota.v, cin["c_iota"], w=[iota.b])
    tri = Tile(cx, [128, 128], BF16, "tri")
    dma(cx, "sp", tri.v, cin["c_tri"], w=[tri.b])
    cx.arena.mark()
    gb = Tile(cx, [128, D], F32, "m_gbc")
    dma(cx, "sp", gb.v, g_d.partition_broadcast(128), w=[gb.b])
    wr = Tile(cx, [128, NC, 40], F32, "wr")
    dma(cx, "sp", wr.v[:, :, 0:8], wg_d.rearrange("(a p) n -> p a n", p=128), w=[wr.b])
    dma(cx, "sp", wr.v[:, :, 8:40], we_d.rearrange("(a p) n -> p a n", p=128), w=[wr.b])
    bias = Tile(cx, [128, 40], F32, "rbias")
    dma(cx, "sp", bias.v[:, 0:8], bg_d.partition_broadcast(128), w=[bias.b])
    dma(cx, "sp", bias.v[:, 8:40], be_d.partition_broadcast(128), w=[bias.b])
    xt = Rot(cx, 2, [128, D], F32, "m_x")
    xn = Rot(cx, 1, [128, D], F32, "m_xn")
    xnb = Rot(cx, 2, [128, D], BF16, "m_xnb")
    junk = Tile(cx, [128, D], BF16, "m_junk")
    xT = Rot(cx, 1, [128, NC, 128], F32, "m_xT")
    sc = Rot(cx, 2, [128, 16], F32, "m_sc")
    lg = Rot(cx, 2, [128, 40], F32, "m_lg")
    tmp = Rot(cx, 2, [128, 8, 4], F32, "m_tmp")
    small = Rot(cx, 2, [128, 32], F32, "m_small")
    nb = 0
    for t in range(NT):
        x = xt.next(); n = xn.next(); nbf = xnb.next(); s = sc.next(); l = lg.next(); tp = tmp.next(); sm = small.next()
        xTt = xT.next()
        dma(cx, "sp", x.v, h_d[t * 128:(t + 1) * 128, :], w=[x.b])
        I(cx, "act", "activation", out=junk.v, in_=x.v, func=AF.Square, accum_out=s.v[:, 0:1], r=[x.b], w=[junk.b, s.b])
        I(cx, "dve", "tensor_scalar", s.v[:, 1:2], s.v[:, 0:1], 1.0 / D, c.EPS, ALU.mult, ALU.add, r=[s.b], w=[s.b])
        I(cx, "act", "activation", out=s.v[:, 1:2], in_=s.v[:, 1:2], func=AF.Sqrt, r=[s.b], w=[s.b])
        I(cx, "dve", "reciprocal", s.v[:, 1:2], s.v[:, 1:2], r=[s.b], w=[s.b])
        I(cx, "dve", "scalar_tensor_tensor", n.v, x.v, s.v[:, 1:2], gb.v, ALU.mult, ALU.mult, r=[x.b, s.b, gb.b], w=[n.b])
        I(cx, "pool", "tensor_copy", nbf.v, n.v, r=[n.b], w=[nbf.b])
        dma(cx, "act", hn2_d[t * 128:(t + 1) * 128, :], nbf.v, r=[nbf.b])
        for g4 in range(0, NC, 4):
            bank = nb % 2
            nb += 1
            pt = cx.psum[bank]
            for j in range(4):
                cc = g4 + j
                I(cx, "pe", "transpose", pt[:, j * 128:(j + 1) * 128], n.v[:, cc * 128:(cc + 1) * 128], iota_ident(cx, K),
                  r=[n.b, K.identf.b], w=[cx.psb[bank]])
            src = pt[:, 0:512].rearrange("p (a b) -> p a b", b=128)
            if nb % 2:
                I(cx, "act", "copy", xTt.v[:, g4:g4 + 4, :], src, r=[cx.psb[bank]], w=[xTt.b])
            else:
                I(cx, "dve", "tensor_copy", xTt.v[:, g4:g4 + 4, :], src, r=[cx.psb[bank]], w=[xTt.b])
        lb = 2
        lp = cx.psum[lb][:, 0:40]
        for kc in range(NC):
            I(cx, "pe", "matmul", lp, xTt.v[:, kc, :], wr.v[:, kc, :], start=(kc == 0), stop=(kc == NC - 1),
              r=[xTt.b, wr.b], w=[cx.psb[lb]])
        I(cx, "dve", "tensor_tensor", l.v, lp, bias.v, ALU.add, r=[cx.psb[lb], bias.b], w=[l.b])
        lgv = l.v[:, 0:8]
        le3 = l.v[:, 8:40].rearrange("p (g e) -> p g e", e=4)
        m = s.v[:, 2:3]; negm = s.v[:, 3:4]; se = s.v[:, 4:5]; m1 = s.v[:, 5:6]; nm1 = s.v[:, 6:7]; m2 = s.v[:, 7:8]
        rr = s.v[:, 8:9]; w1 = s.v[:, 9:10]; w2 = s.v[:, 10:11]
        G1 = sm.v[:, 0:8]; esel = sm.v[:, 8:12]; e1 = sm.v[:, 12:16]; es2 = sm.v[:, 16:20]; e2 = sm.v[:, 20:24]
        asel = sm.v[:, 24:28]; wsel = sm.v[:, 28:32]
        R_ = [l.b, s.b, sm.b, tp.b]
        def D_(meth, *a, **k):
            I(cx, "dve", meth, *a, r=R_, w=R_, **k)
        D_("tensor_reduce", m, lgv, AX.X, ALU.max)
        D_("tensor_scalar", G1, lgv, m, None, ALU.is_equal)
        D_("tensor_scalar", negm, m, -1.0, None, ALU.mult)
        I(cx, "act", "activation", out=sm.v[:, 12:20], in_=lgv, func=AF.Exp, bias=negm, accum_out=se, r=R_, w=R_)
        D_("tensor_tensor", tp.v, le3, G1.unsqueeze(2).broadcast_to([128, 8, 4]), ALU.mult)
        D_("tensor_reduce", esel, tp.v.rearrange("p g e -> p e g"), AX.X, ALU.add)
        D_("tensor_reduce", m1, esel, AX.X, ALU.max)
        D_("tensor_scalar", e1, esel, m1, None, ALU.is_equal)
        D_("scalar_tensor_tensor", es2, e1, -1e30, esel, ALU.mult, ALU.add)
        D_("tensor_reduce", m2, es2, AX.X, ALU.max)
        D_("tensor_scalar", e2, es2, m2, None, ALU.is_equal)
        D_("tensor_scalar", nm1, m1, -1.0, None, ALU.mult)
        I(cx, "act", "activation", out=rr, in_=m2, func=AF.Exp, bias=nm1, r=R_, w=R_)
        D_("tensor_scalar", w1, rr, 1.0, None, ALU.add)
        D_("tensor_tensor", w1, w1, se, ALU.mult)
        D_("reciprocal", w1, w1)
        D_("tensor_tensor", w2, w1, rr, ALU.mult)
        D_("tensor_tensor", asel, e1, e2, ALU.add)
        D_("tensor_scalar", wsel, e1, w1, None, ALU.mult)
        D_("scalar_tensor_tensor", wsel, e2, w2, wsel, ALU.mult, ALU.add)
        A3 = A_f.v[:, t, :].rearrange("p (g e) -> p g e", e=4)
        W3 = Wt.v[:, t, :].rearrange("p (g e) -> p g e", e=4)
        g1b = G1.unsqueeze(2).broadcast_to([128, 8, 4])
        I(cx, "dve", "tensor_tensor", A3, g1b, asel.unsqueeze(1).broadcast_to([128, 8, 4]), ALU.mult, r=R_, w=[A_f.b])
        I(cx, "dve", "tensor_tensor", W3, g1b, wsel.unsqueeze(1).broadcast_to([128, 8, 4]), ALU.mult, r=R_, w=[Wt.b])
    I(cx, "dve", "tensor_copy", A_b.v, A_f.v, r=[A_f.b], w=[A_b.b])
    for t in range(NT):
        g0 = (t // TPG) * TPG
        bank = 3 + t % 2
        pp = cx.psum[bank][:, 0:NE]
        for j in range(g0, t + 1):
            l_ = K.ones.v if j < t else tri.v
            I(cx, "pe", "matmul", pp, l_, A_b.v[:, j, :], start=(j == g0), stop=(j == t),
              r=[K.ones.b, tri.b, A_b.b], w=[cx.psb[bank]])
        I(cx, "act", "copy", pos.v[:, t, :], pp, r=[cx.psb[bank]], w=[pos.b])
    cx.s.barrier()
    cx.arena.release()
    cx.arena.mark()
    hg = Tile(cx, [128, TPG, D], BF16, "hg")
    sel = Rot(cx, 2, [128, TPG, 128], BF16, "sel")
    xg = Rot(cx, 2, [128, NC, 128], BF16, "xg")
    nb = 0
    for gi in range(NGRP):
        dma(cx, "sp", hg.v, hn2_d[gi * c.MG:gi * c.MG + TPG * 128, :].rearrange("(a p) d -> p a d", p=128), w=[hg.b])
        for e in range(NE):
            sl = sel.next()
            for i in range(TPG):
                t = gi * TPG + i
                I(cx, "dve" if i % 2 else "pool", "tensor_scalar", sl.v[:, i, :], iota.v, pos.v[:, t, e:e + 1],
                  A_f.v[:, t, e:e + 1], ALU.is_equal, ALU.mult, r=[iota.b, pos.b, A_f.b], w=[sl.b])
            x = xg.next()
            for c4 in range(0, NC, 4):
                bank = nb % 3
                nb += 1
                for j in range(4):
                    cc = c4 + j
                    for i in range(TPG):
                        I(cx, "pe", "matmul", cx.psum[bank][:, j * 128:(j + 1) * 128], hg.v[:, i, cc * 128:(cc + 1) * 128],
                          sl.v[:, i, :], start=(i == 0), stop=(i == TPG - 1), r=[hg.b, sl.b], w=[cx.psb[bank]])
                src = cx.psum[bank][:, 0:512].rearrange("p (a b) -> p a b", b=128)
                if nb % 2:
                    I(cx, "act", "copy", x.v[:, c4:c4 + 4, :], src, r=[cx.psb[bank]], w=[x.b])
                else:
                    I(cx, "dve", "tensor_copy", x.v[:, c4:c4 + 4, :], src, r=[cx.psb[bank]], w=[x.b])
            dma(cx, "sp", T["xg"][e * D:(e + 1) * D, gi * 128:(gi + 1) * 128].rearrange("(a p) s -> p a s", p=128),
                x.v, r=[x.b])
    cx.s.barrier()
    cx.arena.release()
    cs = Cfg.__new__(Cfg)
    cs.__dict__.update(c.__dict__)
    cs.S = NSL; cs.SB = NSL
    cx.arena.mark()
    gs = Tile(cx, [128, FC, NSL], BF16, "gs")
    ao = Rot(cx, 2, [128, NSL], BF16, "ao")
    yo = Rot(cx, 3, [128, 256], BF16, "yo")
    for e in range(NE):
        xTe = T["xg"][e * D:(e + 1) * D, :]
        tiles = [(128 * f, 128, f) for f in range(FC)]

        def epi_g(cx, tag, s0, ns, ps, psb):
            I(cx, "act", "activation", out=gs.v[:, tag, s0:s0 + ns], in_=ps, func=AF.Silu, r=[psb], w=[gs.b])
        phase_proj(cx, cs, xTe, D, wgate_d[e], tiles, "T", epi_g)

        def epi_u(cx, tag, s0, ns, ps, psb):
            a = ao.next()
            I(cx, "dve", "tensor_tensor", a.v[:, 0:ns], ps, gs.v[:, tag, s0:s0 + ns], ALU.mult, r=[psb, gs.b], w=[a.b])
            dma(cx, "sp", T["aT"][tag * 128:(tag + 1) * 128, s0:s0 + ns], a.v[:, 0:ns], r=[a.b])
        phase_proj(cx, cs, xTe, D, wup_d[e], tiles, "T", epi_u)
        tiles_d = [(256 * j, 256, 256 * j) for j in range(D // 256)]

        def epi_d(cx, tag, s0, ns, ps, psb, e=e):
            y = yo.next()
            I(cx, "act" if (s0 // 128) % 2 else "dve", "copy" if (s0 // 128) % 2 else "tensor_copy", y.v, ps, r=[psb], w=[y.b])
            dma(cx, "sp", T["yexp"][e * NSL + s0:e * NSL + s0 + 128, tag:tag + 256], y.v, r=[y.b])
        phase_proj(cx, cs, T["aT"], c.FF, wdown_d[e], tiles_d, "N", epi_d)
    cx.arena.release()
    cx.arena.mark()
    swT = Tile(cx, [128, NE, TPG * 128], BF16, "swT")
    selw = Rot(cx, 3, [128, 128], BF16, "selw")
    yb = Rot(cx, 2, [128, NE, 512], BF16, "yb")
    ht = Rot(cx, 3, [128, 512], F32, "mh")
    nb = 0
    CW5 = 512 if D >= 512 else D
    for gi in range(NGRP):
        for e in range(NE):
            for i in range(TPG):
                t = gi * TPG + i
                s_ = selw.next()
                I(cx, "dve" if i % 2 else "pool", "tensor_scalar", s_.v, iota.v, pos.v[:, t, e:e + 1], Wt.v[:, t, e:e + 1],
                  ALU.is_equal, ALU.mult, r=[iota.b, pos.b, Wt.b], w=[s_.b])
                bank = 6 + nb % 2
                nb += 1
                pt = psum_bf16(cx, bank)
                I(cx, "pe", "transpose", pt[:, 0:128], s_.v, K.ident.v, r=[s_.b, K.ident.b], w=[cx.psb[bank]])
                I(cx, "act", "copy", swT.v[:, e, i * 128:(i + 1) * 128], pt[:, 0:128], r=[cx.psb[bank]], w=[swT.b])
        for c0 in range(0, D, CW5):
            y = yb.next()
            src = T["yexp"].rearrange("(e s) d -> s e d", e=NE)[gi * 128:(gi + 1) * 128, :, c0:c0 + CW5]
            dma(cx, "sp", y.v[:, :, 0:CW5], src, w=[y.b])
            for i in range(TPG):
                t = gi * TPG + i
                bank = nb % 3
                nb += 1
                ps = cx.psum[bank][:, 0:CW5]
                for e in range(NE):
                    I(cx, "pe", "matmul", ps, swT.v[:, e, i * 128:(i + 1) * 128], y.v[:, e, 0:CW5],
                      start=(e == 0), stop=(e == NE - 1), r=[swT.b, y.b], w=[cx.psb[bank]])
                h = ht.next()
                dma(cx, "act", h.v[:, 0:CW5], h_d[t * 128:(t + 1) * 128, c0:c0 + CW5], w=[h.b])
                I(cx, "dve", "tensor_tensor", h.v[:, 0:CW5], ps, h.v[:, 0:CW5], ALU.add, r=[cx.psb[bank], h.b], w=[h.b])
                dma(cx, "sp", h_d[t * 128:(t + 1) * 128, c0:c0 + CW5], h.v[:, 0:CW5], r=[h.b])
    cx.s.barrier()
    cx.arena.release()
    cx.arena.release()


def iota_ident(cx, K):
    return K.identf.v


def phase_final_norm(cx, c, K, h_d, g_d, out_d):
    D, S = c.D, c.S
    cx.arena.mark()
    gb = Tile(cx, [128, D], F32, "f_gbc")
    dma(cx, "sp", gb.v, g_d.partition_broadcast(128), w=[gb.b])
    xt = Rot(cx, 2, [128, D], F32, "f_x")
    xo = Rot(cx, 2, [128, D], F32, "f_o")
    junk = Tile(cx, [128, D], BF16, "f_junk")
    sc = Rot(cx, 2, [128, 2], F32, "f_sc")
    for t in range(S // 128):
        x = xt.next(); o = xo.next(); s = sc.next()
        dma(cx, "sp", x.v, h_d[t * 128:(t + 1) * 128, :], w=[x.b])
        I(cx, "act", "activation", out=junk.v, in_=x.v, func=AF.Square, accum_out=s.v[:, 0:1], r=[x.b], w=[junk.b, s.b])
        I(cx, "dve", "tensor_scalar", s.v[:, 1:2], s.v[:, 0:1], 1.0 / D, c.EPS, ALU.mult, ALU.add, r=[s.b], w=[s.b])
        I(cx, "act", "activation", out=s.v[:, 1:2], in_=s.v[:, 1:2], func=AF.Sqrt, r=[s.b], w=[s.b])
        I(cx, "dve", "reciprocal", s.v[:, 1:2], s.v[:, 1:2], r=[s.b], w=[s.b])
        I(cx, "dve", "scalar_tensor_tensor", o.v, x.v, s.v[:, 1:2], gb.v, ALU.mult, ALU.mult, r=[x.b, s.b, gb.b], w=[o.b])
        dma(cx, "act", out_d[t * 128:(t + 1) * 128, :], o.v, r=[o.b], out=True)
    cx.s.barrier()
    cx.arena.release()


WEIGHT_SHAPES = lambda c: {
    "attn_norm_g": [c.DEPTH, c.D], "w_in": [c.DEPTH, c.D, c.NIN], "q_norm_g": [c.DEPTH, c.CQ],
    "kv_norm_g": [c.DEPTH, c.CKV], "wq_b": [c.DEPTH, c.CQ, c.CH * (c.CN + c.CR)],
    "wkv_b": [c.DEPTH, c.CKV, c.CH * (c.CN + c.CV)], "sinks": [c.DEPTH, c.BH],
    "w_out_a": [c.DEPTH, c.AW, c.D], "w_out_b": [c.DEPTH, c.BW, c.D], "w_out_c": [c.DEPTH, c.CW, c.D],
    "w_o": [c.DEPTH, c.D, c.D], "ffn_norm_g": [c.DEPTH, c.D], "w_group": [c.DEPTH, c.D, c.NG],
    "b_group": [c.DEPTH, c.NG], "w_expert": [c.DEPTH, c.D, c.NE], "b_expert": [c.DEPTH, c.NE],
    "w_gate": [c.DEPTH, c.NE, c.D, c.FF], "w_up": [c.DEPTH, c.NE, c.D, c.FF],
    "w_down": [c.DEPTH, c.NE, c.FF, c.D], "final_norm_g": [c.D],
}


def build_forward(c, NB, debug=(), stop_after=None):
    cnp = const_inputs(c)

    def body(cx):
        nc = cx.nc
        cx.debug = set(debug)
        S, D = c.S, c.D
        x_d = nc.dram_tensor("x", [NB * S, D], F32, kind="ExternalInput").ap()
        pos_d = nc.dram_tensor("positions", [NB * S], I32, kind="ExternalInput").ap()
        Wd = {k: nc.dram_tensor(k, shp, F32, kind="ExternalInput").ap() for k, shp in WEIGHT_SHAPES(c).items()}
        cin = {k: nc.dram_tensor(k, list(v.shape), CONST_DT[k], kind="ExternalInput").ap() for k, v in cnp.items()}
        out_d = nc.dram_tensor("out", [NB * S, D], F32, kind="ExternalOutput").ap()
        K = load_consts(cx, c, cin)
        K.identf = Tile(cx, [128, 128], F32, "identf")
        dma(cx, "sp", K.identf.v, cin["c_identf"], w=[K.identf.b])
        T = {}
        NGRP = max(1, S // c.MG)
        NSL = NGRP * c.CAP
        spec = {"h": ([S, D], F32), "hnT": ([D, S], BF16), "qaT": ([c.AW, S], BF16), "kaT": ([c.AW, S], BF16),
                "va": ([S, c.AW], BF16), "qbT": ([c.BW, S], BF16), "kbT": ([c.BKW, S], BF16), "vb": ([S, c.BKW], BF16),
                "cqT": ([c.CQ, S], BF16), "ckvT": ([c.CKV, S], BF16), "kpeT": ([64, S], BF16),
                "gT": ([3 * D, S], BF16), "qnT": ([c.CH * 128, S], BF16), "qpeT": ([c.CH * 64, S], BF16),
                "knT": ([c.CH * 128, S], BF16), "vc": ([S, c.CH * 128], BF16),
                "oaT": ([c.AW, S], BF16), "obT": ([c.BW, S], BF16), "ocT": ([c.CW, S], BF16), "yT": ([D, S], BF16),
                "hn2": ([S, D], BF16), "xg": ([c.NE * D, NSL], BF16), "aT": ([c.FF, NSL], BF16),
                "yexp": ([c.NE * NSL, D], BF16),
                "cosA": ([128, S], F32), "sinA": ([128, S], F32), "cosB": ([128, S], F32), "sinB": ([128, S], F32)}
        for k, (shp, dt) in spec.items():
            T[k] = dram(cx, k, shp, dt)
        E = make_epilogues(cx, c, K, None)
        cx.s.barrier()
        for b in range(NB):
            xb_d = x_d[b * S:(b + 1) * S, :]
            cx.arena.mark()
            ct = Tile(cx, [128, S], F32, "ropec"); st = Tile(cx, [128, S], F32, "ropes")
            for which, (cn, sn) in enumerate((("cosA", "sinA"), ("cosB", "sinB"))):
                build_rope(cx, c, K, pos_d[b * S:(b + 1) * S], which, ct, st)
                dma(cx, "sp", T[cn], ct.v, r=[ct.b])
                dma(cx, "sp", T[sn], st.v, r=[st.b])
                cx.s.barrier()
            cx.arena.release()
            for t in range(S // 128):
                dma(cx, "sp" if t % 2 else "act", T["h"][t * 128:(t + 1) * 128, :], xb_d[t * 128:(t + 1) * 128, :])
            cx.s.barrier()
            R = T
            for l in range(c.DEPTH):
                phase_rmsnorm_T(cx, c, K, T["h"], Wd["attn_norm_g"][l], T["hnT"])
                if stop_after == "norm": break
                phase_A(cx, c, K, E, R, T, T["hnT"], Wd["w_in"][l])
                if stop_after == "A": break
                phase_mla_up(cx, c, K, E, R, T, Wd["q_norm_g"][l], Wd["kv_norm_g"][l], Wd["wq_b"][l], Wd["wkv_b"][l])
                if stop_after == "mla_up": break
                phase_attn_causal(cx, c, K, c.AH, [(T["qaT"], T["kaT"], 128)], T["va"], 128, c.AD ** -0.5, T["oaT"],
                                  moba=True, cin=cin)
                if stop_after == "moba": break
                phase_swa(cx, c, K, T["qbT"], T["kbT"], T["vb"], Wd["sinks"][l], T["obT"], cin)
                if stop_after == "swa": break
                phase_attn_causal(cx, c, K, c.CH, [(T["qnT"], T["knT"], 128), (T["qpeT"], T["kpeT"], 64)], T["vc"], 128,
                                  (c.CN + c.CR) ** -0.5, T["ocT"], mla_shared_k=True)
                if stop_after == "mla": break
                phase_out_gate(cx, c, K, [T["oaT"], T["obT"], T["ocT"]],
                               [Wd["w_out_a"][l], Wd["w_out_b"][l], Wd["w_out_c"][l]], T["gT"], T["yT"])
                phase_resid_proj(cx, c, K, T["yT"], D, Wd["w_o"][l], T["h"])
                if stop_after == "mix": break
                phase_moe(cx, c, K, T, cin, T["h"], Wd["ffn_norm_g"][l], Wd["w_group"][l], Wd["b_group"][l],
                          Wd["w_expert"][l], Wd["b_expert"][l], Wd["w_gate"][l], Wd["w_up"][l], Wd["w_down"][l])
                if stop_after == "moe": break
            phase_final_norm(cx, c, K, T["h"], Wd["final_norm_g"], out_d[b * S:(b + 1) * S, :])
    nc = build_program(body)
    return nc, cnp


_CACHE = {}


def kernel(**inputs):
    c = Cfg()
    if "nc" not in _CACHE:
        _CACHE["nc"] = build_forward(c, 1)
    nc, cnp = _CACHE["nc"]
    B = inputs["x"].shape[0]
    in_maps = []
    for b in range(B):
        m = {"x": np.ascontiguousarray(np.asarray(inputs["x"][b], dtype=np.float32)),
             "positions": np.ascontiguousarray(np.asarray(inputs["positions"][b]).astype(np.int32))}
        for k in WEIGHT_SHAPES(c):
            m[k] = np.asarray(inputs[k], dtype=np.float32)
        m.update(cnp)
        in_maps.append(m)
    res = run_bass_kernel_spmd(nc, in_maps, core_ids=list(range(B)))
    out = np.stack([np.asarray(res.results[b]["out"]) for b in range(B)], axis=0)
    return out.astype(np.float32)
```

```python
import numpy as np
import contextlib
import concourse.bass as bass
import concourse.mybir as mybir
from concourse.bass_utils import run_bass_kernel_spmd

F32 = mybir.dt.float32
BF16 = mybir.dt.bfloat16
I32 = mybir.dt.int32
ALU = mybir.AluOpType
AF = mybir.ActivationFunctionType
AX = mybir.AxisListType

ENGS = ("pe", "act", "dve", "pool", "sp")
NDSEM = {"sp": 24, "pool": 24, "act": 8}


class Op:
    __slots__ = ("eng", "fn", "deps", "dma", "sig", "sigval", "dslot", "dval", "out")

    def __init__(self, eng, fn, dma):
        self.eng = eng
        self.fn = fn
        self.dma = dma
        self.deps = []
        self.sig = False
        self.sigval = 0
        self.dslot = 0
        self.dval = 0
        self.out = False


class Buf:
    __slots__ = ("name", "w", "rs", "rd")

    def __init__(self, name=""):
        self.name = name
        self.w = None
        self.rs = {}
        self.rd = []


class Sched:
    def __init__(self):
        self.ops = {e: [] for e in ENGS}
        self.ndma = {e: 0 for e in ENGS}

    def add(self, eng, fn, r=(), w=(), dma=False, out=False):
        op = Op(eng, fn, dma)
        op.out = out
        deps = {}
        def adddep(d):
            if d is None or d is op:
                return
            if d.eng == "pe" and eng == "pe" and not d.dma and not dma:
                return
            deps[id(d)] = d
        for b in r:
            adddep(b.w)
        for b in w:
            adddep(b.w)
            for d in b.rs.values():
                adddep(d)
            for d in b.rd:
                adddep(d)
        op.deps = list(deps.values())
        for b in r:
            if dma:
                b.rd.append(op)
            else:
                b.rs[eng] = op
        for b in w:
            b.w = op
            b.rs = {}
            b.rd = []
        if dma:
            K = NDSEM[eng]
            i = self.ndma[eng]
            self.ndma[eng] = i + 1
            op.dslot = i % K
            op.dval = 16 * (i // K + 1)
        self.ops[eng].append(op)
        return op

    def barrier(self):
        lasts = []
        for e in ENGS:
            comp = [o for o in self.ops[e] if not o.dma and o.fn is not None]
            if comp:
                lasts.append(comp[-1])
            lasts.extend(o for o in self.ops[e][-64:] if o.dma)
        for e in ENGS:
            op = Op(e, None, False)
            op.deps = [d for d in lasts]
            self.ops[e].append(op)

    def emit(self, nc, block, csem, dsem):
        for e in ENGS:
            for op in self.ops[e]:
                for d in op.deps:
                    d.sig = True
        for e in ENGS:
            cnt = 0
            for op in self.ops[e]:
                if not op.dma and op.sig and op.fn is not None:
                    cnt += 1
                    op.sigval = cnt
        sched = self

        def run(e, engine):
            waited = {}
            def wait(key, sem, val):
                if waited.get(key, 0) >= val:
                    return
                engine.wait_ge(sem, val)
                waited[key] = val
            outs = []
            for op in sched.ops[e]:
                for d in op.deps:
                    if d.dma:
                        wait(("d", d.eng, d.dslot), dsem[d.eng][d.dslot], d.dval)
                    else:
                        wait(("c", d.eng), csem[d.eng], d.sigval)
                if op.fn is None:
                    continue
                if op.dma:
                    if op.dval > 16:
                        wait(("d", e, op.dslot), dsem[e][op.dslot], op.dval - 16)
                    ins = op.fn(engine)
                    ins.then_inc(dsem[e][op.dslot], 16)
                    if op.out:
                        outs.append(op)
                else:
                    ins = op.fn(engine)
                    if op.sig:
                        ins.then_inc(csem[e], 1)
            for op in outs:
                wait(("d", e, op.dslot), dsem[e][op.dslot], op.dval)

        @block.tensor
        def _(eng):
            run("pe", eng)

        @block.scalar
        def _(eng):
            run("act", eng)

        @block.vector
        def _(eng):
            run("dve", eng)

        @block.gpsimd
        def _(eng):
            run("pool", eng)

        @block.sync
        def _(eng):
            run("sp", eng)


class Arena:
    def __init__(self, handle_f32, nbytes):
        self.h = handle_f32
        self.nbytes = nbytes
        self.off = 0
        self.marks = []

    def alloc(self, nelem, dtype):
        esz = 2 if dtype == BF16 else 4
        nb = (nelem * esz + 63) // 64 * 64
        assert self.off + nb <= self.nbytes, ("SBUF arena overflow", self.off, nb, self.nbytes)
        a = self.h[:, self.off // 4:(self.off + nb) // 4]
        self.off += nb
        if dtype == BF16:
            a = a.bitcast(BF16)
        elif dtype == I32:
            a = a.bitcast(I32)
        return a[:, 0:nelem]

    def mark(self):
        self.marks.append(self.off)

    def release(self):
        self.off = self.marks.pop()


class Ctx:
    pass


def build_program(body, arena_bytes=180 * 1024):
    nc = bass.Bass("TRN2", target_bir_lowering=False)
    cx = Ctx()
    cx.nc = nc
    cx.s = Sched()
    with contextlib.ExitStack() as es:
        arena_h = es.enter_context(nc.sbuf_tensor("arena", [128, arena_bytes // 4], F32))
        cx.arena = Arena(arena_h, arena_bytes)
        cx.psum = []
        cx.psb = []
        for i in range(8):
            p = es.enter_context(nc.psum_tensor(f"ps{i}", [128, 512], F32))
            cx.psum.append(p)
            cx.psb.append(Buf(f"ps{i}"))
        csem = {e: es.enter_context(nc.semaphore(f"c_{e}")) for e in ENGS}
        dsem = {e: [es.enter_context(nc.semaphore(f"d_{e}{i}")) for i in range(n)] for e, n in NDSEM.items()}
        body(cx)
        block = es.enter_context(nc.Block())
        cx.s.emit(nc, block, csem, dsem)
    return nc


import math
import numpy as np
import ml_dtypes


class Cfg:
    def __init__(self, **kw):
        self.D = 4096; self.S = 4096; self.DEPTH = 2
        self.AH = 16; self.AD = 128; self.MBLK = 256; self.TOPK = 3
        self.BH = 32; self.BKV = 4; self.BD = 64; self.WIN = 128
        self.CH = 16; self.CQ = 1024; self.CKV = 512; self.CN = 128; self.CR = 64; self.CV = 128
        self.NG = 8; self.EPG = 4; self.FF = 768
        self.SB = 1024
        self.MG = 1024
        self.CAP = 128
        self.EPS = 1e-6; self.THETA = 10000.0
        for k, v in kw.items():
            setattr(self, k, v)
        c = self
        c.AW = c.AH * c.AD; c.BW = c.BH * c.BD; c.BKW = c.BKV * c.BD; c.CW = c.CH * c.CV
        sizes = (c.AW, c.AW, c.AW, c.BW, c.BKW, c.BKW, c.CQ, c.CKV, c.CR, c.D, c.D, c.D)
        c.OFF = [0] + list(np.cumsum(sizes))
        c.NIN = int(c.OFF[-1])
        c.NE = c.NG * c.EPG
        c.NT = c.S // 128
        c.NBLK = c.S // c.MBLK


def dma(cx, q, out_ap, in_ap, r=(), w=(), out=False, slow=False):
    if slow:
        return cx.s.add(q, lambda e: e.dma_start(out=out_ap, in_=in_ap, allow_slow_non_contiguous=True),
                        r=r, w=w, dma=True, out=out)
    return cx.s.add(q, lambda e: e.dma_start(out=out_ap, in_=in_ap), r=r, w=w, dma=True, out=out)


def I(cx, eng, meth, *args, r=(), w=(), **kw):
    return cx.s.add(eng, lambda e: getattr(e, meth)(*args, **kw), r=r, w=w)


class Tile:
    def __init__(self, cx, shape, dtype, name=""):
        n = int(np.prod(shape[1:]))
        self.ap = cx.arena.alloc(n, dtype)
        self.p = shape[0]
        if len(shape) == 3:
            self.v = self.ap[0:shape[0], :].rearrange("p (a b) -> p a b", b=shape[2])
        else:
            self.v = self.ap[0:shape[0], :]
        self.b = Buf(name)


def psum_f32(cx, i):
    return cx.psum[i]


def psum_bf16(cx, i):
    return cx.psum[i][:, :].bitcast(BF16)


class Consts:
    pass


def const_inputs(c):
    bf = ml_dtypes.bfloat16
    d = {}
    d["c_ident"] = np.eye(128, dtype=np.float32).astype(bf)
    def rot(dim, reps):
        P = np.zeros((128, 128), np.float32)
        h = dim // 2
        for r in range(reps):
            o = r * dim
            for i in range(dim):
                if i < h:
                    P[o + i + h, o + i] = -1.0
                else:
                    P[o + i - h, o + i] = 1.0
        return P.astype(bf)
    d["c_rot128"] = rot(128, 1)
    d["c_rot64"] = rot(64, 2)
    NEG = -30000.0
    k = np.arange(128)[:, None]
    q = np.arange(512)[None, :]
    m = np.zeros((128, 4, 512), np.float32)
    for j in range(4):
        m[:, j, :] = np.where(j * 128 + k <= q, 0.0, NEG)
    d["c_cmask"] = m.reshape(128, 2048).astype(bf)
    qq = np.arange(128)[None, :]
    md = np.where(k <= qq, 0.0, NEG)
    mp = np.where(k > qq, 0.0, NEG)
    d["c_smask"] = np.concatenate([np.tile(md, (1, 4)), np.tile(mp, (1, 4))], axis=1).astype(bf)
    ng = np.full((128, 128), NEG, np.float32)
    d["c_smask2"] = np.concatenate([mp, md, mp, md, ng, md, ng, md], axis=1).astype(bf)
    E = np.zeros((16, c.S), np.float32)
    for n in range(c.NBLK):
        E[n, n * c.MBLK:(n + 1) * c.MBLK] = 1.0
    d["c_eblk"] = E.astype(bf)
    past = np.zeros((128, c.NT, 16), np.float32)
    gm = np.zeros((128, c.NT, 16), np.float32)
    for t in range(c.NT):
        own = (t * 128) // c.MBLK
        past[:, t, :own] = 1.0
        gm[:, t, own:] = -1e30
    d["c_past"] = past.reshape(128, c.NT * 16)
    d["c_gmask"] = gm.reshape(128, c.NT * 16)
    def fr(dim):
        p = np.arange(128)
        i = (p % dim) % (dim // 2)
        return (-(2.0 * i) / dim).astype(np.float32).reshape(128, 1)
    d["c_fexp"] = np.concatenate([fr(128), fr(64)], axis=1)
    d["c_iota"] = np.tile(np.arange(128, dtype=np.float32)[None, :], (128, 1))
    tri = (np.arange(128)[:, None] < np.arange(128)[None, :]).astype(np.float32)
    d["c_tri"] = tri.astype(bf)
    d["c_ones"] = np.ones((128, 128), np.float32).astype(bf)
    d["c_identf"] = np.eye(128, dtype=np.float32)
    return d


CONST_DT = {"c_ident": BF16, "c_rot128": BF16, "c_rot64": BF16, "c_cmask": BF16, "c_smask": BF16,
            "c_eblk": BF16, "c_smask2": BF16, "c_past": F32, "c_gmask": F32, "c_fexp": F32, "c_iota": F32,
            "c_tri": BF16, "c_ones": BF16, "c_identf": F32}


def load_consts(cx, c, cin):
    K = Consts()
    def ld(name, shape, dt):
        t = Tile(cx, shape, dt, name)
        dma(cx, "sp", t.v, cin[name], w=[t.b])
        return t
    K.ident = ld("c_ident", [128, 128], BF16)
    K.rot128 = ld("c_rot128", [128, 128], BF16)
    K.rot64 = ld("c_rot64", [128, 128], BF16)
    K.cmask = ld("c_cmask", [128, 2048], BF16)
    K.smask = ld("c_smask", [128, 1024], BF16)
    K.ones = ld("c_ones", [128, 128], BF16)
    K.fexp = ld("c_fexp", [128, 2], F32)
    return K


def build_rope(cx, c, K, pos_dram, which, cosT, sinT):
    S = c.S
    cx.arena.mark()
    pi = Tile(cx, [128, S], I32, "pos_i")
    pf = Tile(cx, [128, S], F32, "pos_f")
    invf = Tile(cx, [128, 1], F32, "invf")
    tmp = Tile(cx, [128, S], F32, "rtmp")
    dma(cx, "sp", pi.v, pos_dram.partition_broadcast(128), w=[pi.b])
    I(cx, "dve", "tensor_copy", pf.v, pi.v, r=[pi.b], w=[pf.b])
    col = K.fexp.v[:, which:which + 1]
    I(cx, "act", "activation", out=invf.v, in_=col, func=AF.Exp, scale=math.log(c.THETA),
      r=[K.fexp.b], w=[invf.b])
    TWO_PI = 2.0 * math.pi
    ki = Tile(cx, [128, S], I32, "rk_i")
    for (dst, shift) in ((sinT, 0.0), (cosT, 0.5 * math.pi)):
        I(cx, "dve", "tensor_scalar", tmp.v, pf.v, invf.v[:, 0:1], shift, ALU.mult, ALU.add,
          r=[pf.b, invf.b], w=[tmp.b])
        I(cx, "dve", "tensor_scalar", pi.v.bitcast(F32), tmp.v, 1.0 / TWO_PI, 0.0, ALU.mult, ALU.add,
          r=[tmp.b], w=[pi.b])
        I(cx, "dve", "tensor_copy", ki.v, pi.v.bitcast(F32), r=[pi.b], w=[ki.b])
        I(cx, "dve", "tensor_copy", pi.v.bitcast(F32), ki.v, r=[ki.b], w=[pi.b])
        I(cx, "dve", "scalar_tensor_tensor", tmp.v, pi.v.bitcast(F32), -TWO_PI, tmp.v, ALU.mult, ALU.add,
          r=[pi.b, tmp.b], w=[tmp.b])
        I(cx, "act", "activation", out=dst.v, in_=tmp.v, func=AF.Sin, r=[tmp.b], w=[dst.b])
    cx.s.barrier()
    cx.arena.release()


def phase_rmsnorm_T(cx, c, K, h_d, g_d, hnT_d, hn_tok_d=None, hn_f32T_d=None):
    D, S = c.D, c.S
    NC = D // 128
    cx.arena.mark()
    gb = Tile(cx, [128, D], F32, "g_bc")
    dma(cx, "sp", gb.v, g_d.partition_broadcast(128), w=[gb.b])
    xt = [Tile(cx, [128, D], F32, f"x{i}") for i in range(2)]
    junk = Tile(cx, [128, D], BF16, "junk")
    xn = [Tile(cx, [128, D], BF16, f"xn{i}") for i in range(2)]
    ss = [Tile(cx, [128, 2], F32, f"ss{i}") for i in range(2)]
    TB = 4 if S >= 512 else S // 128
    hT = [Tile(cx, [128, NC, TB * 128], BF16, f"hT{i}") for i in range(2)]
    nblk = S // (128 * TB)
    pb = 0
    for blk in range(nblk):
        ht = hT[blk % 2]
        for sub in range(TB):
            t = blk * TB + sub
            x = xt[t % 2]; n = xn[t % 2]; s2 = ss[t % 2]
            dma(cx, "sp", x.v, h_d[t * 128:(t + 1) * 128, :], w=[x.b])
            I(cx, "act", "activation", out=junk.v, in_=x.v, func=AF.Square, accum_out=s2.v[:, 0:1],
              r=[x.b], w=[junk.b, s2.b])
            I(cx, "dve", "tensor_scalar", s2.v[:, 1:2], s2.v[:, 0:1], 1.0 / D, c.EPS, ALU.mult, ALU.add,
              r=[s2.b], w=[s2.b])
            I(cx, "act", "activation", out=s2.v[:, 1:2], in_=s2.v[:, 1:2], func=AF.Sqrt, r=[s2.b], w=[s2.b])
            I(cx, "dve", "reciprocal", s2.v[:, 1:2], s2.v[:, 1:2], r=[s2.b], w=[s2.b])
            I(cx, "dve", "scalar_tensor_tensor", n.v, x.v, s2.v[:, 1:2], gb.v, ALU.mult, ALU.mult,
              r=[x.b, s2.b, gb.b], w=[n.b])
            if hn_tok_d is not None:
                dma(cx, "sp", hn_tok_d[t * 128:(t + 1) * 128, :], n.v, r=[n.b])
            for g8 in range(0, NC, 8):
                bank = pb % 2
                pb += 1
                pt = psum_bf16(cx, bank)
                nn = min(8, NC - g8)
                for j in range(nn):
                    cc = g8 + j
                    I(cx, "pe", "transpose", pt[:, j * 128:(j + 1) * 128], n.v[:, cc * 128:(cc + 1) * 128],
                      K.ident.v, r=[n.b, K.ident.b], w=[cx.psb[bank]])
                eng = "act" if (pb % 2) else "dve"
                src = pt[:, 0:nn * 128].rearrange("p (a b) -> p a b", b=128)
                dstv = ht.v[:, g8:g8 + nn, sub * 128:(sub + 1) * 128]
                if eng == "act":
                    I(cx, "act", "copy", dstv, src, r=[cx.psb[bank]], w=[ht.b])
                else:
                    I(cx, "dve", "tensor_copy", dstv, src, r=[cx.psb[bank]], w=[ht.b])
        dst = hnT_d[:, blk * TB * 128:(blk + 1) * TB * 128].rearrange("(a p) s -> p a s", p=128)
        dma(cx, "sp", dst, ht.v, r=[ht.b])
    cx.s.barrier()
    cx.arena.release()


def phase_proj(cx, c, xT_d, Kdim, W_d, col_tiles, mode, epi, SB=None, PW=256, banks=(2, 3), prep=None):
    S = c.S
    SB = SB or min(c.SB, S)
    KC = Kdim // 128
    cx.arena.mark()
    xb = Tile(cx, [128, KC, SB], BF16, "xblk")
    KQ = 8 if KC >= 8 else KC
    NP = KC // KQ
    nstg = NP if NP >= 2 else 2
    stg = [Tile(cx, [128, KQ, PW], F32, f"wstg{i}") for i in range(nstg)]
    wp = [Tile(cx, [128, KC, PW], BF16, f"wp{i}") for i in range(2)]
    panels = []
    cur = []
    for ct in col_tiles:
        if cur and (ct[0] != cur[-1][0] + cur[-1][1] or (ct[0] + ct[1] - cur[0][0]) > PW):
            panels.append(cur); cur = []
        cur.append(ct)
    if cur:
        panels.append(cur)
    items = [(s0, pan) for s0 in range(0, S, SB) for pan in panels]
    TS = 512 if SB >= 512 else SB
    state = {"nstg": 0, "nbank": 0}

    def emit_load(idx):
        s0, pan = items[idx]
        w = wp[idx % 2]
        c0 = pan[0][0]
        pw = pan[-1][0] + pan[-1][1] - c0
        sts = []
        for q in range(NP):
            st = stg[state["nstg"] % nstg]
            state["nstg"] += 1
            src = W_d[q * KQ * 128:(q + 1) * KQ * 128, c0:c0 + pw].rearrange("(a p) n -> p a n", p=128)
            dma(cx, "sp", st.v[:, :, 0:pw], src, w=[st.b])
            sts.append(st)
        deferred = []
        for q in range(NP):
            st = sts[q]
            args = (w.v[:, q * KQ:(q + 1) * KQ, 0:pw], st.v[:, :, 0:pw])
            if NP >= 4 and q >= NP - 2:
                deferred.append((q, args, st, w))
            else:
                I(cx, "pool", "tensor_copy", *args, r=[st.b], w=[w.b])
        return deferred

    deferred = emit_load(0)
    for (q, args, st, w) in deferred:
        I(cx, "act", "copy", *args, r=[st.b], w=[w.b])
    cur_s0 = None
    for idx, (s0, pan) in enumerate(items):
        if s0 != cur_s0:
            cur_s0 = s0
            dma(cx, "sp", xb.v, xT_d[:, s0:s0 + SB].rearrange("(a p) s -> p a s", p=128), w=[xb.b])
            if prep is not None:
                prep(cx, xb, SB)
        deferred = emit_load(idx + 1) if idx + 1 < len(items) else []
        w = wp[idx % 2]
        c0 = pan[0][0]
        work = []
        for (cs, cw, tag) in pan:
            lo = cs - c0
            step = TS if mode == "T" else 128
            for ts in range(0, SB, step):
                work.append((lo, cw, tag, ts))
        half = len(work) // 2
        for wi, (lo, cw, tag, ts) in enumerate(work):
            if wi == half:
                for (q, args, st, w2) in deferred:
                    I(cx, "act", "copy", *args, r=[st.b], w=[w2.b])
                deferred = []
            bank = banks[state["nbank"] % len(banks)]
            state["nbank"] += 1
            if mode == "T":
                ps = cx.psum[bank][0:cw, 0:TS]
                for kc in range(KC):
                    I(cx, "pe", "matmul", ps, w.v[:, kc, lo:lo + cw], xb.v[:, kc, ts:ts + TS],
                      start=(kc == 0), stop=(kc == KC - 1), r=[w.b, xb.b], w=[cx.psb[bank]])
                epi(cx, tag, s0 + ts, TS, ps, cx.psb[bank])
            else:
                ps = cx.psum[bank][:, 0:cw]
                for kc in range(KC):
                    I(cx, "pe", "matmul", ps, xb.v[:, kc, ts:ts + 128], w.v[:, kc, lo:lo + cw],
                      start=(kc == 0), stop=(kc == KC - 1), r=[w.b, xb.b], w=[cx.psb[bank]])
                epi(cx, tag, s0 + ts, 128, ps, cx.psb[bank])
        for (q, args, st, w2) in deferred:
            I(cx, "act", "copy", *args, r=[st.b], w=[w2.b])
    cx.s.barrier()
    cx.arena.release()


def dram(cx, name, shape, dtype):
    kind = "ExternalOutput" if name in cx.debug else "Internal"
    return cx.nc.dram_tensor(name, list(shape), dtype, kind=kind).ap()


class Rot:
    def __init__(self, cx, n, shape, dtype, name):
        self.t = [Tile(cx, shape, dtype, f"{name}{i}") for i in range(n)]
        self.i = 0

    def next(self):
        t = self.t[self.i % len(self.t)]
        self.i += 1
        return t


def make_epilogues(cx, c, K, R):
    E = Consts()
    E.xs = Rot(cx, 2, [128, 512], BF16, "e_xs")
    E.t1 = Rot(cx, 2, [128, 512], F32, "e_t1")
    E.t2 = Rot(cx, 2, [128, 512], F32, "e_t2")
    E.cs = Rot(cx, 2, [128, 1024], F32, "e_cs")
    E.ob = Rot(cx, 3, [128, 512], BF16, "e_ob")
    E.rotbank = 4
    E.trbank = 5
    E.n = 0
    return E


def epi_copy(cx, E, ps, psb, cw, ns, dst_ap, func=None, scale_ap=None, scale_buf=None, mul_ap=None, mul_buf=None):
    o = E.ob.next()
    E.n += 1
    ov = o.v[0:cw, 0:ns]
    if func is not None:
        I(cx, "act", "activation", out=ov, in_=ps, func=func, r=[psb], w=[o.b])
    elif scale_ap is not None:
        I(cx, "dve", "tensor_scalar", ov, ps, scale_ap, None, ALU.mult, r=[psb, scale_buf], w=[o.b])
    elif mul_ap is not None:
        I(cx, "dve", "tensor_tensor", ov, ps, mul_ap, ALU.mult, r=[psb, mul_buf], w=[o.b])
    elif E.n % 2:
        I(cx, "act", "copy", ov, ps, r=[psb], w=[o.b])
    else:
        I(cx, "dve", "tensor_copy", ov, ps, r=[psb], w=[o.b])
    dma(cx, "pool", dst_ap, ov, r=[o.b])


def epi_rope(cx, c, K, E, ps, psb, cw, ns, s0, dst_ap, rot, cos_d, sin_d, mul_ap=None, mul_buf=None):
    xs = E.xs.next(); t1 = E.t1.next(); t2 = E.t2.next(); cs = E.cs.next(); o = E.ob.next()
    dma(cx, "sp", cs.v[0:cw, 0:ns], cos_d[0:cw, s0:s0 + ns], w=[cs.b])
    dma(cx, "sp", cs.v[0:cw, 512:512 + ns], sin_d[0:cw, s0:s0 + ns], w=[cs.b])
    xv = xs.v[0:cw, 0:ns]
    if mul_ap is not None:
        I(cx, "dve", "tensor_tensor", xv, ps, mul_ap, ALU.mult, r=[psb, mul_buf], w=[xs.b])
    else:
        I(cx, "act", "copy", xv, ps, r=[psb], w=[xs.b])
    rb = E.rotbank
    rp = cx.psum[rb][0:cw, 0:ns]
    I(cx, "pe", "matmul", rp, rot.v[0:cw, 0:cw], xv, start=True, stop=True, r=[rot.b, xs.b], w=[cx.psb[rb]])
    I(cx, "dve", "tensor_tensor", t1.v[0:cw, 0:ns], xv, cs.v[0:cw, 0:ns], ALU.mult, r=[xs.b, cs.b], w=[t1.b])
    I(cx, "dve", "tensor_tensor", t2.v[0:cw, 0:ns], rp, cs.v[0:cw, 512:512 + ns], ALU.mult,
      r=[cx.psb[rb], cs.b], w=[t2.b])
    I(cx, "pool", "tensor_tensor", o.v[0:cw, 0:ns], t1.v[0:cw, 0:ns], t2.v[0:cw, 0:ns], ALU.add,
      r=[t1.b, t2.b], w=[o.b])
    dma(cx, "pool", dst_ap, o.v[0:cw, 0:ns], r=[o.b])


def epi_T2N(cx, c, K, E, ps, psb, cw, ns, s0, dst_rows_fn, mul_ap=None, mul_buf=None):
    xs = E.xs.next(); o = E.ob.next()
    xv = xs.v[0:cw, 0:ns]
    if mul_ap is not None:
        I(cx, "dve", "tensor_tensor", xv, ps, mul_ap, ALU.mult, r=[psb, mul_buf], w=[xs.b])
    else:
        I(cx, "act", "copy", xv, ps, r=[psb], w=[xs.b])
    tb = E.trbank
    pt = psum_bf16(cx, tb)
    nt = ns // 128
    for i in range(nt):
        I(cx, "pe", "transpose", pt[:, i * 128:i * 128 + cw], xs.v[0:cw, i * 128:(i + 1) * 128],
          K.ident.v[0:cw, 0:cw], r=[xs.b, K.ident.b], w=[cx.psb[tb]])
    ov = o.v[:, 0:nt * cw].rearrange("p (a b) -> p a b", b=cw)
    src = pt[:, 0:nt * 128].rearrange("p (a b) -> p a b", b=128)[:, :, 0:cw]
    I(cx, "act", "copy", ov, src, r=[cx.psb[tb]], w=[o.b])
    dma(cx, "pool", dst_rows_fn(s0, ns), ov, r=[o.b])


def phase_A(cx, c, K, E, R, T, hnT_d, w_in_d):
    O = c.OFF
    tilesT = []
    for h in range(c.AH):
        tilesT.append((O[0] + 128 * h, 128, ("ropeA", T["qaT"], h)))
    for h in range(c.AH):
        tilesT.append((O[1] + 128 * h, 128, ("ropeA", T["kaT"], h)))
    for j in range(c.BW // 128):
        tilesT.append((O[3] + 128 * j, 128, ("ropeB", T["qbT"], j)))
    for j in range(c.BKW // 128):
        tilesT.append((O[4] + 128 * j, 128, ("ropeB", T["kbT"], j)))
    for j in range(c.CQ // 128):
        tilesT.append((O[6] + 128 * j, 128, ("copy", T["cqT"], j)))
    for j in range(c.CKV // 128):
        tilesT.append((O[7] + 128 * j, 128, ("copy", T["ckvT"], j)))
    tilesT.append((O[8], c.CR, ("ropeK", T["kpeT"], 0)))
    for i in range(3):
        for j in range(c.D // 128):
            tilesT.append((O[9 + i] + 128 * j, 128, ("sig", T["gT"], i * (c.D // 128) + j)))

    def epiT(cx, tag, s0, ns, ps, psb):
        kind, dst, j = tag
        if kind == "ropeA":
            epi_rope(cx, c, K, E, ps, psb, 128, ns, s0, dst[j * 128:(j + 1) * 128, s0:s0 + ns], K.rot128,
                     R["cosA"], R["sinA"])
        elif kind == "ropeB":
            epi_rope(cx, c, K, E, ps, psb, 128, ns, s0, dst[j * 128:(j + 1) * 128, s0:s0 + ns], K.rot64,
                     R["cosB"], R["sinB"])
        elif kind == "ropeK":
            epi_rope(cx, c, K, E, ps, psb, 64, ns, s0, dst[0:64, s0:s0 + ns], K.rot64, R["cosB"], R["sinB"])
        elif kind == "copy":
            epi_copy(cx, E, ps, psb, 128, ns, dst[j * 128:(j + 1) * 128, s0:s0 + ns])
        elif kind == "sig":
            epi_copy(cx, E, ps, psb, 128, ns, dst[j * 128:(j + 1) * 128, s0:s0 + ns], func=AF.Sigmoid)
    phase_proj(cx, c, hnT_d, c.D, w_in_d, tilesT, "T", epiT)

    tilesN = []
    for j in range(c.AW // 256):
        tilesN.append((O[2] + 256 * j, 256, (T["va"], 256 * j)))
    for j in range(max(1, c.BKW // 256)):
        wdt = min(256, c.BKW)
        tilesN.append((O[5] + wdt * j, wdt, (T["vb"], wdt * j)))

    def epiN(cx, tag, s0, ns, ps, psb):
        dst, c0 = tag
        cw = ps.shape[1]
        epi_copy(cx, E, ps, psb, 128, cw, dst[s0:s0 + ns, c0:c0 + cw])
    phase_proj(cx, c, hnT_d, c.D, w_in_d, tilesN, "N", epiN)


def phase_mla_up(cx, c, K, E, R, T, qg_d, kvg_d, wq_d, wkv_d):
    for (xT_d, KD, g_d, W_d, which) in ((T["cqT"], c.CQ, qg_d, wq_d, "q"), (T["ckvT"], c.CKV, kvg_d, wkv_d, "kv")):
        KC = KD // 128
        SB = min(c.SB, c.S)
        cx.arena.mark()
        gcol = Tile(cx, [128, KC], F32, "gcol")
        dma(cx, "sp", gcol.v, g_d.rearrange("(a p) -> p a", p=128), w=[gcol.b], slow=True)
        rstd = Tile(cx, [128, SB], F32, "rstd_bc")
        sq = Rot(cx, 2, [128, 512], BF16, "sq")

        def prep(cx, xb, SBn, KC=KC, KD=KD, gcol=gcol, rstd=rstd, sq=sq):
            bank = 6
            for ts in range(0, SBn, 512):
                n = min(512, SBn - ts)
                ps = cx.psum[bank][:, 0:n]
                for kc in range(KC):
                    s = sq.next()
                    I(cx, "act", "activation", out=s.v[:, 0:n], in_=xb.v[:, kc, ts:ts + n], func=AF.Square,
                      r=[xb.b], w=[s.b])
                    I(cx, "pe", "matmul", ps, K.ones.v, s.v[:, 0:n], start=(kc == 0), stop=(kc == KC - 1),
                      r=[K.ones.b, s.b], w=[cx.psb[bank]])
                rv = rstd.v[:, ts:ts + n]
                I(cx, "dve", "tensor_scalar", rv, ps, 1.0 / KD, c.EPS, ALU.mult, ALU.add,
                  r=[cx.psb[bank]], w=[rstd.b])
                I(cx, "act", "activation", out=rv, in_=rv, func=AF.Sqrt, r=[rstd.b], w=[rstd.b])
                I(cx, "dve", "reciprocal", rv, rv, r=[rstd.b], w=[rstd.b])
            for kc in range(KC):
                I(cx, "dve", "tensor_scalar", xb.v[:, kc, :], xb.v[:, kc, :], gcol.v[:, kc:kc + 1], None, ALU.mult,
                  r=[xb.b, gcol.b], w=[xb.b])

        if which == "q":
            tiles = []
            for h in range(c.CH):
                tiles.append((192 * h, 128, ("qn", h)))
                tiles.append((192 * h + 128, 64, ("qpe", h)))

            def epi(cx, tag, s0, ns, ps, psb, rstd=rstd, SB=SB):
                kind, h = tag
                lo = s0 % SB
                if kind == "qn":
                    epi_copy(cx, E, ps, psb, 128, ns, T["qnT"][h * 128:(h + 1) * 128, s0:s0 + ns],
                             mul_ap=rstd.v[:, lo:lo + ns], mul_buf=rstd.b)
                else:
                    epi_rope(cx, c, K, E, ps, psb, 64, ns, s0, T["qpeT"][h * 64:(h + 1) * 64, s0:s0 + ns], K.rot64,
                             R["cosB"], R["sinB"], mul_ap=rstd.v[0:64, lo:lo + ns], mul_buf=rstd.b)
        else:
            tiles = []
            for h in range(c.CH):
                tiles.append((256 * h, 128, ("kn", h)))
                tiles.append((256 * h + 128, 128, ("v", h)))

            def epi(cx, tag, s0, ns, ps, psb, rstd=rstd, SB=SB):
                kind, h = tag
                lo = s0 % SB
                if kind == "kn":
                    epi_copy(cx, E, ps, psb, 128, ns, T["knT"][h * 128:(h + 1) * 128, s0:s0 + ns],
                             mul_ap=rstd.v[:, lo:lo + ns], mul_buf=rstd.b)
                else:
                    def rows(s0, ns, h=h):
                        return T["vc"][s0:s0 + ns, h * 128:(h + 1) * 128].rearrange("(a p) d -> p a d", p=128)
                    epi_T2N(cx, c, K, E, ps, psb, 128, ns, s0, rows, mul_ap=rstd.v[:, lo:lo + ns], mul_buf=rstd.b)
        phase_proj(cx, c, xT_d, KD, W_d, tiles, "T", epi, prep=prep)
        cx.arena.release()


def phase_attn_causal(cx, c, K, H, terms, v_d, dv, scale, oT_d, moba=False, cin=None, mla_shared_k=False):
    S, NT = c.S, c.NT
    NG = S // 512
    cx.arena.mark()
    nterm = len(terms)
    QT = [[Tile(cx, [kd, S], BF16, f"QT{p}") for (_, _, kd) in terms] for p in range(2)]
    KT = [[Tile(cx, [kd, S], BF16, f"KT{p}") for (_, _, kd) in terms] for p in range(2)]
    V = [Tile(cx, [128, NT, dv + 1], BF16, f"V{p}") for p in range(2)]
    for p in range(2):
        I(cx, "pool", "memset", V[p].v[:, :, dv:dv + 1], 1.0, w=[V[p].b])
    PT = Rot(cx, 3, [128, 512], BF16, "PT")
    osb = Rot(cx, 2, [128, 4, dv], BF16, "osb")
    oT = Rot(cx, 2, [128, 512], BF16, "oT")
    rc = Rot(cx, 4, [128, 1], F32, "rc")
    if moba:
        eblk = Tile(cx, [16, S], BF16, "eblk")
        dma(cx, "sp", eblk.v, cin["c_eblk"], w=[eblk.b])
        past = Tile(cx, [128, NT, 16], F32, "past")
        dma(cx, "sp", past.v, cin["c_past"].rearrange("p (a b) -> p a b", b=16), w=[past.b])
        gmask = Tile(cx, [128, NT, 16], F32, "gmask")
        dma(cx, "sp", gmask.v, cin["c_gmask"].rearrange("p (a b) -> p a b", b=16), w=[gmask.b])
        BT = [Tile(cx, [16, S], BF16, f"BT{p}") for p in range(2)]
        kmf = Tile(cx, [128, 16], F32, "kmf")
        kmb = Tile(cx, [128, 16], BF16, "kmb")
        I(cx, "pool", "memset", kmf.v, 0.0, w=[kmf.b])
        g1 = Tile(cx, [128, NT, 16], F32, "g1")
        g2 = Tile(cx, [128, NT, 16], F32, "g2")
        eq = Tile(cx, [128, NT, 16], F32, "eq")
        mx = Tile(cx, [128, NT], F32, "mx")
        bb = Tile(cx, [128, NT, 16], BF16, "bb")
    sstep = 0

    def load_head(h):
        p = h % 2
        for i, (q_d, k_d, kd) in enumerate(terms):
            dma(cx, "sp", QT[p][i].v, q_d[h * kd:(h + 1) * kd, :], w=[QT[p][i].b])
            if mla_shared_k and i == 1:
                if h < 2:
                    dma(cx, "sp", KT[p][i].v, k_d[0:kd, :], w=[KT[p][i].b])
            else:
                dma(cx, "sp", KT[p][i].v, k_d[h * kd:(h + 1) * kd, :], w=[KT[p][i].b])
        dma(cx, "sp", V[p].v[:, :, 0:dv], v_d[:, h * dv:(h + 1) * dv].rearrange("(t p) d -> p t d", p=128),
            w=[V[p].b])

    def bias_head(h):
        p = h % 2
        if moba:
            kt0 = KT[p][0]; qt0 = QT[p][0]; bt = BT[p]
            NB = c.NBLK
            I(cx, "dve", "tensor_reduce", kmf.v[:, 0:NB], kt0.v.rearrange("p (n k) -> p n k", k=c.MBLK), AX.X, ALU.add,
              r=[kt0.b], w=[kmf.b])
            I(cx, "dve", "tensor_copy", kmb.v, kmf.v, r=[kmf.b], w=[kmb.b])
            gb_ = 7
            gp = cx.psum[gb_][:, 0:NT * 16]
            for t in range(NT):
                I(cx, "pe", "matmul", gp[:, t * 16:(t + 1) * 16], qt0.v[:, t * 128:(t + 1) * 128], kmb.v,
                  start=True, stop=True, r=[qt0.b, kmb.b], w=[cx.psb[gb_]])
            gp3 = gp.rearrange("p (a b) -> p a b", b=16)
            I(cx, "dve", "tensor_tensor", g1.v, gp3, gmask.v, ALU.add, r=[cx.psb[gb_], gmask.b], w=[g1.b])
            cur = g1
            for it in range(3):
                I(cx, "dve", "tensor_reduce", mx.v, cur.v, AX.X, ALU.max, r=[cur.b], w=[mx.b])
                if it < 2:
                    mb = mx.v.unsqueeze(2).broadcast_to([128, NT, 16])
                    I(cx, "dve", "tensor_tensor", eq.v, cur.v, mb, ALU.is_equal, r=[cur.b, mx.b], w=[eq.b])
                    I(cx, "dve", "scalar_tensor_tensor", g2.v, eq.v, -1e30, cur.v, ALU.mult, ALU.add,
                      r=[eq.b, cur.b], w=[g2.b])
                    cur = g2
            mb = mx.v.unsqueeze(2).broadcast_to([128, NT, 16])
            I(cx, "dve", "tensor_tensor", eq.v, g1.v, mb, ALU.is_ge, r=[g1.b, mx.b], w=[eq.b])
            I(cx, "dve", "tensor_tensor", eq.v, eq.v, past.v, ALU.mult, r=[eq.b, past.b], w=[eq.b])
            I(cx, "dve", "tensor_tensor", eq.v, eq.v, past.v, ALU.subtract, r=[eq.b, past.b], w=[eq.b])
            I(cx, "dve", "tensor_scalar", bb.v, eq.v, 30000.0, None, ALU.mult, r=[eq.b], w=[bb.b])
            tb = 7
            pt = psum_bf16(cx, tb)
            for t0 in range(0, NT, 8):
                n8 = min(8, NT - t0)
                for j in range(n8):
                    I(cx, "pe", "transpose", pt[0:16, j * 128:(j + 1) * 128], bb.v[:, t0 + j, :], K.ident.v,
                      r=[bb.b, K.ident.b], w=[cx.psb[tb]])
                I(cx, "act", "copy", bt.v[:, t0 * 128:(t0 + n8) * 128], pt[0:16, 0:n8 * 128], r=[cx.psb[tb]], w=[bt.b])

    load_head(0)
    if moba:
        bias_head(0)
    for h in range(H):
        p = h % 2
        if h + 1 < H:
            load_head(h + 1)
        for G in range(NG):
            if G == NG // 2 and h + 1 < H and moba:
                bias_head(h + 1)
            accb = (2, 3, 4, 5)
            def acc(i):
                return cx.psum[accb[i]][:, 0:dv + 1]
            nk = 4 * G + 4
            for kt in range(nk):
                j = kt - 4 * G
                jq = max(j, 0)
                q0 = jq * 128
                N = 512 - q0
                sb_ = sstep % 2
                sstep += 1
                ps = cx.psum[sb_][:, 0:N]
                ops = []
                for i in range(nterm):
                    ops.append((KT[p][i].v[:, kt * 128:(kt + 1) * 128], QT[p][i].v[:, G * 512 + q0:(G + 1) * 512],
                                [KT[p][i].b, QT[p][i].b]))
                if moba:
                    ops.append((eblk.v[:, kt * 128:(kt + 1) * 128], BT[p].v[:, G * 512 + q0:(G + 1) * 512],
                                [eblk.b, BT[p].b]))
                if j >= 0:
                    ops.append((K.ident.v, K.cmask.v[:, j * 512 + q0:(j + 1) * 512], [K.ident.b, K.cmask.b]))
                for oi, (l, r_, bufs) in enumerate(ops):
                    I(cx, "pe", "matmul", ps, l, r_, start=(oi == 0), stop=(oi == len(ops) - 1),
                      r=bufs, w=[cx.psb[sb_]])
                ptile = PT.next()
                I(cx, "act", "activation", out=ptile.v[:, 0:N], in_=ps, func=AF.Exp, scale=scale,
                  r=[cx.psb[sb_]], w=[ptile.b])
                for i in range(jq, 4):
                    I(cx, "pe", "matmul", acc(i), ptile.v[:, i * 128 - q0:i * 128 - q0 + 128], V[p].v[:, kt, :],
                      start=(kt == 0), stop=(kt == 4 * G + i), r=[ptile.b, V[p].b], w=[cx.psb[accb[i]]])
            ob = osb.next()
            for i in range(4):
                r1 = rc.next()
                a = acc(i)
                I(cx, "dve", "reciprocal", r1.v, a[:, dv:dv + 1], r=[cx.psb[accb[i]]], w=[r1.b])
                I(cx, "dve", "tensor_scalar", ob.v[:, i, :], a[:, 0:dv], r1.v[:, 0:1], None, ALU.mult,
                  r=[cx.psb[accb[i]], r1.b], w=[ob.b])
            tb = 6
            pt = psum_bf16(cx, tb)
            for i in range(4):
                I(cx, "pe", "transpose", pt[0:dv, i * 128:(i + 1) * 128], ob.v[:, i, :], K.ident.v,
                  r=[ob.b, K.ident.b], w=[cx.psb[tb]])
            ot = oT.next()
            I(cx, "act", "copy", ot.v[0:dv, :], pt[0:dv, 0:512], r=[cx.psb[tb]], w=[ot.b])
            dma(cx, "pool", oT_d[h * dv:(h + 1) * dv, G * 512:(G + 1) * 512], ot.v[0:dv, :], r=[ot.b])
    cx.s.barrier()
    cx.arena.release()


def phase_swa(cx, c, K, qT_d, kT_d, v_d, sinks_d, oT_d, cin):
    S, NT = c.S, c.NT
    d = c.BD
    r = c.BH // c.BKV
    scale = d ** -0.5
    cx.arena.mark()
    es = Tile(cx, [128, c.BH], F32, "esink")
    dma(cx, "sp", es.v, sinks_d.partition_broadcast(128), w=[es.b])
    I(cx, "act", "activation", out=es.v, in_=es.v, func=AF.Exp, r=[es.b], w=[es.b])
    sm = Tile(cx, [128, 1024], BF16, "smask2")
    dma(cx, "sp", sm.v, cin["c_smask2"], w=[sm.b])
    KTt = [Tile(cx, [64, S], BF16, f"sK{p}") for p in range(2)]
    Vt = [Tile(cx, [128, NT, d + 1], BF16, f"sV{p}") for p in range(2)]
    for p in range(2):
        I(cx, "pool", "memset", Vt[p].v[:, :, d:d + 1], 1.0, w=[Vt[p].b])
    Q2 = [Tile(cx, [64, 2, S], BF16, f"sQ{p}") for p in range(2)]
    PT = Rot(cx, 3, [128, 512], BF16, "sPT")
    ob2 = Rot(cx, 2, [128, 128], BF16, "sob")
    oTt = Rot(cx, 2, [128, 512], BF16, "soT")
    den = Rot(cx, 4, [128, 1], F32, "sden")
    step = 0
    npair = 0
    for g in range(c.BKV):
        kp = g % 2
        dma(cx, "sp", KTt[kp].v, kT_d[g * 64:(g + 1) * 64, :], w=[KTt[kp].b])
        dma(cx, "sp", Vt[kp].v[:, :, 0:d], v_d[:, g * 64:(g + 1) * 64].rearrange("(t p) d -> p t d", p=128),
            w=[Vt[kp].b])
        for hp in range(r // 2):
            h0 = g * r + 2 * hp
            q2 = Q2[npair % 2]
            npair += 1
            dma(cx, "sp", q2.v, qT_d[h0 * 64:(h0 + 2) * 64, :].rearrange("(h d) s -> d h s", d=64), w=[q2.b])
            ot = None
            for t in range(NT):
                sb_ = step % 2
                step += 1
                ps = cx.psum[sb_][:, 0:512]
                mv = sm.v[:, 0:512] if t > 0 else sm.v[:, 512:1024]
                I(cx, "pe", "matmul", ps, K.ident.v, mv, start=True, stop=False, r=[K.ident.b, sm.b], w=[cx.psb[sb_]])
                mms = []
                for hh in range(2):
                    for kk in range(2):
                        kt = t - 1 + kk
                        if kt < 0:
                            continue
                        mms.append((hh, kk, kt))
                for mi, (hh, kk, kt) in enumerate(mms):
                    col = (hh * 2 + kk) * 128
                    I(cx, "pe", "matmul", ps[:, col:col + 128], KTt[kp].v[:, kt * 128:(kt + 1) * 128],
                      q2.v[:, hh, t * 128:(t + 1) * 128], start=False, stop=(mi == len(mms) - 1),
                      r=[KTt[kp].b, q2.b], w=[cx.psb[sb_]])
                pt_ = PT.next()
                I(cx, "act", "activation", out=pt_.v, in_=ps, func=AF.Exp, scale=scale, r=[cx.psb[sb_]], w=[pt_.b])
                ab = 2 + (step % 2)
                o2 = ob2.next()
                for hh in range(2):
                    a = cx.psum[ab][:, hh * 128:hh * 128 + d + 1]
                    kks = [kk for kk in range(2) if t - 1 + kk >= 0]
                    for ki, kk in enumerate(kks):
                        col = (hh * 2 + kk) * 128
                        I(cx, "pe", "matmul", a, pt_.v[:, col:col + 128], Vt[kp].v[:, t - 1 + kk, :],
                          start=(ki == 0), stop=(ki == len(kks) - 1), r=[pt_.b, Vt[kp].b], w=[cx.psb[ab]])
                    dn = den.next()
                    I(cx, "dve", "tensor_tensor", dn.v, a[:, d:d + 1], es.v[:, h0 + hh:h0 + hh + 1], ALU.add,
                      r=[cx.psb[ab], es.b], w=[dn.b])
                    I(cx, "dve", "reciprocal", dn.v, dn.v, r=[dn.b], w=[dn.b])
                    I(cx, "dve", "tensor_scalar", o2.v[:, hh * 64:(hh + 1) * 64], a[:, 0:d], dn.v[:, 0:1], None, ALU.mult,
                      r=[cx.psb[ab], dn.b], w=[o2.b])
                tb = 4
                ptb = psum_bf16(cx, tb)
                if t % 4 == 0:
                    ot = oTt.next()
                I(cx, "pe", "transpose", ptb[:, 0:128], o2.v, K.ident.v, r=[o2.b, K.ident.b], w=[cx.psb[tb]])
                I(cx, "act", "copy", ot.v[:, (t % 4) * 128:(t % 4 + 1) * 128], ptb[:, 0:128], r=[cx.psb[tb]], w=[ot.b])
                if t % 4 == 3 or t == NT - 1:
                    n = (t % 4 + 1) * 128
                    t0 = t - t % 4
                    dma(cx, "pool", oT_d[h0 * 64:(h0 + 2) * 64, t0 * 128:t0 * 128 + n], ot.v[:, 0:n], r=[ot.b])
    cx.s.barrier()
    cx.arena.release()


def phase_out_gate(cx, c, K, oT_list, W_list, gT_d, yT_d):
    S, D = c.S, c.D
    SB = 512 if S >= 512 else S
    PW = 256
    cx.arena.mark()
    KCs = [o.shape[0] // 128 for o in oT_list]
    xb = [Tile(cx, [128, KCs[i], SB], BF16, f"ox{i}") for i in range(3)]
    wp = [[Tile(cx, [128, KCs[i], PW], BF16, f"ow{i}_{p}") for p in range(2)] for i in range(3)]
    stg = Rot(cx, 4, [128, 8, PW], F32, "ostg")
    gt = [Rot(cx, 2, [128, SB], BF16, f"og{i}") for i in range(3)]
    tt = [Rot(cx, 2, [128, SB], F32, f"ot{i}") for i in range(3)]
    yo = Rot(cx, 2, [128, SB], BF16, "oy")
    nb = 0
    items = [(s0, c0) for s0 in range(0, S, SB) for c0 in range(0, D, PW)]
    ncast = [0]

    def emit_load(idx):
        s0, c0 = items[idx]
        pp = idx % 2
        for i in range(3):
            w = wp[i][pp]
            KQ = min(8, KCs[i])
            for q in range(KCs[i] // KQ):
                st = stg.next()
                src = W_list[i][q * KQ * 128:(q + 1) * KQ * 128, c0:c0 + PW].rearrange("(a p) n -> p a n", p=128)
                dma(cx, "sp", st.v[:, 0:KQ, :], src, w=[st.b])
                ncast[0] += 1
                if ncast[0] % 2:
                    I(cx, "act", "copy", w.v[:, q * KQ:(q + 1) * KQ, :], st.v[:, 0:KQ, :], r=[st.b], w=[w.b])
                else:
                    I(cx, "pool", "tensor_copy", w.v[:, q * KQ:(q + 1) * KQ, :], st.v[:, 0:KQ, :], r=[st.b], w=[w.b])

    emit_load(0)
    cur_s0 = None
    for idx, (s0, c0) in enumerate(items):
        pp = idx % 2
        if s0 != cur_s0:
            cur_s0 = s0
            for i in range(3):
                dma(cx, "sp", xb[i].v, oT_list[i][:, s0:s0 + SB].rearrange("(a p) s -> p a s", p=128), w=[xb[i].b])
        if idx + 1 < len(items):
            emit_load(idx + 1)
        for lo in range(0, PW, 128):
            col = c0 + lo
            tts = []
            for i in range(3):
                bank = (nb % 2) * 3 + i
                ps = cx.psum[bank][:, 0:SB]
                w = wp[i][pp]
                for kc in range(KCs[i]):
                    I(cx, "pe", "matmul", ps, w.v[:, kc, lo:lo + 128], xb[i].v[:, kc, :],
                      start=(kc == 0), stop=(kc == KCs[i] - 1), r=[w.b, xb[i].b], w=[cx.psb[bank]])
                g = gt[i].next()
                dma(cx, "sp", g.v, gT_d[i * D + col:i * D + col + 128, s0:s0 + SB], w=[g.b])
                t = tt[i].next()
                I(cx, "dve", "tensor_tensor", t.v, ps, g.v, ALU.mult, r=[cx.psb[bank], g.b], w=[t.b])
                tts.append(t)
            nb += 1
            I(cx, "pool", "tensor_tensor", tts[0].v, tts[0].v, tts[1].v, ALU.add, r=[tts[0].b, tts[1].b], w=[tts[0].b])
            y = yo.next()
            I(cx, "pool", "tensor_tensor", y.v, tts[0].v, tts[2].v, ALU.add, r=[tts[0].b, tts[2].b], w=[y.b])
            dma(cx, "pool", yT_d[col:col + 128, s0:s0 + SB], y.v, r=[y.b])
    cx.s.barrier()
    cx.arena.release()


def phase_resid_proj(cx, c, K, xT_d, Kdim, W_d, h_d):
    cx.arena.mark()
    ht = Rot(cx, 3, [128, 256], F32, "rh")
    tiles = [(256 * j, 256, 256 * j) for j in range(c.D // 256)]

    def epi(cx, tag, s0, ns, ps, psb):
        t = ht.next()
        dma(cx, "sp", t.v, h_d[s0:s0 + 128, tag:tag + 256], w=[t.b])
        I(cx, "dve", "tensor_tensor", t.v, ps, t.v, ALU.add, r=[psb, t.b], w=[t.b])
        dma(cx, "pool", h_d[s0:s0 + 128, tag:tag + 256], t.v, r=[t.b])
    phase_proj(cx, c, xT_d, Kdim, W_d, tiles, "N", epi)
    cx.arena.release()


def phase_moe(cx, c, K, T, cin, h_d, g_d, wg_d, bg_d, we_d, be_d, wgate_d, wup_d, wdown_d):
    S, D, NT, NE = c.S, c.D, c.NT, c.NE
    NC = D // 128
    NGRP = max(1, S // c.MG)
    TPG = NT // NGRP
    NSL = NGRP * c.CAP
    FC = c.FF // 128
    hn2_d = T["hn2"]
    cx.arena.mark()
    A_f = Tile(cx, [128, NT, NE], F32, "A_f")
    A_b = Tile(cx, [128, NT, NE], BF16, "A_b")
    Wt = Tile(cx, [128, NT, NE], F32, "Wt")
    pos = Tile(cx, [128, NT, NE], F32, "pos")
    iota = Tile(cx, [128, 128], F32, "iota")
    dma(cx, "sp", iota.v, cin["c_iota"], w=[iota.b])
    tri = Tile(cx, [128, 128], BF16, "tri")
    dma(cx, "sp", tri.v, cin["c_tri"], w=[tri.b])
    cx.arena.mark()
    gb = Tile(cx, [128, D], F32, "m_gbc")
    dma(cx, "sp", gb.v, g_d.partition_broadcast(128), w=[gb.b])
    wr = Tile(cx, [128, NC, 40], F32, "wr")
    dma(cx, "sp", wr.v[:, :, 0:8], wg_d.rearrange("(a p) n -> p a n", p=128), w=[wr.b])
    dma(cx, "sp", wr.v[:, :, 8:40], we_d.rearrange("(a p) n -> p a n", p=128), w=[wr.b])
    bias = Tile(cx, [128, 40], F32, "rbias")
    dma(cx, "sp", bias.v[:, 0:8], bg_d.partition_broadcast(128), w=[bias.b])
    dma(cx, "sp", bias.v[:, 8:40], be_d.partition_broadcast(128), w=[bias.b])
    xt = Rot(cx, 2, [128, D], F32, "m_x")
    xn = Rot(cx, 1, [128, D], F32, "m_xn")
    xnb = Rot(cx, 2, [128, D], BF16, "m_xnb")
    junk = Tile(cx, [128, D], BF16, "m_junk")
    xT = Rot(cx, 1, [128, NC, 128], F32, "m_xT")
    sc = Rot(cx, 2, [128, 16], F32, "m_sc")
    lg = Rot(cx, 2, [128, 40], F32, "m_lg")
    tmp = Rot(cx, 2, [128, 8, 4], F32, "m_tmp")
    small = Rot(cx, 2, [128, 32], F32, "m_small")
    nb = 0
    for t in range(NT):
        x = xt.next(); n = xn.next(); nbf = xnb.next(); s = sc.next(); l = lg.next(); tp = tmp.next(); sm = small.next()
        xTt = xT.next()
        dma(cx, "sp", x.v, h_d[t * 128:(t + 1) * 128, :], w=[x.b])
        I(cx, "act", "activation", out=junk.v, in_=x.v, func=AF.Square, accum_out=s.v[:, 0:1], r=[x.b], w=[junk.b, s.b])
        I(cx, "dve", "tensor_scalar", s.v[:, 1:2], s.v[:, 0:1], 1.0 / D, c.EPS, ALU.mult, ALU.add, r=[s.b], w=[s.b])
        I(cx, "act", "activation", out=s.v[:, 1:2], in_=s.v[:, 1:2], func=AF.Sqrt, r=[s.b], w=[s.b])
        I(cx, "dve", "reciprocal", s.v[:, 1:2], s.v[:, 1:2], r=[s.b], w=[s.b])
        I(cx, "dve", "scalar_tensor_tensor", n.v, x.v, s.v[:, 1:2], gb.v, ALU.mult, ALU.mult, r=[x.b, s.b, gb.b], w=[n.b])
        I(cx, "pool", "tensor_copy", nbf.v, n.v, r=[n.b], w=[nbf.b])
        dma(cx, "act", hn2_d[t * 128:(t + 1) * 128, :], nbf.v, r=[nbf.b])
        for g4 in range(0, NC, 4):
            bank = nb % 2
            nb += 1
            pt = cx.psum[bank]
            for j in range(4):
                cc = g4 + j
                I(cx, "pe", "transpose", pt[:, j * 128:(j + 1) * 128], n.v[:, cc * 128:(cc + 1) * 128], iota_ident(cx, K),
                  r=[n.b, K.identf.b], w=[cx.psb[bank]])
            src = pt[:, 0:512].rearrange("p (a b) -> p a b", b=128)
            if nb % 2:
                I(cx, "act", "copy", xTt.v[:, g4:g4 + 4, :], src, r=[cx.psb[bank]], w=[xTt.b])
            else:
                I(cx, "dve", "tensor_copy", xTt.v[:, g4:g4 + 4, :], src, r=[cx.psb[bank]], w=[xTt.b])
        lb = 2
        lp = cx.psum[lb][:, 0:40]
        for kc in range(NC):
            I(cx, "pe", "matmul", lp, xTt.v[:, kc, :], wr.v[:, kc, :], start=(kc == 0), stop=(kc == NC - 1),
              r=[xTt.b, wr.b], w=[cx.psb[lb]])
        I(cx, "dve", "tensor_tensor", l.v, lp, bias.v, ALU.add, r=[cx.psb[lb], bias.b], w=[l.b])
        lgv = l.v[:, 0:8]
        le3 = l.v[:, 8:40].rearrange("p (g e) -> p g e", e=4)
        m = s.v[:, 2:3]; negm = s.v[:, 3:4]; se = s.v[:, 4:5]; m1 = s.v[:, 5:6]; nm1 = s.v[:, 6:7]; m2 = s.v[:, 7:8]
        rr = s.v[:, 8:9]; w1 = s.v[:, 9:10]; w2 = s.v[:, 10:11]
        G1 = sm.v[:, 0:8]; esel = sm.v[:, 8:12]; e1 = sm.v[:, 12:16]; es2 = sm.v[:, 16:20]; e2 = sm.v[:, 20:24]
        asel = sm.v[:, 24:28]; wsel = sm.v[:, 28:32]
        R_ = [l.b, s.b, sm.b, tp.b]
        def D_(meth, *a, **k):
            I(cx, "dve", meth, *a, r=R_, w=R_, **k)
        D_("tensor_reduce", m, lgv, AX.X, ALU.max)
        D_("tensor_scalar", G1, lgv, m, None, ALU.is_equal)
        D_("tensor_scalar", negm, m, -1.0, None, ALU.mult)
        I(cx, "act", "activation", out=sm.v[:, 12:20], in_=lgv, func=AF.Exp, bias=negm, accum_out=se, r=R_, w=R_)
        D_("tensor_tensor", tp.v, le3, G1.unsqueeze(2).broadcast_to([128, 8, 4]), ALU.mult)
        D_("tensor_reduce", esel, tp.v.rearrange("p g e -> p e g"), AX.X, ALU.add)
        D_("tensor_reduce", m1, esel, AX.X, ALU.max)
        D_("tensor_scalar", e1, esel, m1, None, ALU.is_equal)
        D_("scalar_tensor_tensor", es2, e1, -1e30, esel, ALU.mult, ALU.add)
        D_("tensor_reduce", m2, es2, AX.X, ALU.max)
        D_("tensor_scalar", e2, es2, m2, None, ALU.is_equal)
        D_("tensor_scalar", nm1, m1, -1.0, None, ALU.mult)
        I(cx, "act", "activation", out=rr, in_=m2, func=AF.Exp, bias=nm1, r=R_, w=R_)
        D_("tensor_scalar", w1, rr, 1.0, None, ALU.add)
        D_("tensor_tensor", w1, w1, se, ALU.mult)
        D_("reciprocal", w1, w1)
        D_("tensor_tensor", w2, w1, rr, ALU.mult)
        D_("tensor_tensor", asel, e1, e2, ALU.add)
        D_("tensor_scalar", wsel, e1, w1, None, ALU.mult)
        D_("scalar_tensor_tensor", wsel, e2, w2, wsel, ALU.mult, ALU.add)
        A3 = A_f.v[:, t, :].rearrange("p (g e) -> p g e", e=4)
        W3 = Wt.v[:, t, :].rearrange("p (g e) -> p g e", e=4)
        g1b = G1.unsqueeze(2).broadcast_to([128, 8, 4])
        I(cx, "dve", "tensor_tensor", A3, g1b, asel.unsqueeze(1).broadcast_to([128, 8, 4]), ALU.mult, r=R_, w=[A_f.b])
        I(cx, "dve", "tensor_tensor", W3, g1b, wsel.unsqueeze(1).broadcast_to([128, 8, 4]), ALU.mult, r=R_, w=[Wt.b])
    I(cx, "dve", "tensor_copy", A_b.v, A_f.v, r=[A_f.b], w=[A_b.b])
    for t in range(NT):
        g0 = (t // TPG) * TPG
        bank = 3 + t % 2
        pp = cx.psum[bank][:, 0:NE]
        for j in range(g0, t + 1):
            l_ = K.ones.v if j < t else tri.v
            I(cx, "pe", "matmul", pp, l_, A_b.v[:, j, :], start=(j == g0), stop=(j == t),
              r=[K.ones.b, tri.b, A_b.b], w=[cx.psb[bank]])
        I(cx, "act", "copy", pos.v[:, t, :], pp, r=[cx.psb[bank]], w=[pos.b])
    cx.s.barrier()
    cx.arena.release()
    cx.arena.mark()
    hg = Tile(cx, [128, TPG, D], BF16, "hg")
    sel = Rot(cx, 2, [128, TPG, 128], BF16, "sel")
    xg = Rot(cx, 2, [128, NC, 128], BF16, "xg")
    nb = 0
    for gi in range(NGRP):
        dma(cx, "sp", hg.v, hn2_d[gi * c.MG:gi * c.MG + TPG * 128, :].rearrange("(a p) d -> p a d", p=128), w=[hg.b])
        for e in range(NE):
            sl = sel.next()
            for i in range(TPG):
                t = gi * TPG + i
                I(cx, "dve" if i % 2 else "pool", "tensor_scalar", sl.v[:, i, :], iota.v, pos.v[:, t, e:e + 1],
                  A_f.v[:, t, e:e + 1], ALU.is_equal, ALU.mult, r=[iota.b, pos.b, A_f.b], w=[sl.b])
            x = xg.next()
            for c4 in range(0, NC, 4):
                bank = nb % 3
                nb += 1
                for j in range(4):
                    cc = c4 + j
                    for i in range(TPG):
                        I(cx, "pe", "matmul", cx.psum[bank][:, j * 128:(j + 1) * 128], hg.v[:, i, cc * 128:(cc + 1) * 128],
                          sl.v[:, i, :], start=(i == 0), stop=(i == TPG - 1), r=[hg.b, sl.b], w=[cx.psb[bank]])
                src = cx.psum[bank][:, 0:512].rearrange("p (a b) -> p a b", b=128)
                if nb % 2:
                    I(cx, "act", "copy", x.v[:, c4:c4 + 4, :], src, r=[cx.psb[bank]], w=[x.b])
                else:
                    I(cx, "dve", "tensor_copy", x.v[:, c4:c4 + 4, :], src, r=[cx.psb[bank]], w=[x.b])
            dma(cx, "pool", T["xg"][e * D:(e + 1) * D, gi * 128:(gi + 1) * 128].rearrange("(a p) s -> p a s", p=128),
                x.v, r=[x.b])
    cx.s.barrier()
    cx.arena.release()
    cs = Cfg.__new__(Cfg)
    cs.__dict__.update(c.__dict__)
    cs.S = NSL; cs.SB = NSL
    cx.arena.mark()
    gs = Tile(cx, [128, FC, NSL], BF16, "gs")
    ao = Rot(cx, 2, [128, NSL], BF16, "ao")
    yo = Rot(cx, 3, [128, 256], BF16, "yo")
    for e in range(NE):
        xTe = T["xg"][e * D:(e + 1) * D, :]
        tiles = [(128 * f, 128, f) for f in range(FC)]

        def epi_g(cx, tag, s0, ns, ps, psb):
            I(cx, "act", "activation", out=gs.v[:, tag, s0:s0 + ns], in_=ps, func=AF.Silu, r=[psb], w=[gs.b])
        phase_proj(cx, cs, xTe, D, wgate_d[e], tiles, "T", epi_g)

        def epi_u(cx, tag, s0, ns, ps, psb):
            a = ao.next()
            I(cx, "dve", "tensor_tensor", a.v[:, 0:ns], ps, gs.v[:, tag, s0:s0 + ns], ALU.mult, r=[psb, gs.b], w=[a.b])
            dma(cx, "pool", T["aT"][tag * 128:(tag + 1) * 128, s0:s0 + ns], a.v[:, 0:ns], r=[a.b])
        phase_proj(cx, cs, xTe, D, wup_d[e], tiles, "T", epi_u)
        tiles_d = [(256 * j, 256, 256 * j) for j in range(D // 256)]

        def epi_d(cx, tag, s0, ns, ps, psb, e=e):
            y = yo.next()
            I(cx, "act" if (s0 // 128) % 2 else "dve", "copy" if (s0 // 128) % 2 else "tensor_copy", y.v, ps, r=[psb], w=[y.b])
            dma(cx, "pool", T["yexp"][e * NSL + s0:e * NSL + s0 + 128, tag:tag + 256], y.v, r=[y.b])
        phase_proj(cx, cs, T["aT"], c.FF, wdown_d[e], tiles_d, "N", epi_d)
    cx.arena.release()
    cx.arena.mark()
    swT = Tile(cx, [128, NE, TPG * 128], BF16, "swT")
    selw = Rot(cx, 3, [128, 128], BF16, "selw")
    yb = Rot(cx, 2, [128, NE, 512], BF16, "yb")
    ht = Rot(cx, 3, [128, 512], F32, "mh")
    nb = 0
    CW5 = 512 if D >= 512 else D
    for gi in range(NGRP):
        for e in range(NE):
            for i in range(TPG):
                t = gi * TPG + i
                s_ = selw.next()
                I(cx, "dve" if i % 2 else "pool", "tensor_scalar", s_.v, iota.v, pos.v[:, t, e:e + 1], Wt.v[:, t, e:e + 1],
                  ALU.is_equal, ALU.mult, r=[iota.b, pos.b, Wt.b], w=[s_.b])
                bank = 6 + nb % 2
                nb += 1
                pt = psum_bf16(cx, bank)
                I(cx, "pe", "transpose", pt[:, 0:128], s_.v, K.ident.v, r=[s_.b, K.ident.b], w=[cx.psb[bank]])
                I(cx, "act", "copy", swT.v[:, e, i * 128:(i + 1) * 128], pt[:, 0:128], r=[cx.psb[bank]], w=[swT.b])
        chunks = list(range(0, D, CW5))

        def load_y(ci, gi=gi):
            y = yb.next()
            c0 = chunks[ci]
            src = T["yexp"].rearrange("(e s) d -> s e d", e=NE)[gi * 128:(gi + 1) * 128, :, c0:c0 + CW5]
            dma(cx, "sp", y.v[:, :, 0:CW5], src, w=[y.b])
            return y
        ynext = load_y(0)
        for ci, c0 in enumerate(chunks):
            y = ynext
            if ci + 1 < len(chunks):
                ynext = load_y(ci + 1)
            for i in range(TPG):
                t = gi * TPG + i
                bank = nb % 3
                nb += 1
                ps = cx.psum[bank][:, 0:CW5]
                for e in range(NE):
                    I(cx, "pe", "matmul", ps, swT.v[:, e, i * 128:(i + 1) * 128], y.v[:, e, 0:CW5],
                      start=(e == 0), stop=(e == NE - 1), r=[swT.b, y.b], w=[cx.psb[bank]])
                h = ht.next()
                dma(cx, "sp", h.v[:, 0:CW5], h_d[t * 128:(t + 1) * 128, c0:c0 + CW5], w=[h.b])
                I(cx, "dve", "tensor_tensor", h.v[:, 0:CW5], ps, h.v[:, 0:CW5], ALU.add, r=[cx.psb[bank], h.b], w=[h.b])
                dma(cx, "pool", h_d[t * 128:(t + 1) * 128, c0:c0 + CW5], h.v[:, 0:CW5], r=[h.b])
    cx.s.barrier()
    cx.arena.release()
    cx.arena.release()


def iota_ident(cx, K):
    return K.identf.v


def phase_final_norm(cx, c, K, h_d, g_d, out_d):
    D, S = c.D, c.S
    cx.arena.mark()
    gb = Tile(cx, [128, D], F32, "f_gbc")
    dma(cx, "sp", gb.v, g_d.partition_broadcast(128), w=[gb.b])
    xt = Rot(cx, 2, [128, D], F32, "f_x")
    xo = Rot(cx, 2, [128, D], F32, "f_o")
    junk = Tile(cx, [128, D], BF16, "f_junk")
    sc = Rot(cx, 2, [128, 2], F32, "f_sc")
    for t in range(S // 128):
        x = xt.next(); o = xo.next(); s = sc.next()
        dma(cx, "sp", x.v, h_d[t * 128:(t + 1) * 128, :], w=[x.b])
        I(cx, "act", "activation", out=junk.v, in_=x.v, func=AF.Square, accum_out=s.v[:, 0:1], r=[x.b], w=[junk.b, s.b])
        I(cx, "dve", "tensor_scalar", s.v[:, 1:2], s.v[:, 0:1], 1.0 / D, c.EPS, ALU.mult, ALU.add, r=[s.b], w=[s.b])
        I(cx, "act", "activation", out=s.v[:, 1:2], in_=s.v[:, 1:2], func=AF.Sqrt, r=[s.b], w=[s.b])
        I(cx, "dve", "reciprocal", s.v[:, 1:2], s.v[:, 1:2], r=[s.b], w=[s.b])
        I(cx, "dve", "scalar_tensor_tensor", o.v, x.v, s.v[:, 1:2], gb.v, ALU.mult, ALU.mult, r=[x.b, s.b, gb.b], w=[o.b])
        dma(cx, "act", out_d[t * 128:(t + 1) * 128, :], o.v, r=[o.b], out=True)
    cx.s.barrier()
    cx.arena.release()


WEIGHT_SHAPES = lambda c: {
    "attn_norm_g": [c.DEPTH, c.D], "w_in": [c.DEPTH, c.D, c.NIN], "q_norm_g": [c.DEPTH, c.CQ],
    "kv_norm_g": [c.DEPTH, c.CKV], "wq_b": [c.DEPTH, c.CQ, c.CH * (c.CN + c.CR)],
    "wkv_b": [c.DEPTH, c.CKV, c.CH * (c.CN + c.CV)], "sinks": [c.DEPTH, c.BH],
    "w_out_a": [c.DEPTH, c.AW, c.D], "w_out_b": [c.DEPTH, c.BW, c.D], "w_out_c": [c.DEPTH, c.CW, c.D],
    "w_o": [c.DEPTH, c.D, c.D], "ffn_norm_g": [c.DEPTH, c.D], "w_group": [c.DEPTH, c.D, c.NG],
    "b_group": [c.DEPTH, c.NG], "w_expert": [c.DEPTH, c.D, c.NE], "b_expert": [c.DEPTH, c.NE],
    "w_gate": [c.DEPTH, c.NE, c.D, c.FF], "w_up": [c.DEPTH, c.NE, c.D, c.FF],
    "w_down": [c.DEPTH, c.NE, c.FF, c.D], "final_norm_g": [c.D],
}


def build_forward(c, NB, debug=(), stop_after=None):
    cnp = const_inputs(c)

    def body(cx):
        nc = cx.nc
        cx.debug = set(debug)
        S, D = c.S, c.D
        x_d = nc.dram_tensor("x", [NB * S, D], F32, kind="ExternalInput").ap()
        pos_d = nc.dram_tensor("positions", [NB * S], I32, kind="ExternalInput").ap()
        Wd = {k: nc.dram_tensor(k, shp, F32, kind="ExternalInput").ap() for k, shp in WEIGHT_SHAPES(c).items()}
        cin = {k: nc.dram_tensor(k, list(v.shape), CONST_DT[k], kind="ExternalInput").ap() for k, v in cnp.items()}
        out_d = nc.dram_tensor("out", [NB * S, D], F32, kind="ExternalOutput").ap()
        K = load_consts(cx, c, cin)
        K.identf = Tile(cx, [128, 128], F32, "identf")
        dma(cx, "sp", K.identf.v, cin["c_identf"], w=[K.identf.b])
        T = {}
        NGRP = max(1, S // c.MG)
        NSL = NGRP * c.CAP
        spec = {"h": ([S, D], F32), "hnT": ([D, S], BF16), "qaT": ([c.AW, S], BF16), "kaT": ([c.AW, S], BF16),
                "va": ([S, c.AW], BF16), "qbT": ([c.BW, S], BF16), "kbT": ([c.BKW, S], BF16), "vb": ([S, c.BKW], BF16),
                "cqT": ([c.CQ, S], BF16), "ckvT": ([c.CKV, S], BF16), "kpeT": ([64, S], BF16),
                "gT": ([3 * D, S], BF16), "qnT": ([c.CH * 128, S], BF16), "qpeT": ([c.CH * 64, S], BF16),
                "knT": ([c.CH * 128, S], BF16), "vc": ([S, c.CH * 128], BF16),
                "oaT": ([c.AW, S], BF16), "obT": ([c.BW, S], BF16), "ocT": ([c.CW, S], BF16), "yT": ([D, S], BF16),
                "hn2": ([S, D], BF16), "xg": ([c.NE * D, NSL], BF16), "aT": ([c.FF, NSL], BF16),
                "yexp": ([c.NE * NSL, D], BF16),
                "cosA": ([128, S], F32), "sinA": ([128, S], F32), "cosB": ([128, S], F32), "sinB": ([128, S], F32)}
        for k, (shp, dt) in spec.items():
            T[k] = dram(cx, k, shp, dt)
        E = make_epilogues(cx, c, K, None)
        cx.s.barrier()
        for b in range(NB):
            xb_d = x_d[b * S:(b + 1) * S, :]
            cx.arena.mark()
            ct = Tile(cx, [128, S], F32, "ropec"); st = Tile(cx, [128, S], F32, "ropes")
            for which, (cn, sn) in enumerate((("cosA", "sinA"), ("cosB", "sinB"))):
                build_rope(cx, c, K, pos_d[b * S:(b + 1) * S], which, ct, st)
                dma(cx, "sp", T[cn], ct.v, r=[ct.b])
                dma(cx, "sp", T[sn], st.v, r=[st.b])
                cx.s.barrier()
            cx.arena.release()
            for t in range(S // 128):
                dma(cx, "sp" if t % 2 else "act", T["h"][t * 128:(t + 1) * 128, :], xb_d[t * 128:(t + 1) * 128, :])
            cx.s.barrier()
            R = T
            for l in range(c.DEPTH):
                phase_rmsnorm_T(cx, c, K, T["h"], Wd["attn_norm_g"][l], T["hnT"])
                if stop_after == "norm": break
                phase_A(cx, c, K, E, R, T, T["hnT"], Wd["w_in"][l])
                if stop_after == "A": break
                phase_mla_up(cx, c, K, E, R, T, Wd["q_norm_g"][l], Wd["kv_norm_g"][l], Wd["wq_b"][l], Wd["wkv_b"][l])
                if stop_after == "mla_up": break
                phase_attn_causal(cx, c, K, c.AH, [(T["qaT"], T["kaT"], 128)], T["va"], 128, c.AD ** -0.5, T["oaT"],
                                  moba=True, cin=cin)
                if stop_after == "moba": break
                phase_swa(cx, c, K, T["qbT"], T["kbT"], T["vb"], Wd["sinks"][l], T["obT"], cin)
                if stop_after == "swa": break
                phase_attn_causal(cx, c, K, c.CH, [(T["qnT"], T["knT"], 128), (T["qpeT"], T["kpeT"], 64)], T["vc"], 128,
                                  (c.CN + c.CR) ** -0.5, T["ocT"], mla_shared_k=True)
                if stop_after == "mla": break
                phase_out_gate(cx, c, K, [T["oaT"], T["obT"], T["ocT"]],
                               [Wd["w_out_a"][l], Wd["w_out_b"][l], Wd["w_out_c"][l]], T["gT"], T["yT"])
                phase_resid_proj(cx, c, K, T["yT"], D, Wd["w_o"][l], T["h"])
                if stop_after == "mix": break
                phase_moe(cx, c, K, T, cin, T["h"], Wd["ffn_norm_g"][l], Wd["w_group"][l], Wd["b_group"][l],
                          Wd["w_expert"][l], Wd["b_expert"][l], Wd["w_gate"][l], Wd["w_up"][l], Wd["w_down"][l])
                if stop_after == "moe": break
            phase_final_norm(cx, c, K, T["h"], Wd["final_norm_g"], out_d[b * S:(b + 1) * S, :])
    nc = build_program(body)
    return nc, cnp


_CACHE = {}


def kernel(**inputs):
    c = Cfg()
    if "nc" not in _CACHE:
        _CACHE["nc"] = build_forward(c, 1)
    nc, cnp = _CACHE["nc"]
    B = inputs["x"].shape[0]
    in_maps = []
    for b in range(B):
        m = {"x": np.ascontiguousarray(np.asarray(inputs["x"][b], dtype=np.float32)),
             "positions": np.ascontiguousarray(np.asarray(inputs["positions"][b]).astype(np.int32))}
        for k in WEIGHT_SHAPES(c):
            m[k] = np.asarray(inputs[k], dtype=np.float32)
        m.update(cnp)
        in_maps.append(m)
    res = run_bass_kernel_spmd(nc, in_maps, core_ids=list(range(B)))
    out = np.stack([np.asarray(res.results[b]["out"]) for b in range(B)], axis=0)
    return out.astype(np.float32)
```

```python
import numpy as np
import contextlib
import concourse.bass as bass
import concourse.mybir as mybir
from concourse.bass_utils import run_bass_kernel_spmd

F32 = mybir.dt.float32
BF16 = mybir.dt.bfloat16
I32 = mybir.dt.int32
ALU = mybir.AluOpType
AF = mybir.ActivationFunctionType
AX = mybir.AxisListType

ENGS = ("pe", "act", "dve", "pool", "sp")
NDSEM = {"sp": 24, "pool": 24, "act": 8}


class Op:
    __slots__ = ("eng", "fn", "deps", "dma", "sig", "sigval", "dslot", "dval", "out")

    def __init__(self, eng, fn, dma):
        self.eng = eng
        self.fn = fn
        self.dma = dma
        self.deps = []
        self.sig = False
        self.sigval = 0
        self.dslot = 0
        self.dval = 0
        self.out = False


class Buf:
    __slots__ = ("name", "w", "rs", "rd")

    def __init__(self, name=""):
        self.name = name
        self.w = None
        self.rs = {}
        self.rd = []


class Sched:
    def __init__(self):
        self.ops = {e: [] for e in ENGS}
        self.ndma = {e: 0 for e in ENGS}

    def add(self, eng, fn, r=(), w=(), dma=False, out=False):
        op = Op(eng, fn, dma)
        op.out = out
        deps = {}
        def adddep(d):
            if d is None or d is op:
                return
            if d.eng == "pe" and eng == "pe" and not d.dma and not dma:
                return
            deps[id(d)] = d
        for b in r:
            adddep(b.w)
        for b in w:
            adddep(b.w)
            for d in b.rs.values():
                adddep(d)
            for d in b.rd:
                adddep(d)
        op.deps = list(deps.values())
        for b in r:
            if dma:
                b.rd.append(op)
            else:
                b.rs[eng] = op
        for b in w:
            b.w = op
            b.rs = {}
            b.rd = []
        if dma:
            K = NDSEM[eng]
            i = self.ndma[eng]
            self.ndma[eng] = i + 1
            op.dslot = i % K
            op.dval = 16 * (i // K + 1)
        self.ops[eng].append(op)
        return op

    def barrier(self):
        lasts = []
        for e in ENGS:
            comp = [o for o in self.ops[e] if not o.dma and o.fn is not None]
            if comp:
                lasts.append(comp[-1])
            lasts.extend(o for o in self.ops[e][-64:] if o.dma)
        for e in ENGS:
            op = Op(e, None, False)
            op.deps = [d for d in lasts]
            self.ops[e].append(op)

    def emit(self, nc, block, csem, dsem):
        for e in ENGS:
            for op in self.ops[e]:
                for d in op.deps:
                    d.sig = True
        for e in ENGS:
            cnt = 0
            for op in self.ops[e]:
                if not op.dma and op.sig and op.fn is not None:
                    cnt += 1
                    op.sigval = cnt
        sched = self

        def run(e, engine):
            waited = {}
            def wait(key, sem, val):
                if waited.get(key, 0) >= val:
                    return
                engine.wait_ge(sem, val)
                waited[key] = val
            outs = []
            for op in sched.ops[e]:
                for d in op.deps:
                    if d.dma:
                        wait(("d", d.eng, d.dslot), dsem[d.eng][d.dslot], d.dval)
                    else:
                        wait(("c", d.eng), csem[d.eng], d.sigval)
                if op.fn is None:
                    continue
                if op.dma:
                    if op.dval > 16:
                        wait(("d", e, op.dslot), dsem[e][op.dslot], op.dval - 16)
                    ins = op.fn(engine)
                    ins.then_inc(dsem[e][op.dslot], 16)
                    if op.out:
                        outs.append(op)
                else:
                    ins = op.fn(engine)
                    if op.sig:
                        ins.then_inc(csem[e], 1)
            for op in outs:
                wait(("d", e, op.dslot), dsem[e][op.dslot], op.dval)

        @block.tensor
        def _(eng):
            run("pe", eng)

        @block.scalar
        def _(eng):
            run("act", eng)

        @block.vector
        def _(eng):
            run("dve", eng)

        @block.gpsimd
        def _(eng):
            run("pool", eng)

        @block.sync
        def _(eng):
            run("sp", eng)


class Arena:
    def __init__(self, handle_f32, nbytes):
        self.h = handle_f32
        self.nbytes = nbytes
        self.off = 0
        self.marks = []

    def alloc(self, nelem, dtype):
        esz = 2 if dtype == BF16 else 4
        nb = (nelem * esz + 63) // 64 * 64
        assert self.off + nb <= self.nbytes, ("SBUF arena overflow", self.off, nb, self.nbytes)
        a = self.h[:, self.off // 4:(self.off + nb) // 4]
        self.off += nb
        if dtype == BF16:
            a = a.bitcast(BF16)
        elif dtype == I32:
            a = a.bitcast(I32)
        return a[:, 0:nelem]

    def mark(self):
        self.marks.append(self.off)

    def release(self):
        self.off = self.marks.pop()


class Ctx:
    pass


def build_program(body, arena_bytes=180 * 1024):
    nc = bass.Bass("TRN2", target_bir_lowering=False)
    cx = Ctx()
    cx.nc = nc
    cx.s = Sched()
    with contextlib.ExitStack() as es:
        arena_h = es.enter_context(nc.sbuf_tensor("arena", [128, arena_bytes // 4], F32))
        cx.arena = Arena(arena_h, arena_bytes)
        cx.psum = []
        cx.psb = []
        for i in range(8):
            p = es.enter_context(nc.psum_tensor(f"ps{i}", [128, 512], F32))
            cx.psum.append(p)
            cx.psb.append(Buf(f"ps{i}"))
        csem = {e: es.enter_context(nc.semaphore(f"c_{e}")) for e in ENGS}
        dsem = {e: [es.enter_context(nc.semaphore(f"d_{e}{i}")) for i in range(n)] for e, n in NDSEM.items()}
        body(cx)
        block = es.enter_context(nc.Block())
        cx.s.emit(nc, block, csem, dsem)
    return nc


import math
import numpy as np
import ml_dtypes


class Cfg:
    def __init__(self, **kw):
        self.D = 4096; self.S = 4096; self.DEPTH = 2
        self.AH = 16; self.AD = 128; self.MBLK = 256; self.TOPK = 3
        self.BH = 32; self.BKV = 4; self.BD = 64; self.WIN = 128
        self.CH = 16; self.CQ = 1024; self.CKV = 512; self.CN = 128; self.CR = 64; self.CV = 128
        self.NG = 8; self.EPG = 4; self.FF = 768
        self.SB = 1024
        self.MG = 1024
        self.CAP = 128
        self.EPS = 1e-6; self.THETA = 10000.0
        for k, v in kw.items():
            setattr(self, k, v)
        c = self
        c.AW = c.AH * c.AD; c.BW = c.BH * c.BD; c.BKW = c.BKV * c.BD; c.CW = c.CH * c.CV
        sizes = (c.AW, c.AW, c.AW, c.BW, c.BKW, c.BKW, c.CQ, c.CKV, c.CR, c.D, c.D, c.D)
        c.OFF = [0] + list(np.cumsum(sizes))
        c.NIN = int(c.OFF[-1])
        c.NE = c.NG * c.EPG
        c.NT = c.S // 128
        c.NBLK = c.S // c.MBLK


def dma(cx, q, out_ap, in_ap, r=(), w=(), out=False, slow=False):
    if slow:
        return cx.s.add(q, lambda e: e.dma_start(out=out_ap, in_=in_ap, allow_slow_non_contiguous=True),
                        r=r, w=w, dma=True, out=out)
    return cx.s.add(q, lambda e: e.dma_start(out=out_ap, in_=in_ap), r=r, w=w, dma=True, out=out)


def I(cx, eng, meth, *args, r=(), w=(), **kw):
    return cx.s.add(eng, lambda e: getattr(e, meth)(*args, **kw), r=r, w=w)


class Tile:
    def __init__(self, cx, shape, dtype, name=""):
        n = int(np.prod(shape[1:]))
        self.ap = cx.arena.alloc(n, dtype)
        self.p = shape[0]
        if len(shape) == 3:
            self.v = self.ap[0:shape[0], :].rearrange("p (a b) -> p a b", b=shape[2])
        else:
            self.v = self.ap[0:shape[0], :]
        self.b = Buf(name)


def psum_f32(cx, i):
    return cx.psum[i]


def psum_bf16(cx, i):
    return cx.psum[i][:, :].bitcast(BF16)


class Consts:
    pass


def const_inputs(c):
    bf = ml_dtypes.bfloat16
    d = {}
    d["c_ident"] = np.eye(128, dtype=np.float32).astype(bf)
    def rot(dim, reps):
        P = np.zeros((128, 128), np.float32)
        h = dim // 2
        for r in range(reps):
            o = r * dim
            for i in range(dim):
                if i < h:
                    P[o + i + h, o + i] = -1.0
                else:
                    P[o + i - h, o + i] = 1.0
        return P.astype(bf)
    d["c_rot128"] = rot(128, 1)
    d["c_rot64"] = rot(64, 2)
    NEG = -30000.0
    k = np.arange(128)[:, None]
    q = np.arange(512)[None, :]
    m = np.zeros((128, 4, 512), np.float32)
    for j in range(4):
        m[:, j, :] = np.where(j * 128 + k <= q, 0.0, NEG)
    d["c_cmask"] = m.reshape(128, 2048).astype(bf)
    qq = np.arange(128)[None, :]
    md = np.where(k <= qq, 0.0, NEG)
    mp = np.where(k > qq, 0.0, NEG)
    d["c_smask"] = np.concatenate([np.tile(md, (1, 4)), np.tile(mp, (1, 4))], axis=1).astype(bf)
    ng = np.full((128, 128), NEG, np.float32)
    d["c_smask2"] = np.concatenate([mp, md, mp, md, ng, md, ng, md], axis=1).astype(bf)
    E = np.zeros((16, c.S), np.float32)
    for n in range(c.NBLK):
        E[n, n * c.MBLK:(n + 1) * c.MBLK] = 1.0
    d["c_eblk"] = E.astype(bf)
    past = np.zeros((128, c.NT, 16), np.float32)
    gm = np.zeros((128, c.NT, 16), np.float32)
    for t in range(c.NT):
        own = (t * 128) // c.MBLK
        past[:, t, :own] = 1.0
        gm[:, t, own:] = -1e30
    d["c_past"] = past.reshape(128, c.NT * 16)
    d["c_gmask"] = gm.reshape(128, c.NT * 16)
    def fr(dim):
        p = np.arange(128)
        i = (p % dim) % (dim // 2)
        return (-(2.0 * i) / dim).astype(np.float32).reshape(128, 1)
    d["c_fexp"] = np.concatenate([fr(128), fr(64)], axis=1)
    d["c_iota"] = np.tile(np.arange(128, dtype=np.float32)[None, :], (128, 1))
    tri = (np.arange(128)[:, None] < np.arange(128)[None, :]).astype(np.float32)
    d["c_tri"] = tri.astype(bf)
    d["c_ones"] = np.ones((128, 128), np.float32).astype(bf)
    d["c_identf"] = np.eye(128, dtype=np.float32)
    return d


CONST_DT = {"c_ident": BF16, "c_rot128": BF16, "c_rot64": BF16, "c_cmask": BF16, "c_smask": BF16,
            "c_eblk": BF16, "c_smask2": BF16, "c_past": F32, "c_gmask": F32, "c_fexp": F32, "c_iota": F32,
            "c_tri": BF16, "c_ones": BF16, "c_identf": F32}


def load_consts(cx, c, cin):
    K = Consts()
    def ld(name, shape, dt):
        t = Tile(cx, shape, dt, name)
        dma(cx, "sp", t.v, cin[name], w=[t.b])
        return t
    K.ident = ld("c_ident", [128, 128], BF16)
    K.rot128 = ld("c_rot128", [128, 128], BF16)
    K.rot64 = ld("c_rot64", [128, 128], BF16)
    K.cmask = ld("c_cmask", [128, 2048], BF16)
    K.smask = ld("c_smask", [128, 1024], BF16)
    K.ones = ld("c_ones", [128, 128], BF16)
    K.fexp = ld("c_fexp", [128, 2], F32)
    return K


def build_rope(cx, c, K, pos_dram, which, cosT, sinT):
    S = c.S
    cx.arena.mark()
    pi = Tile(cx, [128, S], I32, "pos_i")
    pf = Tile(cx, [128, S], F32, "pos_f")
    invf = Tile(cx, [128, 1], F32, "invf")
    tmp = Tile(cx, [128, S], F32, "rtmp")
    dma(cx, "sp", pi.v, pos_dram.partition_broadcast(128), w=[pi.b])
    I(cx, "dve", "tensor_copy", pf.v, pi.v, r=[pi.b], w=[pf.b])
    col = K.fexp.v[:, which:which + 1]
    I(cx, "act", "activation", out=invf.v, in_=col, func=AF.Exp, scale=math.log(c.THETA),
      r=[K.fexp.b], w=[invf.b])
    TWO_PI = 2.0 * math.pi
    ki = Tile(cx, [128, S], I32, "rk_i")
    for (dst, shift) in ((sinT, 0.0), (cosT, 0.5 * math.pi)):
        I(cx, "dve", "tensor_scalar", tmp.v, pf.v, invf.v[:, 0:1], shift, ALU.mult, ALU.add,
          r=[pf.b, invf.b], w=[tmp.b])
        I(cx, "dve", "tensor_scalar", pi.v.bitcast(F32), tmp.v, 1.0 / TWO_PI, 0.0, ALU.mult, ALU.add,
          r=[tmp.b], w=[pi.b])
        I(cx, "dve", "tensor_copy", ki.v, pi.v.bitcast(F32), r=[pi.b], w=[ki.b])
        I(cx, "dve", "tensor_copy", pi.v.bitcast(F32), ki.v, r=[ki.b], w=[pi.b])
        I(cx, "dve", "scalar_tensor_tensor", tmp.v, pi.v.bitcast(F32), -TWO_PI, tmp.v, ALU.mult, ALU.add,
          r=[pi.b, tmp.b], w=[tmp.b])
        I(cx, "act", "activation", out=dst.v, in_=tmp.v, func=AF.Sin, r=[tmp.b], w=[dst.b])
    cx.s.barrier()
    cx.arena.release()


def phase_rmsnorm_T(cx, c, K, h_d, g_d, hnT_d, hn_tok_d=None, hn_f32T_d=None):
    D, S = c.D, c.S
    NC = D // 128
    cx.arena.mark()
    gb = Tile(cx, [128, D], F32, "g_bc")
    dma(cx, "sp", gb.v, g_d.partition_broadcast(128), w=[gb.b])
    xt = [Tile(cx, [128, D], F32, f"x{i}") for i in range(2)]
    junk = Tile(cx, [128, D], BF16, "junk")
    xn = [Tile(cx, [128, D], BF16, f"xn{i}") for i in range(2)]
    ss = [Tile(cx, [128, 2], F32, f"ss{i}") for i in range(2)]
    TB = 4 if S >= 512 else S // 128
    hT = [Tile(cx, [128, NC, TB * 128], BF16, f"hT{i}") for i in range(2)]
    nblk = S // (128 * TB)
    pb = 0
    for blk in range(nblk):
        ht = hT[blk % 2]
        for sub in range(TB):
            t = blk * TB + sub
            x = xt[t % 2]; n = xn[t % 2]; s2 = ss[t % 2]
            dma(cx, "sp", x.v, h_d[t * 128:(t + 1) * 128, :], w=[x.b])
            I(cx, "act", "activation", out=junk.v, in_=x.v, func=AF.Square, accum_out=s2.v[:, 0:1],
              r=[x.b], w=[junk.b, s2.b])
            I(cx, "dve", "tensor_scalar", s2.v[:, 1:2], s2.v[:, 0:1], 1.0 / D, c.EPS, ALU.mult, ALU.add,
              r=[s2.b], w=[s2.b])
            I(cx, "act", "activation", out=s2.v[:, 1:2], in_=s2.v[:, 1:2], func=AF.Sqrt, r=[s2.b], w=[s2.b])
            I(cx, "dve", "reciprocal", s2.v[:, 1:2], s2.v[:, 1:2], r=[s2.b], w=[s2.b])
            I(cx, "dve", "scalar_tensor_tensor", n.v, x.v, s2.v[:, 1:2], gb.v, ALU.mult, ALU.mult,
              r=[x.b, s2.b, gb.b], w=[n.b])
            if hn_tok_d is not None:
                dma(cx, "sp", hn_tok_d[t * 128:(t + 1) * 128, :], n.v, r=[n.b])
            for g8 in range(0, NC, 8):
                bank = pb % 2
                pb += 1
                pt = psum_bf16(cx, bank)
                nn = min(8, NC - g8)
                for j in range(nn):
                    cc = g8 + j
                    I(cx, "pe", "transpose", pt[:, j * 128:(j + 1) * 128], n.v[:, cc * 128:(cc + 1) * 128],
                      K.ident.v, r=[n.b, K.ident.b], w=[cx.psb[bank]])
                eng = "act" if (pb % 2) else "dve"
                src = pt[:, 0:nn * 128].rearrange("p (a b) -> p a b", b=128)
                dstv = ht.v[:, g8:g8 + nn, sub * 128:(sub + 1) * 128]
                if eng == "act":
                    I(cx, "act", "copy", dstv, src, r=[cx.psb[bank]], w=[ht.b])
                else:
                    I(cx, "dve", "tensor_copy", dstv, src, r=[cx.psb[bank]], w=[ht.b])
        dst = hnT_d[:, blk * TB * 128:(blk + 1) * TB * 128].rearrange("(a p) s -> p a s", p=128)
        dma(cx, "sp", dst, ht.v, r=[ht.b])
    cx.s.barrier()
    cx.arena.release()


def phase_proj(cx, c, xT_d, Kdim, W_d, col_tiles, mode, epi, SB=None, PW=256, banks=(2, 3), prep=None):
    S = c.S
    SB = SB or min(c.SB, S)
    KC = Kdim // 128
    cx.arena.mark()
    xb = Tile(cx, [128, KC, SB], BF16, "xblk")
    KQ = 8 if KC >= 8 else KC
    NP = KC // KQ
    nstg = NP if NP >= 2 else 2
    stg = [Tile(cx, [128, KQ, PW], F32, f"wstg{i}") for i in range(nstg)]
    wp = [Tile(cx, [128, KC, PW], BF16, f"wp{i}") for i in range(2)]
    panels = []
    cur = []
    for ct in col_tiles:
        if cur and (ct[0] != cur[-1][0] + cur[-1][1] or (ct[0] + ct[1] - cur[0][0]) > PW):
            panels.append(cur); cur = []
        cur.append(ct)
    if cur:
        panels.append(cur)
    items = [(s0, pan) for s0 in range(0, S, SB) for pan in panels]
    TS = 512 if SB >= 512 else SB
    state = {"nstg": 0, "nbank": 0}

    def emit_load(idx):
        s0, pan = items[idx]
        w = wp[idx % 2]
        c0 = pan[0][0]
        pw = pan[-1][0] + pan[-1][1] - c0
        sts = []
        for q in range(NP):
            st = stg[state["nstg"] % nstg]
            state["nstg"] += 1
            src = W_d[q * KQ * 128:(q + 1) * KQ * 128, c0:c0 + pw].rearrange("(a p) n -> p a n", p=128)
            dma(cx, "sp", st.v[:, :, 0:pw], src, w=[st.b])
            sts.append(st)
        deferred = []
        for q in range(NP):
            st = sts[q]
            args = (w.v[:, q * KQ:(q + 1) * KQ, 0:pw], st.v[:, :, 0:pw])
            if NP >= 2 and q == 0:
                I(cx, "pool", "tensor_copy", *args, r=[st.b], w=[w.b])
            else:
                state["ncast"] = state.get("ncast", 0) + 1
                deferred.append((("act", "copy") if state["ncast"] % 3 else ("dve", "tensor_copy"), args, st, w))
        return deferred

    def run_deferred(deferred):
        for ((eng, meth), args, st, w2) in deferred:
            I(cx, eng, meth, *args, r=[st.b], w=[w2.b])

    run_deferred(emit_load(0))
    cur_s0 = None
    for idx, (s0, pan) in enumerate(items):
        if s0 != cur_s0:
            cur_s0 = s0
            dma(cx, "sp", xb.v, xT_d[:, s0:s0 + SB].rearrange("(a p) s -> p a s", p=128), w=[xb.b])
            if prep is not None:
                prep(cx, xb, SB)
        deferred = emit_load(idx + 1) if idx + 1 < len(items) else []
        w = wp[idx % 2]
        c0 = pan[0][0]
        work = []
        for (cs, cw, tag) in pan:
            lo = cs - c0
            step = TS if mode == "T" else 128
            for ts in range(0, SB, step):
                work.append((lo, cw, tag, ts))
        half = len(work) // 2
        for wi, (lo, cw, tag, ts) in enumerate(work):
            if wi == half:
                run_deferred(deferred)
                deferred = []
            bank = banks[state["nbank"] % len(banks)]
            state["nbank"] += 1
            if mode == "T":
                ps = cx.psum[bank][0:cw, 0:TS]
                for kc in range(KC):
                    I(cx, "pe", "matmul", ps, w.v[:, kc, lo:lo + cw], xb.v[:, kc, ts:ts + TS],
                      start=(kc == 0), stop=(kc == KC - 1), r=[w.b, xb.b], w=[cx.psb[bank]])
                epi(cx, tag, s0 + ts, TS, ps, cx.psb[bank])
            else:
                ps = cx.psum[bank][:, 0:cw]
                for kc in range(KC):
                    I(cx, "pe", "matmul", ps, xb.v[:, kc, ts:ts + 128], w.v[:, kc, lo:lo + cw],
                      start=(kc == 0), stop=(kc == KC - 1), r=[w.b, xb.b], w=[cx.psb[bank]])
                epi(cx, tag, s0 + ts, 128, ps, cx.psb[bank])
        run_deferred(deferred)
    cx.s.barrier()
    cx.arena.release()


def dram(cx, name, shape, dtype):
    kind = "ExternalOutput" if name in cx.debug else "Internal"
    return cx.nc.dram_tensor(name, list(shape), dtype, kind=kind).ap()


class Rot:
    def __init__(self, cx, n, shape, dtype, name):
        self.t = [Tile(cx, shape, dtype, f"{name}{i}") for i in range(n)]
        self.i = 0

    def next(self):
        t = self.t[self.i % len(self.t)]
        self.i += 1
        return t


def make_epilogues(cx, c, K, R):
    E = Consts()
    E.xs = Rot(cx, 2, [128, 512], BF16, "e_xs")
    E.t1 = Rot(cx, 2, [128, 512], F32, "e_t1")
    E.t2 = Rot(cx, 2, [128, 512], F32, "e_t2")
    E.cs = Rot(cx, 2, [128, 1024], F32, "e_cs")
    E.ob = Rot(cx, 3, [128, 512], BF16, "e_ob")
    E.rotbank = 4
    E.trbank = 5
    E.n = 0
    return E


def epi_copy(cx, E, ps, psb, cw, ns, dst_ap, func=None, scale_ap=None, scale_buf=None, mul_ap=None, mul_buf=None):
    o = E.ob.next()
    E.n += 1
    ov = o.v[0:cw, 0:ns]
    if func is not None:
        I(cx, "act", "activation", out=ov, in_=ps, func=func, r=[psb], w=[o.b])
    elif scale_ap is not None:
        I(cx, "dve", "tensor_scalar", ov, ps, scale_ap, None, ALU.mult, r=[psb, scale_buf], w=[o.b])
    elif mul_ap is not None:
        I(cx, "dve", "tensor_tensor", ov, ps, mul_ap, ALU.mult, r=[psb, mul_buf], w=[o.b])
    elif E.n % 2:
        I(cx, "act", "copy", ov, ps, r=[psb], w=[o.b])
    else:
        I(cx, "dve", "tensor_copy", ov, ps, r=[psb], w=[o.b])
    dma(cx, "pool", dst_ap, ov, r=[o.b])


def epi_rope(cx, c, K, E, ps, psb, cw, ns, s0, dst_ap, rot, cos_d, sin_d, mul_ap=None, mul_buf=None):
    xs = E.xs.next(); t1 = E.t1.next(); t2 = E.t2.next(); cs = E.cs.next(); o = E.ob.next()
    dma(cx, "sp", cs.v[0:cw, 0:ns], cos_d[0:cw, s0:s0 + ns], w=[cs.b])
    dma(cx, "sp", cs.v[0:cw, 512:512 + ns], sin_d[0:cw, s0:s0 + ns], w=[cs.b])
    xv = xs.v[0:cw, 0:ns]
    if mul_ap is not None:
        I(cx, "dve", "tensor_tensor", xv, ps, mul_ap, ALU.mult, r=[psb, mul_buf], w=[xs.b])
    else:
        I(cx, "act", "copy", xv, ps, r=[psb], w=[xs.b])
    rb = E.rotbank
    rp = cx.psum[rb][0:cw, 0:ns]
    I(cx, "pe", "matmul", rp, rot.v[0:cw, 0:cw], xv, start=True, stop=True, r=[rot.b, xs.b], w=[cx.psb[rb]])
    I(cx, "dve", "tensor_tensor", t1.v[0:cw, 0:ns], xv, cs.v[0:cw, 0:ns], ALU.mult, r=[xs.b, cs.b], w=[t1.b])
    I(cx, "dve", "tensor_tensor", t2.v[0:cw, 0:ns], rp, cs.v[0:cw, 512:512 + ns], ALU.mult,
      r=[cx.psb[rb], cs.b], w=[t2.b])
    I(cx, "pool", "tensor_tensor", o.v[0:cw, 0:ns], t1.v[0:cw, 0:ns], t2.v[0:cw, 0:ns], ALU.add,
      r=[t1.b, t2.b], w=[o.b])
    dma(cx, "pool", dst_ap, o.v[0:cw, 0:ns], r=[o.b])


def epi_T2N(cx, c, K, E, ps, psb, cw, ns, s0, dst_rows_fn, mul_ap=None, mul_buf=None):
    xs = E.xs.next(); o = E.ob.next()
    xv = xs.v[0:cw, 0:ns]
    if mul_ap is not None:
        I(cx, "dve", "tensor_tensor", xv, ps, mul_ap, ALU.mult, r=[psb, mul_buf], w=[xs.b])
    else:
        I(cx, "act", "copy", xv, ps, r=[psb], w=[xs.b])
    tb = E.trbank
    pt = psum_bf16(cx, tb)
    nt = ns // 128
    for i in range(nt):
        I(cx, "pe", "transpose", pt[:, i * 128:i * 128 + cw], xs.v[0:cw, i * 128:(i + 1) * 128],
          K.ident.v[0:cw, 0:cw], r=[xs.b, K.ident.b], w=[cx.psb[tb]])
    ov = o.v[:, 0:nt * cw].rearrange("p (a b) -> p a b", b=cw)
    src = pt[:, 0:nt * 128].rearrange("p (a b) -> p a b", b=128)[:, :, 0:cw]
    I(cx, "act", "copy", ov, src, r=[cx.psb[tb]], w=[o.b])
    dma(cx, "pool", dst_rows_fn(s0, ns), ov, r=[o.b])


def phase_A(cx, c, K, E, R, T, hnT_d, w_in_d):
    O = c.OFF
    tilesT = []
    for h in range(c.AH):
        tilesT.append((O[0] + 128 * h, 128, ("ropeA", T["qaT"], h)))
    for h in range(c.AH):
        tilesT.append((O[1] + 128 * h, 128, ("ropeA", T["kaT"], h)))
    for j in range(c.BW // 128):
        tilesT.append((O[3] + 128 * j, 128, ("ropeB", T["qbT"], j)))
    for j in range(c.BKW // 128):
        tilesT.append((O[4] + 128 * j, 128, ("ropeB", T["kbT"], j)))
    for j in range(c.CQ // 128):
        tilesT.append((O[6] + 128 * j, 128, ("copy", T["cqT"], j)))
    for j in range(c.CKV // 128):
        tilesT.append((O[7] + 128 * j, 128, ("copy", T["ckvT"], j)))
    tilesT.append((O[8], c.CR, ("ropeK", T["kpeT"], 0)))
    for i in range(3):
        for j in range(c.D // 128):
            tilesT.append((O[9 + i] + 128 * j, 128, ("sig", T["gT"], i * (c.D // 128) + j)))

    def epiT(cx, tag, s0, ns, ps, psb):
        kind, dst, j = tag
        if kind == "ropeA":
            epi_rope(cx, c, K, E, ps, psb, 128, ns, s0, dst[j * 128:(j + 1) * 128, s0:s0 + ns], K.rot128,
                     R["cosA"], R["sinA"])
        elif kind == "ropeB":
            epi_rope(cx, c, K, E, ps, psb, 128, ns, s0, dst[j * 128:(j + 1) * 128, s0:s0 + ns], K.rot64,
                     R["cosB"], R["sinB"])
        elif kind == "ropeK":
            epi_rope(cx, c, K, E, ps, psb, 64, ns, s0, dst[0:64, s0:s0 + ns], K.rot64, R["cosB"], R["sinB"])
        elif kind == "copy":
            epi_copy(cx, E, ps, psb, 128, ns, dst[j * 128:(j + 1) * 128, s0:s0 + ns])
        elif kind == "sig":
            epi_copy(cx, E, ps, psb, 128, ns, dst[j * 128:(j + 1) * 128, s0:s0 + ns], func=AF.Sigmoid)
    phase_proj(cx, c, hnT_d, c.D, w_in_d, tilesT, "T", epiT)

    tilesN = []
    for j in range(c.AW // 256):
        tilesN.append((O[2] + 256 * j, 256, (T["va"], 256 * j)))
    for j in range(max(1, c.BKW // 256)):
        wdt = min(256, c.BKW)
        tilesN.append((O[5] + wdt * j, wdt, (T["vb"], wdt * j)))

    def epiN(cx, tag, s0, ns, ps, psb):
        dst, c0 = tag
        cw = ps.shape[1]
        epi_copy(cx, E, ps, psb, 128, cw, dst[s0:s0 + ns, c0:c0 + cw])
    phase_proj(cx, c, hnT_d, c.D, w_in_d, tilesN, "N", epiN)


def phase_mla_up(cx, c, K, E, R, T, qg_d, kvg_d, wq_d, wkv_d):
    for (xT_d, KD, g_d, W_d, which) in ((T["cqT"], c.CQ, qg_d, wq_d, "q"), (T["ckvT"], c.CKV, kvg_d, wkv_d, "kv")):
        KC = KD // 128
        SB = min(c.SB, c.S)
        cx.arena.mark()
        gcol = Tile(cx, [128, KC], F32, "gcol")
        dma(cx, "sp", gcol.v, g_d.rearrange("(a p) -> p a", p=128), w=[gcol.b], slow=True)
        rstd = Tile(cx, [128, SB], F32, "rstd_bc")
        sq = Rot(cx, 2, [128, 512], BF16, "sq")

        def prep(cx, xb, SBn, KC=KC, KD=KD, gcol=gcol, rstd=rstd, sq=sq):
            bank = 6
            for ts in range(0, SBn, 512):
                n = min(512, SBn - ts)
                ps = cx.psum[bank][:, 0:n]
                for kc in range(KC):
                    s = sq.next()
                    I(cx, "act", "activation", out=s.v[:, 0:n], in_=xb.v[:, kc, ts:ts + n], func=AF.Square,
                      r=[xb.b], w=[s.b])
                    I(cx, "pe", "matmul", ps, K.ones.v, s.v[:, 0:n], start=(kc == 0), stop=(kc == KC - 1),
                      r=[K.ones.b, s.b], w=[cx.psb[bank]])
                rv = rstd.v[:, ts:ts + n]
                I(cx, "dve", "tensor_scalar", rv, ps, 1.0 / KD, c.EPS, ALU.mult, ALU.add,
                  r=[cx.psb[bank]], w=[rstd.b])
                I(cx, "act", "activation", out=rv, in_=rv, func=AF.Sqrt, r=[rstd.b], w=[rstd.b])
                I(cx, "dve", "reciprocal", rv, rv, r=[rstd.b], w=[rstd.b])
            for kc in range(KC):
                I(cx, "dve", "tensor_scalar", xb.v[:, kc, :], xb.v[:, kc, :], gcol.v[:, kc:kc + 1], None, ALU.mult,
                  r=[xb.b, gcol.b], w=[xb.b])

        if which == "q":
            tiles = []
            for h in range(c.CH):
                tiles.append((192 * h, 128, ("qn", h)))
                tiles.append((192 * h + 128, 64, ("qpe", h)))

            def epi(cx, tag, s0, ns, ps, psb, rstd=rstd, SB=SB):
                kind, h = tag
                lo = s0 % SB
                if kind == "qn":
                    epi_copy(cx, E, ps, psb, 128, ns, T["qnT"][h * 128:(h + 1) * 128, s0:s0 + ns],
                             mul_ap=rstd.v[:, lo:lo + ns], mul_buf=rstd.b)
                else:
                    epi_rope(cx, c, K, E, ps, psb, 64, ns, s0, T["qpeT"][h * 64:(h + 1) * 64, s0:s0 + ns], K.rot64,
                             R["cosB"], R["sinB"], mul_ap=rstd.v[0:64, lo:lo + ns], mul_buf=rstd.b)
        else:
            tiles = []
            for h in range(c.CH):
                tiles.append((256 * h, 128, ("kn", h)))
                tiles.append((256 * h + 128, 128, ("v", h)))

            def epi(cx, tag, s0, ns, ps, psb, rstd=rstd, SB=SB):
                kind, h = tag
                lo = s0 % SB
                if kind == "kn":
                    epi_copy(cx, E, ps, psb, 128, ns, T["knT"][h * 128:(h + 1) * 128, s0:s0 + ns],
                             mul_ap=rstd.v[:, lo:lo + ns], mul_buf=rstd.b)
                else:
                    def rows(s0, ns, h=h):
                        return T["vc"][s0:s0 + ns, h * 128:(h + 1) * 128].rearrange("(a p) d -> p a d", p=128)
                    epi_T2N(cx, c, K, E, ps, psb, 128, ns, s0, rows, mul_ap=rstd.v[:, lo:lo + ns], mul_buf=rstd.b)
        phase_proj(cx, c, xT_d, KD, W_d, tiles, "T", epi, prep=prep)
        cx.arena.release()


def phase_attn_causal(cx, c, K, H, terms, v_d, dv, scale, oT_d, moba=False, cin=None, mla_shared_k=False):
    S, NT = c.S, c.NT
    NG = S // 512
    cx.arena.mark()
    nterm = len(terms)
    QT = [[Tile(cx, [kd, S], BF16, f"QT{p}") for (_, _, kd) in terms] for p in range(2)]
    KT = [[Tile(cx, [kd, S], BF16, f"KT{p}") for (_, _, kd) in terms] for p in range(2)]
    V = [Tile(cx, [128, NT, dv + 1], BF16, f"V{p}") for p in range(2)]
    for p in range(2):
        I(cx, "pool", "memset", V[p].v[:, :, dv:dv + 1], 1.0, w=[V[p].b])
    PT = Rot(cx, 3, [128, 512], BF16, "PT")
    osb = Rot(cx, 2, [128, 4, dv], BF16, "osb")
    oT = Rot(cx, 2, [128, 512], BF16, "oT")
    rc = Rot(cx, 4, [128, 1], F32, "rc")
    if moba:
        eblk = Tile(cx, [16, S], BF16, "eblk")
        dma(cx, "sp", eblk.v, cin["c_eblk"], w=[eblk.b])
        past = Tile(cx, [128, NT, 16], F32, "past")
        dma(cx, "sp", past.v, cin["c_past"].rearrange("p (a b) -> p a b", b=16), w=[past.b])
        gmask = Tile(cx, [128, NT, 16], F32, "gmask")
        dma(cx, "sp", gmask.v, cin["c_gmask"].rearrange("p (a b) -> p a b", b=16), w=[gmask.b])
        BT = [Tile(cx, [16, S], BF16, f"BT{p}") for p in range(2)]
        kmf = Tile(cx, [128, 16], F32, "kmf")
        kmb = Tile(cx, [128, 16], BF16, "kmb")
        I(cx, "pool", "memset", kmf.v, 0.0, w=[kmf.b])
        g1 = Tile(cx, [128, NT, 16], F32, "g1")
        g2 = Tile(cx, [128, NT, 16], F32, "g2")
        eq = Tile(cx, [128, NT, 16], F32, "eq")
        mx = Tile(cx, [128, NT], F32, "mx")
        bb = Tile(cx, [128, NT, 16], BF16, "bb")
    sstep = 0

    def load_head(h):
        p = h % 2
        for i, (q_d, k_d, kd) in enumerate(terms):
            dma(cx, "sp", QT[p][i].v, q_d[h * kd:(h + 1) * kd, :], w=[QT[p][i].b])
            if mla_shared_k and i == 1:
                if h < 2:
                    dma(cx, "sp", KT[p][i].v, k_d[0:kd, :], w=[KT[p][i].b])
            else:
                dma(cx, "sp", KT[p][i].v, k_d[h * kd:(h + 1) * kd, :], w=[KT[p][i].b])
        dma(cx, "sp", V[p].v[:, :, 0:dv], v_d[:, h * dv:(h + 1) * dv].rearrange("(t p) d -> p t d", p=128),
            w=[V[p].b])

    def bias_head(h):
        p = h % 2
        if moba:
            kt0 = KT[p][0]; qt0 = QT[p][0]; bt = BT[p]
            NB = c.NBLK
            I(cx, "dve", "tensor_reduce", kmf.v[:, 0:NB], kt0.v.rearrange("p (n k) -> p n k", k=c.MBLK), AX.X, ALU.add,
              r=[kt0.b], w=[kmf.b])
            I(cx, "dve", "tensor_copy", kmb.v, kmf.v, r=[kmf.b], w=[kmb.b])
            gb_ = 7
            gp = cx.psum[gb_][:, 0:NT * 16]
            for t in range(NT):
                I(cx, "pe", "matmul", gp[:, t * 16:(t + 1) * 16], qt0.v[:, t * 128:(t + 1) * 128], kmb.v,
                  start=True, stop=True, r=[qt0.b, kmb.b], w=[cx.psb[gb_]])
            gp3 = gp.rearrange("p (a b) -> p a b", b=16)
            I(cx, "dve", "tensor_tensor", g1.v, gp3, gmask.v, ALU.add, r=[cx.psb[gb_], gmask.b], w=[g1.b])
            cur = g1
            for it in range(3):
                I(cx, "dve", "tensor_reduce", mx.v, cur.v, AX.X, ALU.max, r=[cur.b], w=[mx.b])
                if it < 2:
                    mb = mx.v.unsqueeze(2).broadcast_to([128, NT, 16])
                    I(cx, "dve", "tensor_tensor", eq.v, cur.v, mb, ALU.is_equal, r=[cur.b, mx.b], w=[eq.b])
                    I(cx, "dve", "scalar_tensor_tensor", g2.v, eq.v, -1e30, cur.v, ALU.mult, ALU.add,
                      r=[eq.b, cur.b], w=[g2.b])
                    cur = g2
            mb = mx.v.unsqueeze(2).broadcast_to([128, NT, 16])
            I(cx, "dve", "tensor_tensor", eq.v, g1.v, mb, ALU.is_ge, r=[g1.b, mx.b], w=[eq.b])
            I(cx, "dve", "tensor_tensor", eq.v, eq.v, past.v, ALU.mult, r=[eq.b, past.b], w=[eq.b])
            I(cx, "dve", "tensor_tensor", eq.v, eq.v, past.v, ALU.subtract, r=[eq.b, past.b], w=[eq.b])
            I(cx, "dve", "tensor_scalar", bb.v, eq.v, 30000.0, None, ALU.mult, r=[eq.b], w=[bb.b])
            tb = 7
            pt = psum_bf16(cx, tb)
            for t0 in range(0, NT, 8):
                n8 = min(8, NT - t0)
                for j in range(n8):
                    I(cx, "pe", "transpose", pt[0:16, j * 128:(j + 1) * 128], bb.v[:, t0 + j, :], K.ident.v,
                      r=[bb.b, K.ident.b], w=[cx.psb[tb]])
                I(cx, "act", "copy", bt.v[:, t0 * 128:(t0 + n8) * 128], pt[0:16, 0:n8 * 128], r=[cx.psb[tb]], w=[bt.b])

    load_head(0)
    if moba:
        bias_head(0)
    for h in range(H):
        p = h % 2
        if h + 1 < H:
            load_head(h + 1)
        for G in range(NG):
            if G == NG // 2 and h + 1 < H and moba:
                bias_head(h + 1)
            accb = (2, 3, 4, 5)
            def acc(i):
                return cx.psum[accb[i]][:, 0:dv + 1]
            nk = 4 * G + 4

            def emit_S(kt, G=G, p=p):
                nonlocal sstep
                j = kt - 4 * G
                jq = max(j, 0)
                q0 = jq * 128
                N = 512 - q0
                sb_ = sstep % 2
                sstep += 1
                ps = cx.psum[sb_][:, 0:N]
                ops = []
                for i in range(nterm):
                    ops.append((KT[p][i].v[:, kt * 128:(kt + 1) * 128], QT[p][i].v[:, G * 512 + q0:(G + 1) * 512],
                                [KT[p][i].b, QT[p][i].b]))
                if moba:
                    ops.append((eblk.v[:, kt * 128:(kt + 1) * 128], BT[p].v[:, G * 512 + q0:(G + 1) * 512],
                                [eblk.b, BT[p].b]))
                if j >= 0:
                    ops.append((K.ident.v, K.cmask.v[:, j * 512 + q0:(j + 1) * 512], [K.ident.b, K.cmask.b]))
                for oi, (l, r_, bufs) in enumerate(ops):
                    I(cx, "pe", "matmul", ps, l, r_, start=(oi == 0), stop=(oi == len(ops) - 1),
                      r=bufs, w=[cx.psb[sb_]])
                return (ps, sb_, jq, q0, N)

            nxt = emit_S(0)
            for kt in range(nk):
                (ps, sb_, jq, q0, N) = nxt
                if kt + 1 < nk:
                    nxt = emit_S(kt + 1)
                ptile = PT.next()
                I(cx, "act", "activation", out=ptile.v[:, 0:N], in_=ps, func=AF.Exp, scale=scale,
                  r=[cx.psb[sb_]], w=[ptile.b])
                for i in range(jq, 4):
                    I(cx, "pe", "matmul", acc(i), ptile.v[:, i * 128 - q0:i * 128 - q0 + 128], V[p].v[:, kt, :],
                      start=(kt == 0), stop=(kt == 4 * G + i), r=[ptile.b, V[p].b], w=[cx.psb[accb[i]]])
            ob = osb.next()
            for i in range(4):
                r1 = rc.next()
                a = acc(i)
                I(cx, "dve", "reciprocal", r1.v, a[:, dv:dv + 1], r=[cx.psb[accb[i]]], w=[r1.b])
                I(cx, "dve", "tensor_scalar", ob.v[:, i, :], a[:, 0:dv], r1.v[:, 0:1], None, ALU.mult,
                  r=[cx.psb[accb[i]], r1.b], w=[ob.b])
            tb = 6
            pt = psum_bf16(cx, tb)
            for i in range(4):
                I(cx, "pe", "transpose", pt[0:dv, i * 128:(i + 1) * 128], ob.v[:, i, :], K.ident.v,
                  r=[ob.b, K.ident.b], w=[cx.psb[tb]])
            ot = oT.next()
            I(cx, "act", "copy", ot.v[0:dv, :], pt[0:dv, 0:512], r=[cx.psb[tb]], w=[ot.b])
            dma(cx, "pool", oT_d[h * dv:(h + 1) * dv, G * 512:(G + 1) * 512], ot.v[0:dv, :], r=[ot.b])
    cx.s.barrier()
    cx.arena.release()


def phase_swa(cx, c, K, qT_d, kT_d, v_d, sinks_d, oT_d, cin):
    S, NT = c.S, c.NT
    d = c.BD
    r = c.BH // c.BKV
    scale = d ** -0.5
    cx.arena.mark()
    es = Tile(cx, [128, c.BH], F32, "esink")
    dma(cx, "sp", es.v, sinks_d.partition_broadcast(128), w=[es.b])
    I(cx, "act", "activation", out=es.v, in_=es.v, func=AF.Exp, r=[es.b], w=[es.b])
    sm = Tile(cx, [128, 1024], BF16, "smask2")
    dma(cx, "sp", sm.v, cin["c_smask2"], w=[sm.b])
    KTt = [Tile(cx, [64, S], BF16, f"sK{p}") for p in range(2)]
    Vt = [Tile(cx, [128, NT, d + 1], BF16, f"sV{p}") for p in range(2)]
    for p in range(2):
        I(cx, "pool", "memset", Vt[p].v[:, :, d:d + 1], 1.0, w=[Vt[p].b])
    Q2 = [Tile(cx, [64, 2, S], BF16, f"sQ{p}") for p in range(2)]
    PT = Rot(cx, 3, [128, 512], BF16, "sPT")
    ob2 = Rot(cx, 2, [128, 128], BF16, "sob")
    oTt = Rot(cx, 2, [128, 512], BF16, "soT")
    den = Rot(cx, 4, [128, 1], F32, "sden")
    step = 0
    npair = 0
    for g in range(c.BKV):
        kp = g % 2
        dma(cx, "sp", KTt[kp].v, kT_d[g * 64:(g + 1) * 64, :], w=[KTt[kp].b])
        dma(cx, "sp", Vt[kp].v[:, :, 0:d], v_d[:, g * 64:(g + 1) * 64].rearrange("(t p) d -> p t d", p=128),
            w=[Vt[kp].b])
        for hp in range(r // 2):
            h0 = g * r + 2 * hp
            q2 = Q2[npair % 2]
            npair += 1
            dma(cx, "sp", q2.v, qT_d[h0 * 64:(h0 + 2) * 64, :].rearrange("(h d) s -> d h s", d=64), w=[q2.b])
            ot = None

            def emit_S(t, q2=q2, kp=kp):
                nonlocal step
                sb_ = step % 2
                step += 1
                ps = cx.psum[sb_][:, 0:512]
                mv = sm.v[:, 0:512] if t > 0 else sm.v[:, 512:1024]
                I(cx, "pe", "matmul", ps, K.ident.v, mv, start=True, stop=False, r=[K.ident.b, sm.b], w=[cx.psb[sb_]])
                mms = []
                for hh in range(2):
                    for kk in range(2):
                        kt = t - 1 + kk
                        if kt < 0:
                            continue
                        mms.append((hh, kk, kt))
                for mi, (hh, kk, kt) in enumerate(mms):
                    col = (hh * 2 + kk) * 128
                    I(cx, "pe", "matmul", ps[:, col:col + 128], KTt[kp].v[:, kt * 128:(kt + 1) * 128],
                      q2.v[:, hh, t * 128:(t + 1) * 128], start=False, stop=(mi == len(mms) - 1),
                      r=[KTt[kp].b, q2.b], w=[cx.psb[sb_]])
                return ps, sb_

            nxt = emit_S(0)
            for t in range(NT):
                ps, sb_ = nxt
                if t + 1 < NT:
                    nxt = emit_S(t + 1)
                pt_ = PT.next()
                I(cx, "act", "activation", out=pt_.v, in_=ps, func=AF.Exp, scale=scale, r=[cx.psb[sb_]], w=[pt_.b])
                ab = 2 + (t % 2)
                o2 = ob2.next()
                for hh in range(2):
                    a = cx.psum[ab][:, hh * 128:hh * 128 + d + 1]
                    kks = [kk for kk in range(2) if t - 1 + kk >= 0]
                    for ki, kk in enumerate(kks):
                        col = (hh * 2 + kk) * 128
                        I(cx, "pe", "matmul", a, pt_.v[:, col:col + 128], Vt[kp].v[:, t - 1 + kk, :],
                          start=(ki == 0), stop=(ki == len(kks) - 1), r=[pt_.b, Vt[kp].b], w=[cx.psb[ab]])
                    dn = den.next()
                    I(cx, "dve", "tensor_tensor", dn.v, a[:, d:d + 1], es.v[:, h0 + hh:h0 + hh + 1], ALU.add,
                      r=[cx.psb[ab], es.b], w=[dn.b])
                    I(cx, "dve", "reciprocal", dn.v, dn.v, r=[dn.b], w=[dn.b])
                    I(cx, "dve", "tensor_scalar", o2.v[:, hh * 64:(hh + 1) * 64], a[:, 0:d], dn.v[:, 0:1], None, ALU.mult,
                      r=[cx.psb[ab], dn.b], w=[o2.b])
                tb = 4
                ptb = psum_bf16(cx, tb)
                if t % 4 == 0:
                    ot = oTt.next()
                I(cx, "pe", "transpose", ptb[:, 0:128], o2.v, K.ident.v, r=[o2.b, K.ident.b], w=[cx.psb[tb]])
                I(cx, "act", "copy", ot.v[:, (t % 4) * 128:(t % 4 + 1) * 128], ptb[:, 0:128], r=[cx.psb[tb]], w=[ot.b])
                if t % 4 == 3 or t == NT - 1:
                    n = (t % 4 + 1) * 128
                    t0 = t - t % 4
                    dma(cx, "pool", oT_d[h0 * 64:(h0 + 2) * 64, t0 * 128:t0 * 128 + n], ot.v[:, 0:n], r=[ot.b])
    cx.s.barrier()
    cx.arena.release()


def phase_out_gate(cx, c, K, oT_list, W_list, gT_d, yT_d):
    S, D = c.S, c.D
    SB = 512 if S >= 512 else S
    PW = 256
    cx.arena.mark()
    KCs = [o.shape[0] // 128 for o in oT_list]
    xb = [Tile(cx, [128, KCs[i], SB], BF16, f"ox{i}") for i in range(3)]
    wp = [[Tile(cx, [128, KCs[i], PW], BF16, f"ow{i}_{p}") for p in range(2)] for i in range(3)]
    stg = Rot(cx, 4, [128, 8, PW], F32, "ostg")
    gt = [Rot(cx, 2, [128, SB], BF16, f"og{i}") for i in range(3)]
    tt = [Rot(cx, 2, [128, SB], F32, f"ot{i}") for i in range(3)]
    yo = Rot(cx, 2, [128, SB], BF16, "oy")
    nb = 0
    items = [(s0, c0) for s0 in range(0, S, SB) for c0 in range(0, D, PW)]
    ncast = [0]

    def emit_load(idx):
        s0, c0 = items[idx]
        pp = idx % 2
        for i in range(3):
            w = wp[i][pp]
            KQ = min(8, KCs[i])
            for q in range(KCs[i] // KQ):
                st = stg.next()
                src = W_list[i][q * KQ * 128:(q + 1) * KQ * 128, c0:c0 + PW].rearrange("(a p) n -> p a n", p=128)
                dma(cx, "sp", st.v[:, 0:KQ, :], src, w=[st.b])
                I(cx, "act", "copy", w.v[:, q * KQ:(q + 1) * KQ, :], st.v[:, 0:KQ, :], r=[st.b], w=[w.b])

    emit_load(0)
    cur_s0 = None
    for idx, (s0, c0) in enumerate(items):
        pp = idx % 2
        if s0 != cur_s0:
            cur_s0 = s0
            for i in range(3):
                dma(cx, "sp", xb[i].v, oT_list[i][:, s0:s0 + SB].rearrange("(a p) s -> p a s", p=128), w=[xb[i].b])
        if idx + 1 < len(items):
            emit_load(idx + 1)
        for lo in range(0, PW, 128):
            col = c0 + lo
            tts = []
            for i in range(3):
                bank = (nb % 2) * 3 + i
                ps = cx.psum[bank][:, 0:SB]
                w = wp[i][pp]
                for kc in range(KCs[i]):
                    I(cx, "pe", "matmul", ps, w.v[:, kc, lo:lo + 128], xb[i].v[:, kc, :],
                      start=(kc == 0), stop=(kc == KCs[i] - 1), r=[w.b, xb[i].b], w=[cx.psb[bank]])
                g = gt[i].next()
                dma(cx, "sp", g.v, gT_d[i * D + col:i * D + col + 128, s0:s0 + SB], w=[g.b])
                t = tt[i].next()
                I(cx, "dve", "tensor_tensor", t.v, ps, g.v, ALU.mult, r=[cx.psb[bank], g.b], w=[t.b])
                tts.append(t)
            nb += 1
            I(cx, "pool", "tensor_tensor", tts[0].v, tts[0].v, tts[1].v, ALU.add, r=[tts[0].b, tts[1].b], w=[tts[0].b])
            y = yo.next()
            I(cx, "pool", "tensor_tensor", y.v, tts[0].v, tts[2].v, ALU.add, r=[tts[0].b, tts[2].b], w=[y.b])
            dma(cx, "pool", yT_d[col:col + 128, s0:s0 + SB], y.v, r=[y.b])
    cx.s.barrier()
    cx.arena.release()


def phase_resid_proj(cx, c, K, xT_d, Kdim, W_d, h_d):
    cx.arena.mark()
    ht = Rot(cx, 3, [128, 256], F32, "rh")
    tiles = [(256 * j, 256, 256 * j) for j in range(c.D // 256)]

    def epi(cx, tag, s0, ns, ps, psb):
        t = ht.next()
        dma(cx, "sp", t.v, h_d[s0:s0 + 128, tag:tag + 256], w=[t.b])
        I(cx, "dve", "tensor_tensor", t.v, ps, t.v, ALU.add, r=[psb, t.b], w=[t.b])
        dma(cx, "pool", h_d[s0:s0 + 128, tag:tag + 256], t.v, r=[t.b])
    phase_proj(cx, c, xT_d, Kdim, W_d, tiles, "N", epi)
    cx.arena.release()


def phase_moe(cx, c, K, T, cin, h_d, g_d, wg_d, bg_d, we_d, be_d, wgate_d, wup_d, wdown_d):
    S, D, NT, NE = c.S, c.D, c.NT, c.NE
    NC = D // 128
    NGRP = max(1, S // c.MG)
    TPG = NT // NGRP
    NSL = NGRP * c.CAP
    FC = c.FF // 128
    hn2_d = T["hn2"]
    cx.arena.mark()
    A_f = Tile(cx, [128, NT, NE], F32, "A_f")
    A_b = Tile(cx, [128, NT, NE], BF16, "A_b")
    Wt = Tile(cx, [128, NT, NE], F32, "Wt")
    pos = Tile(cx, [128, NT, NE], F32, "pos")
    iota = Tile(cx, [128, 128], F32, "iota")
    dma(cx, "sp", iota.v, cin["c_iota"], w=[iota.b])
    tri = Tile(cx, [128, 128], BF16, "tri")
    dma(cx, "sp", tri.v, cin["c_tri"], w=[tri.b])
    cx.arena.mark()
    gb = Tile(cx, [128, D], F32, "m_gbc")
    dma(cx, "sp", gb.v, g_d.partition_broadcast(128), w=[gb.b])
    wr = Tile(cx, [128, NC, 40], F32, "wr")
    dma(cx, "sp", wr.v[:, :, 0:8], wg_d.rearrange("(a p) n -> p a n", p=128), w=[wr.b])
    dma(cx, "sp", wr.v[:, :, 8:40], we_d.rearrange("(a p) n -> p a n", p=128), w=[wr.b])
    bias = Tile(cx, [128, 40], F32, "rbias")
    dma(cx, "sp", bias.v[:, 0:8], bg_d.partition_broadcast(128), w=[bias.b])
    dma(cx, "sp", bias.v[:, 8:40], be_d.partition_broadcast(128), w=[bias.b])
    xt = Rot(cx, 2, [128, D], F32, "m_x")
    xn = Rot(cx, 1, [128, D], F32, "m_xn")
    xnb = Rot(cx, 2, [128, D], BF16, "m_xnb")
    junk = Tile(cx, [128, D], BF16, "m_junk")
    xT = Rot(cx, 1, [128, NC, 128], F32, "m_xT")
    sc = Rot(cx, 2, [128, 16], F32, "m_sc")
    lg = Rot(cx, 2, [128, 40], F32, "m_lg")
    tmp = Rot(cx, 2, [128, 8, 4], F32, "m_tmp")
    small = Rot(cx, 2, [128, 32], F32, "m_small")
    nb = 0
    for t in range(NT):
        x = xt.next(); n = xn.next(); nbf = xnb.next(); s = sc.next(); l = lg.next(); tp = tmp.next(); sm = small.next()
        xTt = xT.next()
        dma(cx, "sp", x.v, h_d[t * 128:(t + 1) * 128, :], w=[x.b])
        I(cx, "act", "activation", out=junk.v, in_=x.v, func=AF.Square, accum_out=s.v[:, 0:1], r=[x.b], w=[junk.b, s.b])
        I(cx, "dve", "tensor_scalar", s.v[:, 1:2], s.v[:, 0:1], 1.0 / D, c.EPS, ALU.mult, ALU.add, r=[s.b], w=[s.b])
        I(cx, "act", "activation", out=s.v[:, 1:2], in_=s.v[:, 1:2], func=AF.Sqrt, r=[s.b], w=[s.b])
        I(cx, "dve", "reciprocal", s.v[:, 1:2], s.v[:, 1:2], r=[s.b], w=[s.b])
        I(cx, "dve", "scalar_tensor_tensor", n.v, x.v, s.v[:, 1:2], gb.v, ALU.mult, ALU.mult, r=[x.b, s.b, gb.b], w=[n.b])
        I(cx, "pool", "tensor_copy", nbf.v, n.v, r=[n.b], w=[nbf.b])
        dma(cx, "act", hn2_d[t * 128:(t + 1) * 128, :], nbf.v, r=[nbf.b])
        for g4 in range(0, NC, 4):
            bank = nb % 2
            nb += 1
            pt = cx.psum[bank]
            for j in range(4):
                cc = g4 + j
                I(cx, "pe", "transpose", pt[:, j * 128:(j + 1) * 128], n.v[:, cc * 128:(cc + 1) * 128], iota_ident(cx, K),
                  r=[n.b, K.identf.b], w=[cx.psb[bank]])
            src = pt[:, 0:512].rearrange("p (a b) -> p a b", b=128)
            if nb % 2:
                I(cx, "act", "copy", xTt.v[:, g4:g4 + 4, :], src, r=[cx.psb[bank]], w=[xTt.b])
            else:
                I(cx, "dve", "tensor_copy", xTt.v[:, g4:g4 + 4, :], src, r=[cx.psb[bank]], w=[xTt.b])
        lb = 2
        lp = cx.psum[lb][:, 0:40]
        for kc in range(NC):
            I(cx, "pe", "matmul", lp, xTt.v[:, kc, :], wr.v[:, kc, :], start=(kc == 0), stop=(kc == NC - 1),
              r=[xTt.b, wr.b], w=[cx.psb[lb]])
        I(cx, "dve", "tensor_tensor", l.v, lp, bias.v, ALU.add, r=[cx.psb[lb], bias.b], w=[l.b])
        lgv = l.v[:, 0:8]
        le3 = l.v[:, 8:40].rearrange("p (g e) -> p g e", e=4)
        m = s.v[:, 2:3]; negm = s.v[:, 3:4]; se = s.v[:, 4:5]; m1 = s.v[:, 5:6]; nm1 = s.v[:, 6:7]; m2 = s.v[:, 7:8]
        rr = s.v[:, 8:9]; w1 = s.v[:, 9:10]; w2 = s.v[:, 10:11]
        G1 = sm.v[:, 0:8]; esel = sm.v[:, 8:12]; e1 = sm.v[:, 12:16]; es2 = sm.v[:, 16:20]; e2 = sm.v[:, 20:24]
        asel = sm.v[:, 24:28]; wsel = sm.v[:, 28:32]
        R_ = [l.b, s.b, sm.b, tp.b]
        def D_(meth, *a, **k):
            I(cx, "dve", meth, *a, r=R_, w=R_, **k)
        D_("tensor_reduce", m, lgv, AX.X, ALU.max)
        D_("tensor_scalar", G1, lgv, m, None, ALU.is_equal)
        D_("tensor_scalar", negm, m, -1.0, None, ALU.mult)
        I(cx, "act", "activation", out=sm.v[:, 12:20], in_=lgv, func=AF.Exp, bias=negm, accum_out=se, r=R_, w=R_)
        D_("tensor_tensor", tp.v, le3, G1.unsqueeze(2).broadcast_to([128, 8, 4]), ALU.mult)
        D_("tensor_reduce", esel, tp.v.rearrange("p g e -> p e g"), AX.X, ALU.add)
        D_("tensor_reduce", m1, esel, AX.X, ALU.max)
        D_("tensor_scalar", e1, esel, m1, None, ALU.is_equal)
        D_("scalar_tensor_tensor", es2, e1, -1e30, esel, ALU.mult, ALU.add)
        D_("tensor_reduce", m2, es2, AX.X, ALU.max)
        D_("tensor_scalar", e2, es2, m2, None, ALU.is_equal)
        D_("tensor_scalar", nm1, m1, -1.0, None, ALU.mult)
        I(cx, "act", "activation", out=rr, in_=m2, func=AF.Exp, bias=nm1, r=R_, w=R_)
        D_("tensor_scalar", w1, rr, 1.0, None, ALU.add)
        D_("tensor_tensor", w1, w1, se, ALU.mult)
        D_("reciprocal", w1, w1)
        D_("tensor_tensor", w2, w1, rr, ALU.mult)
        D_("tensor_tensor", asel, e1, e2, ALU.add)
        D_("tensor_scalar", wsel, e1, w1, None, ALU.mult)
        D_("scalar_tensor_tensor", wsel, e2, w2, wsel, ALU.mult, ALU.add)
        A3 = A_f.v[:, t, :].rearrange("p (g e) -> p g e", e=4)
        W3 = Wt.v[:, t, :].rearrange("p (g e) -> p g e", e=4)
        g1b = G1.unsqueeze(2).broadcast_to([128, 8, 4])
        I(cx, "dve", "tensor_tensor", A3, g1b, asel.unsqueeze(1).broadcast_to([128, 8, 4]), ALU.mult, r=R_, w=[A_f.b])
        I(cx, "dve", "tensor_tensor", W3, g1b, wsel.unsqueeze(1).broadcast_to([128, 8, 4]), ALU.mult, r=R_, w=[Wt.b])
    I(cx, "dve", "tensor_copy", A_b.v, A_f.v, r=[A_f.b], w=[A_b.b])
    for t in range(NT):
        g0 = (t // TPG) * TPG
        bank = 3 + t % 2
        pp = cx.psum[bank][:, 0:NE]
        for j in range(g0, t + 1):
            l_ = K.ones.v if j < t else tri.v
            I(cx, "pe", "matmul", pp, l_, A_b.v[:, j, :], start=(j == g0), stop=(j == t),
              r=[K.ones.b, tri.b, A_b.b], w=[cx.psb[bank]])
        I(cx, "act", "copy", pos.v[:, t, :], pp, r=[cx.psb[bank]], w=[pos.b])
    cx.s.barrier()
    cx.arena.release()
    cx.arena.mark()
    hg = Tile(cx, [128, TPG, D], BF16, "hg")
    sel = Rot(cx, 2, [128, TPG, 128], BF16, "sel")
    xg = Rot(cx, 2, [128, NC, 128], BF16, "xg")
    nb = 0
    for gi in range(NGRP):
        dma(cx, "sp", hg.v, hn2_d[gi * c.MG:gi * c.MG + TPG * 128, :].rearrange("(a p) d -> p a d", p=128), w=[hg.b])
        for e in range(NE):
            sl = sel.next()
            for i in range(TPG):
                t = gi * TPG + i
                I(cx, "dve" if i % 2 else "pool", "tensor_scalar", sl.v[:, i, :], iota.v, pos.v[:, t, e:e + 1],
                  A_f.v[:, t, e:e + 1], ALU.is_equal, ALU.mult, r=[iota.b, pos.b, A_f.b], w=[sl.b])
            x = xg.next()
            for c4 in range(0, NC, 4):
                bank = nb % 3
                nb += 1
                for j in range(4):
                    cc = c4 + j
                    for i in range(TPG):
                        I(cx, "pe", "matmul", cx.psum[bank][:, j * 128:(j + 1) * 128], hg.v[:, i, cc * 128:(cc + 1) * 128],
                          sl.v[:, i, :], start=(i == 0), stop=(i == TPG - 1), r=[hg.b, sl.b], w=[cx.psb[bank]])
                src = cx.psum[bank][:, 0:512].rearrange("p (a b) -> p a b", b=128)
                if nb % 2:
                    I(cx, "act", "copy", x.v[:, c4:c4 + 4, :], src, r=[cx.psb[bank]], w=[x.b])
                else:
                    I(cx, "dve", "tensor_copy", x.v[:, c4:c4 + 4, :], src, r=[cx.psb[bank]], w=[x.b])
            dma(cx, "pool", T["xg"][e * D:(e + 1) * D, gi * 128:(gi + 1) * 128].rearrange("(a p) s -> p a s", p=128),
                x.v, r=[x.b])
    cx.s.barrier()
    cx.arena.release()
    cs = Cfg.__new__(Cfg)
    cs.__dict__.update(c.__dict__)
    cs.S = NSL; cs.SB = NSL
    cx.arena.mark()
    gs = Tile(cx, [128, FC, NSL], BF16, "gs")
    ao = Rot(cx, 2, [128, NSL], BF16, "ao")
    yo = Rot(cx, 3, [128, 256], BF16, "yo")
    for e in range(NE):
        xTe = T["xg"][e * D:(e + 1) * D, :]
        tiles = [(128 * f, 128, f) for f in range(FC)]

        def epi_g(cx, tag, s0, ns, ps, psb):
            I(cx, "act", "activation", out=gs.v[:, tag, s0:s0 + ns], in_=ps, func=AF.Silu, r=[psb], w=[gs.b])
        phase_proj(cx, cs, xTe, D, wgate_d[e], tiles, "T", epi_g)

        def epi_u(cx, tag, s0, ns, ps, psb):
            a = ao.next()
            I(cx, "dve", "tensor_tensor", a.v[:, 0:ns], ps, gs.v[:, tag, s0:s0 + ns], ALU.mult, r=[psb, gs.b], w=[a.b])
            dma(cx, "pool", T["aT"][tag * 128:(tag + 1) * 128, s0:s0 + ns], a.v[:, 0:ns], r=[a.b])
        phase_proj(cx, cs, xTe, D, wup_d[e], tiles, "T", epi_u)
        tiles_d = [(256 * j, 256, 256 * j) for j in range(D // 256)]

        def epi_d(cx, tag, s0, ns, ps, psb, e=e):
            y = yo.next()
            I(cx, "act" if (s0 // 128) % 2 else "dve", "copy" if (s0 // 128) % 2 else "tensor_copy", y.v, ps, r=[psb], w=[y.b])
            dma(cx, "pool", T["yexp"][e * NSL + s0:e * NSL + s0 + 128, tag:tag + 256], y.v, r=[y.b])
        phase_proj(cx, cs, T["aT"], c.FF, wdown_d[e], tiles_d, "N", epi_d)
    cx.arena.release()
    cx.arena.mark()
    swT = Tile(cx, [128, NE, TPG * 128], BF16, "swT")
    selw = Rot(cx, 3, [128, 128], BF16, "selw")
    yb = Rot(cx, 2, [128, NE, 512], BF16, "yb")
    ht = Rot(cx, 3, [128, 512], F32, "mh")
    nb = 0
    CW5 = 512 if D >= 512 else D
    for gi in range(NGRP):
        for e in range(NE):
            for i in range(TPG):
                t = gi * TPG + i
                s_ = selw.next()
                I(cx, "dve" if i % 2 else "pool", "tensor_scalar", s_.v, iota.v, pos.v[:, t, e:e + 1], Wt.v[:, t, e:e + 1],
                  ALU.is_equal, ALU.mult, r=[iota.b, pos.b, Wt.b], w=[s_.b])
                bank = 6 + nb % 2
                nb += 1
                pt = psum_bf16(cx, bank)
                I(cx, "pe", "transpose", pt[:, 0:128], s_.v, K.ident.v, r=[s_.b, K.ident.b], w=[cx.psb[bank]])
                I(cx, "act", "copy", swT.v[:, e, i * 128:(i + 1) * 128], pt[:, 0:128], r=[cx.psb[bank]], w=[swT.b])
        chunks = list(range(0, D, CW5))

        def load_y(ci, gi=gi):
            y = yb.next()
            c0 = chunks[ci]
            src = T["yexp"].rearrange("(e s) d -> s e d", e=NE)[gi * 128:(gi + 1) * 128, :, c0:c0 + CW5]
            dma(cx, "sp", y.v[:, :, 0:CW5], src, w=[y.b])
            return y
        ynext = load_y(0)
        for ci, c0 in enumerate(chunks):
            y = ynext
            if ci + 1 < len(chunks):
                ynext = load_y(ci + 1)
            for i in range(TPG):
                t = gi * TPG + i
                bank = nb % 3
                nb += 1
                ps = cx.psum[bank][:, 0:CW5]
                for e in range(NE):
                    I(cx, "pe", "matmul", ps, swT.v[:, e, i * 128:(i + 1) * 128], y.v[:, e, 0:CW5],
                      start=(e == 0), stop=(e == NE - 1), r=[swT.b, y.b], w=[cx.psb[bank]])
                h = ht.next()
                dma(cx, "sp", h.v[:, 0:CW5], h_d[t * 128:(t + 1) * 128, c0:c0 + CW5], w=[h.b])
                I(cx, "dve", "tensor_tensor", h.v[:, 0:CW5], ps, h.v[:, 0:CW5], ALU.add, r=[cx.psb[bank], h.b], w=[h.b])
                dma(cx, "pool", h_d[t * 128:(t + 1) * 128, c0:c0 + CW5], h.v[:, 0:CW5], r=[h.b])
    cx.s.barrier()
    cx.arena.release()
    cx.arena.release()


def iota_ident(cx, K):
    return K.identf.v


def phase_final_norm(cx, c, K, h_d, g_d, out_d):
    D, S = c.D, c.S
    cx.arena.mark()
    gb = Tile(cx, [128, D], F32, "f_gbc")
    dma(cx, "sp", gb.v, g_d.partition_broadcast(128), w=[gb.b])
    xt = Rot(cx, 2, [128, D], F32, "f_x")
    xo = Rot(cx, 2, [128, D], F32, "f_o")
    junk = Tile(cx, [128, D], BF16, "f_junk")
    sc = Rot(cx, 2, [128, 2], F32, "f_sc")
    for t in range(S // 128):
        x = xt.next(); o = xo.next(); s = sc.next()
        dma(cx, "sp", x.v, h_d[t * 128:(t + 1) * 128, :], w=[x.b])
        I(cx, "act", "activation", out=junk.v, in_=x.v, func=AF.Square, accum_out=s.v[:, 0:1], r=[x.b], w=[junk.b, s.b])
        I(cx, "dve", "tensor_scalar", s.v[:, 1:2], s.v[:, 0:1], 1.0 / D, c.EPS, ALU.mult, ALU.add, r=[s.b], w=[s.b])
        I(cx, "act", "activation", out=s.v[:, 1:2], in_=s.v[:, 1:2], func=AF.Sqrt, r=[s.b], w=[s.b])
        I(cx, "dve", "reciprocal", s.v[:, 1:2], s.v[:, 1:2], r=[s.b], w=[s.b])
        I(cx, "dve", "scalar_tensor_tensor", o.v, x.v, s.v[:, 1:2], gb.v, ALU.mult, ALU.mult, r=[x.b, s.b, gb.b], w=[o.b])
        dma(cx, "act", out_d[t * 128:(t + 1) * 128, :], o.v, r=[o.b], out=True)
    cx.s.barrier()
    cx.arena.release()


WEIGHT_SHAPES = lambda c: {
    "attn_norm_g": [c.DEPTH, c.D], "w_in": [c.DEPTH, c.D, c.NIN], "q_norm_g": [c.DEPTH, c.CQ],
    "kv_norm_g": [c.DEPTH, c.CKV], "wq_b": [c.DEPTH, c.CQ, c.CH * (c.CN + c.CR)],
    "wkv_b": [c.DEPTH, c.CKV, c.CH * (c.CN + c.CV)], "sinks": [c.DEPTH, c.BH],
    "w_out_a": [c.DEPTH, c.AW, c.D], "w_out_b": [c.DEPTH, c.BW, c.D], "w_out_c": [c.DEPTH, c.CW, c.D],
    "w_o": [c.DEPTH, c.D, c.D], "ffn_norm_g": [c.DEPTH, c.D], "w_group": [c.DEPTH, c.D, c.NG],
    "b_group": [c.DEPTH, c.NG], "w_expert": [c.DEPTH, c.D, c.NE], "b_expert": [c.DEPTH, c.NE],
    "w_gate": [c.DEPTH, c.NE, c.D, c.FF], "w_up": [c.DEPTH, c.NE, c.D, c.FF],
    "w_down": [c.DEPTH, c.NE, c.FF, c.D], "final_norm_g": [c.D],
}


def build_forward(c, NB, debug=(), stop_after=None):
    cnp = const_inputs(c)

    def body(cx):
        nc = cx.nc
        cx.debug = set(debug)
        S, D = c.S, c.D
        x_d = nc.dram_tensor("x", [NB * S, D], F32, kind="ExternalInput").ap()
        pos_d = nc.dram_tensor("positions", [NB * S], I32, kind="ExternalInput").ap()
        Wd = {k: nc.dram_tensor(k, shp, F32, kind="ExternalInput").ap() for k, shp in WEIGHT_SHAPES(c).items()}
        cin = {k: nc.dram_tensor(k, list(v.shape), CONST_DT[k], kind="ExternalInput").ap() for k, v in cnp.items()}
        out_d = nc.dram_tensor("out", [NB * S, D], F32, kind="ExternalOutput").ap()
        K = load_consts(cx, c, cin)
        K.identf = Tile(cx, [128, 128], F32, "identf")
        dma(cx, "sp", K.identf.v, cin["c_identf"], w=[K.identf.b])
        T = {}
        NGRP = max(1, S // c.MG)
        NSL = NGRP * c.CAP
        spec = {"h": ([S, D], F32), "hnT": ([D, S], BF16), "qaT": ([c.AW, S], BF16), "kaT": ([c.AW, S], BF16),
                "va": ([S, c.AW], BF16), "qbT": ([c.BW, S], BF16), "kbT": ([c.BKW, S], BF16), "vb": ([S, c.BKW], BF16),
                "cqT": ([c.CQ, S], BF16), "ckvT": ([c.CKV, S], BF16), "kpeT": ([64, S], BF16),
                "gT": ([3 * D, S], BF16), "qnT": ([c.CH * 128, S], BF16), "qpeT": ([c.CH * 64, S], BF16),
                "knT": ([c.CH * 128, S], BF16), "vc": ([S, c.CH * 128], BF16),
                "oaT": ([c.AW, S], BF16), "obT": ([c.BW, S], BF16), "ocT": ([c.CW, S], BF16), "yT": ([D, S], BF16),
                "hn2": ([S, D], BF16), "xg": ([c.NE * D, NSL], BF16), "aT": ([c.FF, NSL], BF16),
                "yexp": ([c.NE * NSL, D], BF16),
                "cosA": ([128, S], F32), "sinA": ([128, S], F32), "cosB": ([128, S], F32), "sinB": ([128, S], F32)}
        for k, (shp, dt) in spec.items():
            T[k] = dram(cx, k, shp, dt)
        E = make_epilogues(cx, c, K, None)
        cx.s.barrier()
        for b in range(NB):
            xb_d = x_d[b * S:(b + 1) * S, :]
            cx.arena.mark()
            ct = Tile(cx, [128, S], F32, "ropec"); st = Tile(cx, [128, S], F32, "ropes")
            for which, (cn, sn) in enumerate((("cosA", "sinA"), ("cosB", "sinB"))):
                build_rope(cx, c, K, pos_d[b * S:(b + 1) * S], which, ct, st)
                dma(cx, "sp", T[cn], ct.v, r=[ct.b])
                dma(cx, "sp", T[sn], st.v, r=[st.b])
                cx.s.barrier()
            cx.arena.release()
            for t in range(S // 128):
                dma(cx, "sp" if t % 2 else "act", T["h"][t * 128:(t + 1) * 128, :], xb_d[t * 128:(t + 1) * 128, :])
            cx.s.barrier()
            R = T
            for l in range(c.DEPTH):
                phase_rmsnorm_T(cx, c, K, T["h"], Wd["attn_norm_g"][l], T["hnT"])
                if stop_after == "norm": break
                phase_A(cx, c, K, E, R, T, T["hnT"], Wd["w_in"][l])
                if stop_after == "A": break
                phase_mla_up(cx, c, K, E, R, T, Wd["q_norm_g"][l], Wd["kv_norm_g"][l], Wd["wq_b"][l], Wd["wkv_b"][l])
                if stop_after == "mla_up": break
                phase_attn_causal(cx, c, K, c.AH, [(T["qaT"], T["kaT"], 128)], T["va"], 128, c.AD ** -0.5, T["oaT"],
                                  moba=True, cin=cin)
                if stop_after == "moba": break
                phase_swa(cx, c, K, T["qbT"], T["kbT"], T["vb"], Wd["sinks"][l], T["obT"], cin)
                if stop_after == "swa": break
                phase_attn_causal(cx, c, K, c.CH, [(T["qnT"], T["knT"], 128), (T["qpeT"], T["kpeT"], 64)], T["vc"], 128,
                                  (c.CN + c.CR) ** -0.5, T["ocT"], mla_shared_k=True)
                if stop_after == "mla": break
                phase_out_gate(cx, c, K, [T["oaT"], T["obT"], T["ocT"]],
                               [Wd["w_out_a"][l], Wd["w_out_b"][l], Wd["w_out_c"][l]], T["gT"], T["yT"])
                phase_resid_proj(cx, c, K, T["yT"], D, Wd["w_o"][l], T["h"])
                if stop_after == "mix": break
                phase_moe(cx, c, K, T, cin, T["h"], Wd["ffn_norm_g"][l], Wd["w_group"][l], Wd["b_group"][l],
                          Wd["w_expert"][l], Wd["b_expert"][l], Wd["w_gate"][l], Wd["w_up"][l], Wd["w_down"][l])
                if stop_after == "moe": break
            phase_final_norm(cx, c, K, T["h"], Wd["final_norm_g"], out_d[b * S:(b + 1) * S, :])
    nc = build_program(body)
    return nc, cnp


_CACHE = {}


def kernel(**inputs):
    c = Cfg()
    if "nc" not in _CACHE:
        _CACHE["nc"] = build_forward(c, 1)
    nc, cnp = _CACHE["nc"]
    B = inputs["x"].shape[0]
    in_maps = []
    for b in range(B):
        m = {"x": np.ascontiguousarray(np.asarray(inputs["x"][b], dtype=np.float32)),
             "positions": np.ascontiguousarray(np.asarray(inputs["positions"][b]).astype(np.int32))}
        for k in WEIGHT_SHAPES(c):
            m[k] = np.asarray(inputs[k], dtype=np.float32)
        m.update(cnp)
        in_maps.append(m)
    res = run_bass_kernel_spmd(nc, in_maps, core_ids=list(range(B)))
    out = np.stack([np.asarray(res.results[b]["out"]) for b in range(B)], axis=0)
    return out.astype(np.float32)
```

```python
import numpy as np
import contextlib
import concourse.bass as bass
import concourse.mybir as mybir
from concourse.bass_utils import run_bass_kernel_spmd

F32 = mybir.dt.float32
BF16 = mybir.dt.bfloat16
I32 = mybir.dt.int32
ALU = mybir.AluOpType
AF = mybir.ActivationFunctionType
AX = mybir.AxisListType

ENGS = ("pe", "act", "dve", "pool", "sp")
NDSEM = {"sp": 24, "pool": 24, "act": 8}


class Op:
    __slots__ = ("eng", "fn", "deps", "dma", "sig", "sigval", "dslot", "dval", "out")

    def __init__(self, eng, fn, dma):
        self.eng = eng
        self.fn = fn
        self.dma = dma
        self.deps = []
        self.sig = False
        self.sigval = 0
        self.dslot = 0
        self.dval = 0
        self.out = False


class Buf:
    __slots__ = ("name", "w", "rs", "rd")

    def __init__(self, name=""):
        self.name = name
        self.w = None
        self.rs = {}
        self.rd = []


class Sched:
    def __init__(self):
        self.ops = {e: [] for e in ENGS}
        self.ndma = {e: 0 for e in ENGS}

    def add(self, eng, fn, r=(), w=(), dma=False, out=False):
        op = Op(eng, fn, dma)
        op.out = out
        deps = {}
        def adddep(d):
            if d is None or d is op:
                return
            if d.eng == "pe" and eng == "pe" and not d.dma and not dma:
                return
            deps[id(d)] = d
        for b in r:
            adddep(b.w)
        for b in w:
            adddep(b.w)
            for d in b.rs.values():
                adddep(d)
            for d in b.rd:
                adddep(d)
        op.deps = list(deps.values())
        for b in r:
            if dma:
                b.rd.append(op)
            else:
                b.rs[eng] = op
        for b in w:
            b.w = op
            b.rs = {}
            b.rd = []
        if dma:
            K = NDSEM[eng]
            i = self.ndma[eng]
            self.ndma[eng] = i + 1
            op.dslot = i % K
            op.dval = 16 * (i // K + 1)
        self.ops[eng].append(op)
        return op

    def barrier(self):
        lasts = []
        for e in ENGS:
            comp = [o for o in self.ops[e] if not o.dma and o.fn is not None]
            if comp:
                lasts.append(comp[-1])
            lasts.extend(o for o in self.ops[e][-64:] if o.dma)
        for e in ENGS:
            op = Op(e, None, False)
            op.deps = [d for d in lasts]
            self.ops[e].append(op)

    def emit(self, nc, block, csem, dsem):
        for e in ENGS:
            for op in self.ops[e]:
                for d in op.deps:
                    d.sig = True
        for e in ENGS:
            cnt = 0
            for op in self.ops[e]:
                if not op.dma and op.sig and op.fn is not None:
                    cnt += 1
                    op.sigval = cnt
        sched = self

        def run(e, engine):
            waited = {}
            def wait(key, sem, val):
                if waited.get(key, 0) >= val:
                    return
                engine.wait_ge(sem, val)
                waited[key] = val
            outs = []
            for op in sched.ops[e]:
                for d in op.deps:
                    if d.dma:
                        wait(("d", d.eng, d.dslot), dsem[d.eng][d.dslot], d.dval)
                    else:
                        wait(("c", d.eng), csem[d.eng], d.sigval)
                if op.fn is None:
                    continue
                if op.dma:
                    if op.dval > 16:
                        wait(("d", e, op.dslot), dsem[e][op.dslot], op.dval - 16)
                    ins = op.fn(engine)
                    ins.then_inc(dsem[e][op.dslot], 16)
                    if op.out:
                        outs.append(op)
                else:
                    ins = op.fn(engine)
                    if op.sig:
                        ins.then_inc(csem[e], 1)
            for op in outs:
                wait(("d", e, op.dslot), dsem[e][op.dslot], op.dval)

        @block.tensor
        def _(eng):
            run("pe", eng)

        @block.scalar
        def _(eng):
            run("act", eng)

        @block.vector
        def _(eng):
            run("dve", eng)

        @block.gpsimd
        def _(eng):
            run("pool", eng)

        @block.sync
        def _(eng):
            run("sp", eng)


class Arena:
    def __init__(self, handle_f32, nbytes):
        self.h = handle_f32
        self.nbytes = nbytes
        self.off = 0
        self.marks = []

    def alloc(self, nelem, dtype):
        esz = 2 if dtype == BF16 else 4
        nb = (nelem * esz + 63) // 64 * 64
        assert self.off + nb <= self.nbytes, ("SBUF arena overflow", self.off, nb, self.nbytes)
        a = self.h[:, self.off // 4:(self.off + nb) // 4]
        self.off += nb
        if dtype == BF16:
            a = a.bitcast(BF16)
        elif dtype == I32:
            a = a.bitcast(I32)
        return a[:, 0:nelem]

    def mark(self):
        self.marks.append(self.off)

    def release(self):
        self.off = self.marks.pop()


class Ctx:
    pass


def build_program(body, arena_bytes=180 * 1024):
    nc = bass.Bass("TRN2", target_bir_lowering=False)
    cx = Ctx()
    cx.nc = nc
    cx.s = Sched()
    with contextlib.ExitStack() as es:
        arena_h = es.enter_context(nc.sbuf_tensor("arena", [128, arena_bytes // 4], F32))
        cx.arena = Arena(arena_h, arena_bytes)
        cx.psum = []
        cx.psb = []
        for i in range(8):
            p = es.enter_context(nc.psum_tensor(f"ps{i}", [128, 512], F32))
            cx.psum.append(p)
            cx.psb.append(Buf(f"ps{i}"))
        csem = {e: es.enter_context(nc.semaphore(f"c_{e}")) for e in ENGS}
        dsem = {e: [es.enter_context(nc.semaphore(f"d_{e}{i}")) for i in range(n)] for e, n in NDSEM.items()}
        body(cx)
        block = es.enter_context(nc.Block())
        cx.s.emit(nc, block, csem, dsem)
    return nc


import math
import numpy as np
import ml_dtypes


class Cfg:
    def __init__(self, **kw):
        self.D = 4096; self.S = 4096; self.DEPTH = 2
        self.AH = 16; self.AD = 128; self.MBLK = 256; self.TOPK = 3
        self.BH = 32; self.BKV = 4; self.BD = 64; self.WIN = 128
        self.CH = 16; self.CQ = 1024; self.CKV = 512; self.CN = 128; self.CR = 64; self.CV = 128
        self.NG = 8; self.EPG = 4; self.FF = 768
        self.SB = 1024
        self.MG = 1024
        self.CAP = 128
        self.EPS = 1e-6; self.THETA = 10000.0
        for k, v in kw.items():
            setattr(self, k, v)
        c = self
        c.AW = c.AH * c.AD; c.BW = c.BH * c.BD; c.BKW = c.BKV * c.BD; c.CW = c.CH * c.CV
        sizes = (c.AW, c.AW, c.AW, c.BW, c.BKW, c.BKW, c.CQ, c.CKV, c.CR, c.D, c.D, c.D)
        c.OFF = [0] + list(np.cumsum(sizes))
        c.NIN = int(c.OFF[-1])
        c.NE = c.NG * c.EPG
        c.NT = c.S // 128
        c.NBLK = c.S // c.MBLK


def dma(cx, q, out_ap, in_ap, r=(), w=(), out=False, slow=False):
    if slow:
        return cx.s.add(q, lambda e: e.dma_start(out=out_ap, in_=in_ap, allow_slow_non_contiguous=True),
                        r=r, w=w, dma=True, out=out)
    return cx.s.add(q, lambda e: e.dma_start(out=out_ap, in_=in_ap), r=r, w=w, dma=True, out=out)


def I(cx, eng, meth, *args, r=(), w=(), **kw):
    return cx.s.add(eng, lambda e: getattr(e, meth)(*args, **kw), r=r, w=w)


class Tile:
    def __init__(self, cx, shape, dtype, name=""):
        n = int(np.prod(shape[1:]))
        self.ap = cx.arena.alloc(n, dtype)
        self.p = shape[0]
        if len(shape) == 3:
            self.v = self.ap[0:shape[0], :].rearrange("p (a b) -> p a b", b=shape[2])
        else:
            self.v = self.ap[0:shape[0], :]
        self.b = Buf(name)


def psum_f32(cx, i):
    return cx.psum[i]


def psum_bf16(cx, i):
    return cx.psum[i][:, :].bitcast(BF16)


class Consts:
    pass


def const_inputs(c):
    bf = ml_dtypes.bfloat16
    d = {}
    d["c_ident"] = np.eye(128, dtype=np.float32).astype(bf)
    def rot(dim, reps):
        P = np.zeros((128, 128), np.float32)
        h = dim // 2
        for r in range(reps):
            o = r * dim
            for i in range(dim):
                if i < h:
                    P[o + i + h, o + i] = -1.0
                else:
                    P[o + i - h, o + i] = 1.0
        return P.astype(bf)
    d["c_rot128"] = rot(128, 1)
    d["c_rot64"] = rot(64, 2)
    NEG = -30000.0
    k = np.arange(128)[:, None]
    q = np.arange(512)[None, :]
    m = np.zeros((128, 4, 512), np.float32)
    for j in range(4):
        m[:, j, :] = np.where(j * 128 + k <= q, 0.0, NEG)
    d["c_cmask"] = m.reshape(128, 2048).astype(bf)
    qq = np.arange(128)[None, :]
    md = np.where(k <= qq, 0.0, NEG)
    mp = np.where(k > qq, 0.0, NEG)
    d["c_smask"] = np.concatenate([np.tile(md, (1, 4)), np.tile(mp, (1, 4))], axis=1).astype(bf)
    ng = np.full((128, 128), NEG, np.float32)
    d["c_smask2"] = np.concatenate([mp, md, mp, md, ng, md, ng, md], axis=1).astype(bf)
    E = np.zeros((16, c.S), np.float32)
    for n in range(c.NBLK):
        E[n, n * c.MBLK:(n + 1) * c.MBLK] = 1.0
    d["c_eblk"] = E.astype(bf)
    past = np.zeros((128, c.NT, 16), np.float32)
    gm = np.zeros((128, c.NT, 16), np.float32)
    for t in range(c.NT):
        own = (t * 128) // c.MBLK
        past[:, t, :own] = 1.0
        gm[:, t, own:] = -1e30
    d["c_past"] = past.reshape(128, c.NT * 16)
    d["c_gmask"] = gm.reshape(128, c.NT * 16)
    def fr(dim):
        p = np.arange(128)
        i = (p % dim) % (dim // 2)
        return (-(2.0 * i) / dim).astype(np.float32).reshape(128, 1)
    d["c_fexp"] = np.concatenate([fr(128), fr(64)], axis=1)
    d["c_iota"] = np.tile(np.arange(128, dtype=np.float32)[None, :], (128, 1))
    tri = (np.arange(128)[:, None] < np.arange(128)[None, :]).astype(np.float32)
    d["c_tri"] = tri.astype(bf)
    d["c_ones"] = np.ones((128, 128), np.float32).astype(bf)
    d["c_identf"] = np.eye(128, dtype=np.float32)
    return d


CONST_DT = {"c_ident": BF16, "c_rot128": BF16, "c_rot64": BF16, "c_cmask": BF16, "c_smask": BF16,
            "c_eblk": BF16, "c_smask2": BF16, "c_past": F32, "c_gmask": F32, "c_fexp": F32, "c_iota": F32,
            "c_tri": BF16, "c_ones": BF16, "c_identf": F32}


def load_consts(cx, c, cin):
    K = Consts()
    def ld(name, shape, dt):
        t = Tile(cx, shape, dt, name)
        dma(cx, "sp", t.v, cin[name], w=[t.b])
        return t
    K.ident = ld("c_ident", [128, 128], BF16)
    K.rot128 = ld("c_rot128", [128, 128], BF16)
    K.rot64 = ld("c_rot64", [128, 128], BF16)
    K.cmask = ld("c_cmask", [128, 2048], BF16)
    K.smask = ld("c_smask", [128, 1024], BF16)
    K.ones = ld("c_ones", [128, 128], BF16)
    K.fexp = ld("c_fexp", [128, 2], F32)
    return K


def build_rope(cx, c, K, pos_dram, which, cosT, sinT):
    S = c.S
    cx.arena.mark()
    pi = Tile(cx, [128, S], I32, "pos_i")
    pf = Tile(cx, [128, S], F32, "pos_f")
    invf = Tile(cx, [128, 1], F32, "invf")
    tmp = Tile(cx, [128, S], F32, "rtmp")
    dma(cx, "sp", pi.v, pos_dram.partition_broadcast(128), w=[pi.b])
    I(cx, "dve", "tensor_copy", pf.v, pi.v, r=[pi.b], w=[pf.b])
    col = K.fexp.v[:, which:which + 1]
    I(cx, "act", "activation", out=invf.v, in_=col, func=AF.Exp, scale=math.log(c.THETA),
      r=[K.fexp.b], w=[invf.b])
    TWO_PI = 2.0 * math.pi
    ki = Tile(cx, [128, S], I32, "rk_i")
    for (dst, shift) in ((sinT, 0.0), (cosT, 0.5 * math.pi)):
        I(cx, "dve", "tensor_scalar", tmp.v, pf.v, invf.v[:, 0:1], shift, ALU.mult, ALU.add,
          r=[pf.b, invf.b], w=[tmp.b])
        I(cx, "dve", "tensor_scalar", pi.v.bitcast(F32), tmp.v, 1.0 / TWO_PI, 0.0, ALU.mult, ALU.add,
          r=[tmp.b], w=[pi.b])
        I(cx, "dve", "tensor_copy", ki.v, pi.v.bitcast(F32), r=[pi.b], w=[ki.b])
        I(cx, "dve", "tensor_copy", pi.v.bitcast(F32), ki.v, r=[ki.b], w=[pi.b])
        I(cx, "dve", "scalar_tensor_tensor", tmp.v, pi.v.bitcast(F32), -TWO_PI, tmp.v, ALU.mult, ALU.add,
          r=[pi.b, tmp.b], w=[tmp.b])
        I(cx, "act", "activation", out=dst.v, in_=tmp.v, func=AF.Sin, r=[tmp.b], w=[dst.b])
    cx.s.barrier()
    cx.arena.release()


def phase_rmsnorm_T(cx, c, K, h_d, g_d, hnT_d, hn_tok_d=None, hn_f32T_d=None):
    D, S = c.D, c.S
    NC = D // 128
    cx.arena.mark()
    gb = Tile(cx, [128, D], F32, "g_bc")
    dma(cx, "sp", gb.v, g_d.partition_broadcast(128), w=[gb.b])
    xt = [Tile(cx, [128, D], F32, f"x{i}") for i in range(2)]
    junk = Tile(cx, [128, D], BF16, "junk")
    xn = [Tile(cx, [128, D], BF16, f"xn{i}") for i in range(2)]
    ss = [Tile(cx, [128, 2], F32, f"ss{i}") for i in range(2)]
    TB = 4 if S >= 512 else S // 128
    hT = [Tile(cx, [128, NC, TB * 128], BF16, f"hT{i}") for i in range(2)]
    nblk = S // (128 * TB)
    pb = 0
    for blk in range(nblk):
        ht = hT[blk % 2]
        for sub in range(TB):
            t = blk * TB + sub
            x = xt[t % 2]; n = xn[t % 2]; s2 = ss[t % 2]
            dma(cx, "sp", x.v, h_d[t * 128:(t + 1) * 128, :], w=[x.b])
            I(cx, "act", "activation", out=junk.v, in_=x.v, func=AF.Square, accum_out=s2.v[:, 0:1],
              r=[x.b], w=[junk.b, s2.b])
            I(cx, "dve", "tensor_scalar", s2.v[:, 1:2], s2.v[:, 0:1], 1.0 / D, c.EPS, ALU.mult, ALU.add,
              r=[s2.b], w=[s2.b])
            I(cx, "act", "activation", out=s2.v[:, 1:2], in_=s2.v[:, 1:2], func=AF.Sqrt, r=[s2.b], w=[s2.b])
            I(cx, "dve", "reciprocal", s2.v[:, 1:2], s2.v[:, 1:2], r=[s2.b], w=[s2.b])
            I(cx, "dve", "scalar_tensor_tensor", n.v, x.v, s2.v[:, 1:2], gb.v, ALU.mult, ALU.mult,
              r=[x.b, s2.b, gb.b], w=[n.b])
            if hn_tok_d is not None:
                dma(cx, "sp", hn_tok_d[t * 128:(t + 1) * 128, :], n.v, r=[n.b])
            for g8 in range(0, NC, 8):
                bank = pb % 2
                pb += 1
                pt = psum_bf16(cx, bank)
                nn = min(8, NC - g8)
                for j in range(nn):
                    cc = g8 + j
                    I(cx, "pe", "transpose", pt[:, j * 128:(j + 1) * 128], n.v[:, cc * 128:(cc + 1) * 128],
                      K.ident.v, r=[n.b, K.ident.b], w=[cx.psb[bank]])
                eng = "act" if (pb % 2) else "dve"
                src = pt[:, 0:nn * 128].rearrange("p (a b) -> p a b", b=128)
                dstv = ht.v[:, g8:g8 + nn, sub * 128:(sub + 1) * 128]
                if eng == "act":
                    I(cx, "act", "copy", dstv, src, r=[cx.psb[bank]], w=[ht.b])
                else:
                    I(cx, "dve", "tensor_copy", dstv, src, r=[cx.psb[bank]], w=[ht.b])
        dst = hnT_d[:, blk * TB * 128:(blk + 1) * TB * 128].rearrange("(a p) s -> p a s", p=128)
        dma(cx, "sp", dst, ht.v, r=[ht.b])
    cx.s.barrier()
    cx.arena.release()


def phase_proj(cx, c, xT_d, Kdim, W_d, col_tiles, mode, epi, SB=None, PW=256, banks=(2, 3), prep=None):
    S = c.S
    SB = SB or min(c.SB, S)
    KC = Kdim // 128
    cx.arena.mark()
    xb = Tile(cx, [128, KC, SB], BF16, "xblk")
    KQ = 8 if KC >= 8 else KC
    NP = KC // KQ
    nstg = NP if NP >= 2 else 2
    stg = [Tile(cx, [128, KQ, PW], F32, f"wstg{i}") for i in range(nstg)]
    wp = [Tile(cx, [128, KC, PW], BF16, f"wp{i}") for i in range(2)]
    W_list = list(W_d) if isinstance(W_d, (list, tuple)) else [W_d]
    col_tiles = [ct if len(ct) > 3 else (ct[0], ct[1], ct[2], 0) for ct in col_tiles]
    panels = []
    cur = []
    for ct in col_tiles:
        if cur and (ct[3] != cur[-1][3] or ct[0] != cur[-1][0] + cur[-1][1] or (ct[0] + ct[1] - cur[0][0]) > PW):
            panels.append(cur); cur = []
        cur.append(ct)
    if cur:
        panels.append(cur)
    items = [(s0, pan) for s0 in range(0, S, SB) for pan in panels]
    TS = 512 if SB >= 512 else SB
    state = {"nstg": 0, "nbank": 0}

    def emit_load(idx):
        s0, pan = items[idx]
        w = wp[idx % 2]
        c0 = pan[0][0]
        pw = pan[-1][0] + pan[-1][1] - c0
        sts = []
        for q in range(NP):
            st = stg[state["nstg"] % nstg]
            state["nstg"] += 1
            src = W_list[pan[0][3]][q * KQ * 128:(q + 1) * KQ * 128, c0:c0 + pw].rearrange("(a p) n -> p a n", p=128)
            dma(cx, "sp", st.v[:, :, 0:pw], src, w=[st.b])
            sts.append(st)
        deferred = []
        for q in range(NP):
            st = sts[q]
            args = (w.v[:, q * KQ:(q + 1) * KQ, 0:pw], st.v[:, :, 0:pw])
            if NP >= 2 and q == 0:
                I(cx, "pool", "tensor_copy", *args, r=[st.b], w=[w.b])
            else:
                state["ncast"] = state.get("ncast", 0) + 1
                deferred.append((("act", "copy") if state["ncast"] % 3 else ("dve", "tensor_copy"), args, st, w))
        return deferred

    def run_deferred(deferred):
        for ((eng, meth), args, st, w2) in deferred:
            I(cx, eng, meth, *args, r=[st.b], w=[w2.b])

    run_deferred(emit_load(0))
    cur_s0 = None
    for idx, (s0, pan) in enumerate(items):
        if s0 != cur_s0:
            cur_s0 = s0
            dma(cx, "sp", xb.v, xT_d[:, s0:s0 + SB].rearrange("(a p) s -> p a s", p=128), w=[xb.b])
            if prep is not None:
                prep(cx, xb, SB)
        deferred = emit_load(idx + 1) if idx + 1 < len(items) else []
        w = wp[idx % 2]
        c0 = pan[0][0]
        work = []
        for (cs, cw, tag, _wi) in pan:
            lo = cs - c0
            step = TS if mode == "T" else 128
            for ts in range(0, SB, step):
                work.append((lo, cw, tag, ts))
        half = len(work) // 2
        for wi, (lo, cw, tag, ts) in enumerate(work):
            if wi == half:
                run_deferred(deferred)
                deferred = []
            bank = banks[state["nbank"] % len(banks)]
            state["nbank"] += 1
            if mode == "T":
                ps = cx.psum[bank][0:cw, 0:TS]
                for kc in range(KC):
                    I(cx, "pe", "matmul", ps, w.v[:, kc, lo:lo + cw], xb.v[:, kc, ts:ts + TS],
                      start=(kc == 0), stop=(kc == KC - 1), r=[w.b, xb.b], w=[cx.psb[bank]])
                epi(cx, tag, s0 + ts, TS, ps, cx.psb[bank])
            else:
                ps = cx.psum[bank][:, 0:cw]
                for kc in range(KC):
                    I(cx, "pe", "matmul", ps, xb.v[:, kc, ts:ts + 128], w.v[:, kc, lo:lo + cw],
                      start=(kc == 0), stop=(kc == KC - 1), r=[w.b, xb.b], w=[cx.psb[bank]])
                epi(cx, tag, s0 + ts, 128, ps, cx.psb[bank])
        run_deferred(deferred)
    cx.s.barrier()
    cx.arena.release()


def dram(cx, name, shape, dtype):
    kind = "ExternalOutput" if name in cx.debug else "Internal"
    return cx.nc.dram_tensor(name, list(shape), dtype, kind=kind).ap()


class Rot:
    def __init__(self, cx, n, shape, dtype, name):
        self.t = [Tile(cx, shape, dtype, f"{name}{i}") for i in range(n)]
        self.i = 0

    def next(self):
        t = self.t[self.i % len(self.t)]
        self.i += 1
        return t


def make_epilogues(cx, c, K, R):
    E = Consts()
    E.xs = Rot(cx, 2, [128, 512], BF16, "e_xs")
    E.t1 = Rot(cx, 2, [128, 512], F32, "e_t1")
    E.t2 = Rot(cx, 2, [128, 512], F32, "e_t2")
    E.cs = Rot(cx, 2, [128, 1024], F32, "e_cs")
    E.ob = Rot(cx, 3, [128, 512], BF16, "e_ob")
    E.rotbank = 4
    E.trbank = 5
    E.n = 0
    return E


def epi_copy(cx, E, ps, psb, cw, ns, dst_ap, func=None, scale_ap=None, scale_buf=None, mul_ap=None, mul_buf=None):
    o = E.ob.next()
    E.n += 1
    ov = o.v[0:cw, 0:ns]
    if func is not None:
        I(cx, "act", "activation", out=ov, in_=ps, func=func, r=[psb], w=[o.b])
    elif scale_ap is not None:
        I(cx, "dve", "tensor_scalar", ov, ps, scale_ap, None, ALU.mult, r=[psb, scale_buf], w=[o.b])
    elif mul_ap is not None:
        I(cx, "dve", "tensor_tensor", ov, ps, mul_ap, ALU.mult, r=[psb, mul_buf], w=[o.b])
    elif E.n % 2:
        I(cx, "act", "copy", ov, ps, r=[psb], w=[o.b])
    else:
        I(cx, "dve", "tensor_copy", ov, ps, r=[psb], w=[o.b])
    dma(cx, "pool", dst_ap, ov, r=[o.b])


def epi_rope(cx, c, K, E, ps, psb, cw, ns, s0, dst_ap, rot, cos_d, sin_d, mul_ap=None, mul_buf=None):
    xs = E.xs.next(); t1 = E.t1.next(); t2 = E.t2.next(); cs = E.cs.next(); o = E.ob.next()
    dma(cx, "sp", cs.v[0:cw, 0:ns], cos_d[0:cw, s0:s0 + ns], w=[cs.b])
    dma(cx, "sp", cs.v[0:cw, 512:512 + ns], sin_d[0:cw, s0:s0 + ns], w=[cs.b])
    xv = xs.v[0:cw, 0:ns]
    if mul_ap is not None:
        I(cx, "dve", "tensor_tensor", xv, ps, mul_ap, ALU.mult, r=[psb, mul_buf], w=[xs.b])
    else:
        I(cx, "act", "copy", xv, ps, r=[psb], w=[xs.b])
    rb = E.rotbank
    rp = cx.psum[rb][0:cw, 0:ns]
    I(cx, "pe", "matmul", rp, rot.v[0:cw, 0:cw], xv, start=True, stop=True, r=[rot.b, xs.b], w=[cx.psb[rb]])
    I(cx, "dve", "tensor_tensor", t1.v[0:cw, 0:ns], xv, cs.v[0:cw, 0:ns], ALU.mult, r=[xs.b, cs.b], w=[t1.b])
    I(cx, "dve", "tensor_tensor", t2.v[0:cw, 0:ns], rp, cs.v[0:cw, 512:512 + ns], ALU.mult,
      r=[cx.psb[rb], cs.b], w=[t2.b])
    I(cx, "pool", "tensor_tensor", o.v[0:cw, 0:ns], t1.v[0:cw, 0:ns], t2.v[0:cw, 0:ns], ALU.add,
      r=[t1.b, t2.b], w=[o.b])
    dma(cx, "pool", dst_ap, o.v[0:cw, 0:ns], r=[o.b])


def epi_T2N(cx, c, K, E, ps, psb, cw, ns, s0, dst_rows_fn, mul_ap=None, mul_buf=None):
    xs = E.xs.next(); o = E.ob.next()
    xv = xs.v[0:cw, 0:ns]
    if mul_ap is not None:
        I(cx, "dve", "tensor_tensor", xv, ps, mul_ap, ALU.mult, r=[psb, mul_buf], w=[xs.b])
    else:
        I(cx, "act", "copy", xv, ps, r=[psb], w=[xs.b])
    tb = E.trbank
    pt = psum_bf16(cx, tb)
    nt = ns // 128
    for i in range(nt):
        I(cx, "pe", "transpose", pt[:, i * 128:i * 128 + cw], xs.v[0:cw, i * 128:(i + 1) * 128],
          K.ident.v[0:cw, 0:cw], r=[xs.b, K.ident.b], w=[cx.psb[tb]])
    ov = o.v[:, 0:nt * cw].rearrange("p (a b) -> p a b", b=cw)
    src = pt[:, 0:nt * 128].rearrange("p (a b) -> p a b", b=128)[:, :, 0:cw]
    I(cx, "act", "copy", ov, src, r=[cx.psb[tb]], w=[o.b])
    dma(cx, "pool", dst_rows_fn(s0, ns), ov, r=[o.b])


def phase_A(cx, c, K, E, R, T, hnT_d, w_in_d):
    O = c.OFF
    tilesT = []
    for h in range(c.AH):
        tilesT.append((O[0] + 128 * h, 128, ("ropeA", T["qaT"], h)))
    for h in range(c.AH):
        tilesT.append((O[1] + 128 * h, 128, ("ropeA", T["kaT"], h)))
    for j in range(c.BW // 128):
        tilesT.append((O[3] + 128 * j, 128, ("ropeB", T["qbT"], j)))
    for j in range(c.BKW // 128):
        tilesT.append((O[4] + 128 * j, 128, ("ropeB", T["kbT"], j)))
    for j in range(c.CQ // 128):
        tilesT.append((O[6] + 128 * j, 128, ("copy", T["cqT"], j)))
    for j in range(c.CKV // 128):
        tilesT.append((O[7] + 128 * j, 128, ("copy", T["ckvT"], j)))
    tilesT.append((O[8], c.CR, ("ropeK", T["kpeT"], 0)))
    for i in range(3):
        for j in range(c.D // 128):
            tilesT.append((O[9 + i] + 128 * j, 128, ("sig", T["gT"], i * (c.D // 128) + j)))

    def epiT(cx, tag, s0, ns, ps, psb):
        kind, dst, j = tag
        if kind == "ropeA":
            epi_rope(cx, c, K, E, ps, psb, 128, ns, s0, dst[j * 128:(j + 1) * 128, s0:s0 + ns], K.rot128,
                     R["cosA"], R["sinA"])
        elif kind == "ropeB":
            epi_rope(cx, c, K, E, ps, psb, 128, ns, s0, dst[j * 128:(j + 1) * 128, s0:s0 + ns], K.rot64,
                     R["cosB"], R["sinB"])
        elif kind == "ropeK":
            epi_rope(cx, c, K, E, ps, psb, 64, ns, s0, dst[0:64, s0:s0 + ns], K.rot64, R["cosB"], R["sinB"])
        elif kind == "copy":
            epi_copy(cx, E, ps, psb, 128, ns, dst[j * 128:(j + 1) * 128, s0:s0 + ns])
        elif kind == "sig":
            epi_copy(cx, E, ps, psb, 128, ns, dst[j * 128:(j + 1) * 128, s0:s0 + ns], func=AF.Sigmoid)
    phase_proj(cx, c, hnT_d, c.D, w_in_d, tilesT, "T", epiT)

    tilesN = []
    for j in range(c.AW // 256):
        tilesN.append((O[2] + 256 * j, 256, (T["va"], 256 * j)))
    for j in range(max(1, c.BKW // 256)):
        wdt = min(256, c.BKW)
        tilesN.append((O[5] + wdt * j, wdt, (T["vb"], wdt * j)))

    def epiN(cx, tag, s0, ns, ps, psb):
        dst, c0 = tag
        cw = ps.shape[1]
        epi_copy(cx, E, ps, psb, 128, cw, dst[s0:s0 + ns, c0:c0 + cw])
    phase_proj(cx, c, hnT_d, c.D, w_in_d, tilesN, "N", epiN)


def phase_mla_up(cx, c, K, E, R, T, qg_d, kvg_d, wq_d, wkv_d):
    for (xT_d, KD, g_d, W_d, which) in ((T["cqT"], c.CQ, qg_d, wq_d, "q"), (T["ckvT"], c.CKV, kvg_d, wkv_d, "kv")):
        KC = KD // 128
        SB = min(c.SB, c.S)
        cx.arena.mark()
        gcol = Tile(cx, [128, KC], F32, "gcol")
        dma(cx, "sp", gcol.v, g_d.rearrange("(a p) -> p a", p=128), w=[gcol.b], slow=True)
        rstd = Tile(cx, [128, SB], F32, "rstd_bc")
        sq = Rot(cx, 2, [128, 512], BF16, "sq")

        def prep(cx, xb, SBn, KC=KC, KD=KD, gcol=gcol, rstd=rstd, sq=sq):
            bank = 6
            for ts in range(0, SBn, 512):
                n = min(512, SBn - ts)
                ps = cx.psum[bank][:, 0:n]
                for kc in range(KC):
                    s = sq.next()
                    I(cx, "act", "activation", out=s.v[:, 0:n], in_=xb.v[:, kc, ts:ts + n], func=AF.Square,
                      r=[xb.b], w=[s.b])
                    I(cx, "pe", "matmul", ps, K.ones.v, s.v[:, 0:n], start=(kc == 0), stop=(kc == KC - 1),
                      r=[K.ones.b, s.b], w=[cx.psb[bank]])
                rv = rstd.v[:, ts:ts + n]
                I(cx, "dve", "tensor_scalar", rv, ps, 1.0 / KD, c.EPS, ALU.mult, ALU.add,
                  r=[cx.psb[bank]], w=[rstd.b])
                I(cx, "act", "activation", out=rv, in_=rv, func=AF.Sqrt, r=[rstd.b], w=[rstd.b])
                I(cx, "dve", "reciprocal", rv, rv, r=[rstd.b], w=[rstd.b])
            for kc in range(KC):
                I(cx, "dve", "tensor_scalar", xb.v[:, kc, :], xb.v[:, kc, :], gcol.v[:, kc:kc + 1], None, ALU.mult,
                  r=[xb.b, gcol.b], w=[xb.b])

        if which == "q":
            tiles = []
            for h in range(c.CH):
                tiles.append((192 * h, 128, ("qn", h)))
                tiles.append((192 * h + 128, 64, ("qpe", h)))

            def epi(cx, tag, s0, ns, ps, psb, rstd=rstd, SB=SB):
                kind, h = tag
                lo = s0 % SB
                if kind == "qn":
                    epi_copy(cx, E, ps, psb, 128, ns, T["qnT"][h * 128:(h + 1) * 128, s0:s0 + ns],
                             mul_ap=rstd.v[:, lo:lo + ns], mul_buf=rstd.b)
                else:
                    epi_rope(cx, c, K, E, ps, psb, 64, ns, s0, T["qpeT"][h * 64:(h + 1) * 64, s0:s0 + ns], K.rot64,
                             R["cosB"], R["sinB"], mul_ap=rstd.v[0:64, lo:lo + ns], mul_buf=rstd.b)
        else:
            tiles = []
            for h in range(c.CH):
                tiles.append((256 * h, 128, ("kn", h)))
                tiles.append((256 * h + 128, 128, ("v", h)))

            def epi(cx, tag, s0, ns, ps, psb, rstd=rstd, SB=SB):
                kind, h = tag
                lo = s0 % SB
                if kind == "kn":
                    epi_copy(cx, E, ps, psb, 128, ns, T["knT"][h * 128:(h + 1) * 128, s0:s0 + ns],
                             mul_ap=rstd.v[:, lo:lo + ns], mul_buf=rstd.b)
                else:
                    def rows(s0, ns, h=h):
                        return T["vc"][s0:s0 + ns, h * 128:(h + 1) * 128].rearrange("(a p) d -> p a d", p=128)
                    epi_T2N(cx, c, K, E, ps, psb, 128, ns, s0, rows, mul_ap=rstd.v[:, lo:lo + ns], mul_buf=rstd.b)
        phase_proj(cx, c, xT_d, KD, W_d, tiles, "T", epi, prep=prep)
        cx.arena.release()


def phase_attn_causal(cx, c, K, H, terms, v_d, dv, scale, oT_d, moba=False, cin=None, mla_shared_k=False):
    S, NT = c.S, c.NT
    NG = S // 512
    cx.arena.mark()
    nterm = len(terms)
    QT = [[Tile(cx, [kd, S], BF16, f"QT{p}") for (_, _, kd) in terms] for p in range(2)]
    KT = [[Tile(cx, [kd, S], BF16, f"KT{p}") for (_, _, kd) in terms] for p in range(2)]
    V = [Tile(cx, [128, NT, dv + 1], BF16, f"V{p}") for p in range(2)]
    for p in range(2):
        I(cx, "pool", "memset", V[p].v[:, :, dv:dv + 1], 1.0, w=[V[p].b])
    PT = Rot(cx, 3, [128, 512], BF16, "PT")
    osb = Rot(cx, 2, [128, 4, dv], BF16, "osb")
    oT = Rot(cx, 2, [128, 512], BF16, "oT")
    rc = Rot(cx, 4, [128, 1], F32, "rc")
    if moba:
        eblk = Tile(cx, [16, S], BF16, "eblk")
        dma(cx, "sp", eblk.v, cin["c_eblk"], w=[eblk.b])
        past = Tile(cx, [128, NT, 16], F32, "past")
        dma(cx, "sp", past.v, cin["c_past"].rearrange("p (a b) -> p a b", b=16), w=[past.b])
        gmask = Tile(cx, [128, NT, 16], F32, "gmask")
        dma(cx, "sp", gmask.v, cin["c_gmask"].rearrange("p (a b) -> p a b", b=16), w=[gmask.b])
        BT = [Tile(cx, [16, S], BF16, f"BT{p}") for p in range(2)]
        kmf = Tile(cx, [128, 16], F32, "kmf")
        kmb = Tile(cx, [128, 16], BF16, "kmb")
        I(cx, "pool", "memset", kmf.v, 0.0, w=[kmf.b])
        g1 = Tile(cx, [128, NT, 16], F32, "g1")
        g2 = Tile(cx, [128, NT, 16], F32, "g2")
        eq = Tile(cx, [128, NT, 16], F32, "eq")
        mx = Tile(cx, [128, NT], F32, "mx")
        bb = Tile(cx, [128, NT, 16], BF16, "bb")
    sstep = 0

    def load_head(h):
        p = h % 2
        for i, (q_d, k_d, kd) in enumerate(terms):
            dma(cx, "sp", QT[p][i].v, q_d[h * kd:(h + 1) * kd, :], w=[QT[p][i].b])
            if mla_shared_k and i == 1:
                if h < 2:
                    dma(cx, "sp", KT[p][i].v, k_d[0:kd, :], w=[KT[p][i].b])
            else:
                dma(cx, "sp", KT[p][i].v, k_d[h * kd:(h + 1) * kd, :], w=[KT[p][i].b])
        dma(cx, "sp", V[p].v[:, :, 0:dv], v_d[:, h * dv:(h + 1) * dv].rearrange("(t p) d -> p t d", p=128),
            w=[V[p].b])

    def bias_head(h):
        p = h % 2
        if moba:
            kt0 = KT[p][0]; qt0 = QT[p][0]; bt = BT[p]
            NB = c.NBLK
            I(cx, "dve", "tensor_reduce", kmf.v[:, 0:NB], kt0.v.rearrange("p (n k) -> p n k", k=c.MBLK), AX.X, ALU.add,
              r=[kt0.b], w=[kmf.b])
            I(cx, "dve", "tensor_copy", kmb.v, kmf.v, r=[kmf.b], w=[kmb.b])
            gb_ = 7
            gp = cx.psum[gb_][:, 0:NT * 16]
            for t in range(NT):
                I(cx, "pe", "matmul", gp[:, t * 16:(t + 1) * 16], qt0.v[:, t * 128:(t + 1) * 128], kmb.v,
                  start=True, stop=True, r=[qt0.b, kmb.b], w=[cx.psb[gb_]])
            gp3 = gp.rearrange("p (a b) -> p a b", b=16)
            I(cx, "dve", "tensor_tensor", g1.v, gp3, gmask.v, ALU.add, r=[cx.psb[gb_], gmask.b], w=[g1.b])
            cur = g1
            for it in range(3):
                I(cx, "dve", "tensor_reduce", mx.v, cur.v, AX.X, ALU.max, r=[cur.b], w=[mx.b])
                if it < 2:
                    mb = mx.v.unsqueeze(2).broadcast_to([128, NT, 16])
                    I(cx, "dve", "tensor_tensor", eq.v, cur.v, mb, ALU.is_equal, r=[cur.b, mx.b], w=[eq.b])
                    I(cx, "dve", "scalar_tensor_tensor", g2.v, eq.v, -1e30, cur.v, ALU.mult, ALU.add,
                      r=[eq.b, cur.b], w=[g2.b])
                    cur = g2
            mb = mx.v.unsqueeze(2).broadcast_to([128, NT, 16])
            I(cx, "dve", "tensor_tensor", eq.v, g1.v, mb, ALU.is_ge, r=[g1.b, mx.b], w=[eq.b])
            I(cx, "dve", "tensor_tensor", eq.v, eq.v, past.v, ALU.mult, r=[eq.b, past.b], w=[eq.b])
            I(cx, "dve", "tensor_tensor", eq.v, eq.v, past.v, ALU.subtract, r=[eq.b, past.b], w=[eq.b])
            I(cx, "dve", "tensor_scalar", bb.v, eq.v, 30000.0, None, ALU.mult, r=[eq.b], w=[bb.b])
            tb = 7
            pt = psum_bf16(cx, tb)
            for t0 in range(0, NT, 8):
                n8 = min(8, NT - t0)
                for j in range(n8):
                    I(cx, "pe", "transpose", pt[0:16, j * 128:(j + 1) * 128], bb.v[:, t0 + j, :], K.ident.v,
                      r=[bb.b, K.ident.b], w=[cx.psb[tb]])
                I(cx, "act", "copy", bt.v[:, t0 * 128:(t0 + n8) * 128], pt[0:16, 0:n8 * 128], r=[cx.psb[tb]], w=[bt.b])

    load_head(0)
    if moba:
        bias_head(0)
    for h in range(H):
        p = h % 2
        if h + 1 < H:
            load_head(h + 1)
        for G in range(NG):
            if G == NG // 2 and h + 1 < H and moba:
                bias_head(h + 1)
            accb = (2, 3, 4, 5)
            def acc(i):
                return cx.psum[accb[i]][:, 0:dv + 1]
            nk = 4 * G + 4

            def emit_S(kt, G=G, p=p):
                nonlocal sstep
                j = kt - 4 * G
                jq = max(j, 0)
                q0 = jq * 128
                N = 512 - q0
                sb_ = sstep % 2
                sstep += 1
                ps = cx.psum[sb_][:, 0:N]
                ops = []
                for i in range(nterm):
                    ops.append((KT[p][i].v[:, kt * 128:(kt + 1) * 128], QT[p][i].v[:, G * 512 + q0:(G + 1) * 512],
                                [KT[p][i].b, QT[p][i].b]))
                if moba:
                    ops.append((eblk.v[:, kt * 128:(kt + 1) * 128], BT[p].v[:, G * 512 + q0:(G + 1) * 512],
                                [eblk.b, BT[p].b]))
                if j >= 0:
                    ops.append((K.ident.v, K.cmask.v[:, j * 512 + q0:(j + 1) * 512], [K.ident.b, K.cmask.b]))
                for oi, (l, r_, bufs) in enumerate(ops):
                    I(cx, "pe", "matmul", ps, l, r_, start=(oi == 0), stop=(oi == len(ops) - 1),
                      r=bufs, w=[cx.psb[sb_]])
                return (ps, sb_, jq, q0, N)

            nxt = emit_S(0)
            for kt in range(nk):
                (ps, sb_, jq, q0, N) = nxt
                if kt + 1 < nk:
                    nxt = emit_S(kt + 1)
                ptile = PT.next()
                I(cx, "act", "activation", out=ptile.v[:, 0:N], in_=ps, func=AF.Exp, scale=scale,
                  r=[cx.psb[sb_]], w=[ptile.b])
                for i in range(jq, 4):
                    I(cx, "pe", "matmul", acc(i), ptile.v[:, i * 128 - q0:i * 128 - q0 + 128], V[p].v[:, kt, :],
                      start=(kt == 0), stop=(kt == 4 * G + i), r=[ptile.b, V[p].b], w=[cx.psb[accb[i]]])
            ob = osb.next()
            for i in range(4):
                r1 = rc.next()
                a = acc(i)
                I(cx, "dve", "reciprocal", r1.v, a[:, dv:dv + 1], r=[cx.psb[accb[i]]], w=[r1.b])
                I(cx, "dve", "tensor_scalar", ob.v[:, i, :], a[:, 0:dv], r1.v[:, 0:1], None, ALU.mult,
                  r=[cx.psb[accb[i]], r1.b], w=[ob.b])
            tb = 6
            pt = psum_bf16(cx, tb)
            for i in range(4):
                I(cx, "pe", "transpose", pt[0:dv, i * 128:(i + 1) * 128], ob.v[:, i, :], K.ident.v,
                  r=[ob.b, K.ident.b], w=[cx.psb[tb]])
            ot = oT.next()
            I(cx, "act", "copy", ot.v[0:dv, :], pt[0:dv, 0:512], r=[cx.psb[tb]], w=[ot.b])
            dma(cx, "pool", oT_d[h * dv:(h + 1) * dv, G * 512:(G + 1) * 512], ot.v[0:dv, :], r=[ot.b])
    cx.s.barrier()
    cx.arena.release()


def phase_swa(cx, c, K, qT_d, kT_d, v_d, sinks_d, oT_d, cin):
    S, NT = c.S, c.NT
    d = c.BD
    r = c.BH // c.BKV
    scale = d ** -0.5
    cx.arena.mark()
    es = Tile(cx, [128, c.BH], F32, "esink")
    dma(cx, "sp", es.v, sinks_d.partition_broadcast(128), w=[es.b])
    I(cx, "act", "activation", out=es.v, in_=es.v, func=AF.Exp, r=[es.b], w=[es.b])
    sm = Tile(cx, [128, 1024], BF16, "smask2")
    dma(cx, "sp", sm.v, cin["c_smask2"], w=[sm.b])
    KTt = [Tile(cx, [64, S], BF16, f"sK{p}") for p in range(2)]
    Vt = [Tile(cx, [128, NT, d + 1], BF16, f"sV{p}") for p in range(2)]
    for p in range(2):
        I(cx, "pool", "memset", Vt[p].v[:, :, d:d + 1], 1.0, w=[Vt[p].b])
    Q2 = [Tile(cx, [64, 2, S], BF16, f"sQ{p}") for p in range(2)]
    PT = Rot(cx, 3, [128, 512], BF16, "sPT")
    ob2 = Rot(cx, 2, [128, 128], BF16, "sob")
    oTt = Rot(cx, 2, [128, 512], BF16, "soT")
    den = Rot(cx, 4, [128, 1], F32, "sden")
    step = 0
    npair = 0
    for g in range(c.BKV):
        kp = g % 2
        dma(cx, "sp", KTt[kp].v, kT_d[g * 64:(g + 1) * 64, :], w=[KTt[kp].b])
        dma(cx, "sp", Vt[kp].v[:, :, 0:d], v_d[:, g * 64:(g + 1) * 64].rearrange("(t p) d -> p t d", p=128),
            w=[Vt[kp].b])
        for hp in range(r // 2):
            h0 = g * r + 2 * hp
            q2 = Q2[npair % 2]
            npair += 1
            dma(cx, "sp", q2.v, qT_d[h0 * 64:(h0 + 2) * 64, :].rearrange("(h d) s -> d h s", d=64), w=[q2.b])
            ot = None

            def emit_S(t, q2=q2, kp=kp):
                nonlocal step
                sb_ = step % 2
                step += 1
                ps = cx.psum[sb_][:, 0:512]
                mv = sm.v[:, 0:512] if t > 0 else sm.v[:, 512:1024]
                I(cx, "pe", "matmul", ps, K.ident.v, mv, start=True, stop=False, r=[K.ident.b, sm.b], w=[cx.psb[sb_]])
                mms = []
                for hh in range(2):
                    for kk in range(2):
                        kt = t - 1 + kk
                        if kt < 0:
                            continue
                        mms.append((hh, kk, kt))
                for mi, (hh, kk, kt) in enumerate(mms):
                    col = (hh * 2 + kk) * 128
                    I(cx, "pe", "matmul", ps[:, col:col + 128], KTt[kp].v[:, kt * 128:(kt + 1) * 128],
                      q2.v[:, hh, t * 128:(t + 1) * 128], start=False, stop=(mi == len(mms) - 1),
                      r=[KTt[kp].b, q2.b], w=[cx.psb[sb_]])
                return ps, sb_

            nxt = emit_S(0)
            for t in range(NT):
                ps, sb_ = nxt
                if t + 1 < NT:
                    nxt = emit_S(t + 1)
                pt_ = PT.next()
                I(cx, "act", "activation", out=pt_.v, in_=ps, func=AF.Exp, scale=scale, r=[cx.psb[sb_]], w=[pt_.b])
                ab = 2 + (t % 2)
                o2 = ob2.next()
                for hh in range(2):
                    a = cx.psum[ab][:, hh * 128:hh * 128 + d + 1]
                    kks = [kk for kk in range(2) if t - 1 + kk >= 0]
                    for ki, kk in enumerate(kks):
                        col = (hh * 2 + kk) * 128
                        I(cx, "pe", "matmul", a, pt_.v[:, col:col + 128], Vt[kp].v[:, t - 1 + kk, :],
                          start=(ki == 0), stop=(ki == len(kks) - 1), r=[pt_.b, Vt[kp].b], w=[cx.psb[ab]])
                    dn = den.next()
                    I(cx, "dve", "tensor_tensor", dn.v, a[:, d:d + 1], es.v[:, h0 + hh:h0 + hh + 1], ALU.add,
                      r=[cx.psb[ab], es.b], w=[dn.b])
                    I(cx, "dve", "reciprocal", dn.v, dn.v, r=[dn.b], w=[dn.b])
                    I(cx, "dve", "tensor_scalar", o2.v[:, hh * 64:(hh + 1) * 64], a[:, 0:d], dn.v[:, 0:1], None, ALU.mult,
                      r=[cx.psb[ab], dn.b], w=[o2.b])
                tb = 4
                ptb = psum_bf16(cx, tb)
                if t % 4 == 0:
                    ot = oTt.next()
                I(cx, "pe", "transpose", ptb[:, 0:128], o2.v, K.ident.v, r=[o2.b, K.ident.b], w=[cx.psb[tb]])
                I(cx, "act", "copy", ot.v[:, (t % 4) * 128:(t % 4 + 1) * 128], ptb[:, 0:128], r=[cx.psb[tb]], w=[ot.b])
                if t % 4 == 3 or t == NT - 1:
                    n = (t % 4 + 1) * 128
                    t0 = t - t % 4
                    dma(cx, "pool", oT_d[h0 * 64:(h0 + 2) * 64, t0 * 128:t0 * 128 + n], ot.v[:, 0:n], r=[ot.b])
    cx.s.barrier()
    cx.arena.release()


def phase_out_gate(cx, c, K, oT_list, W_list, gT_d, yT_d):
    S, D = c.S, c.D
    SB = 512 if S >= 512 else S
    PW = 256
    cx.arena.mark()
    KCs = [o.shape[0] // 128 for o in oT_list]
    xb = [Tile(cx, [128, KCs[i], SB], BF16, f"ox{i}") for i in range(3)]
    wp = [[Tile(cx, [128, KCs[i], PW], BF16, f"ow{i}_{p}") for p in range(2)] for i in range(3)]
    stg = Rot(cx, 4, [128, 8, PW], F32, "ostg")
    gt = [Rot(cx, 2, [128, SB], BF16, f"og{i}") for i in range(3)]
    tt = [Rot(cx, 2, [128, SB], F32, f"ot{i}") for i in range(3)]
    yo = Rot(cx, 2, [128, SB], BF16, "oy")
    nb = 0
    items = [(s0, c0) for s0 in range(0, S, SB) for c0 in range(0, D, PW)]
    ncast = [0]

    def emit_load(idx):
        s0, c0 = items[idx]
        pp = idx % 2
        for i in range(3):
            w = wp[i][pp]
            KQ = min(8, KCs[i])
            for q in range(KCs[i] // KQ):
                st = stg.next()
                src = W_list[i][q * KQ * 128:(q + 1) * KQ * 128, c0:c0 + PW].rearrange("(a p) n -> p a n", p=128)
                dma(cx, "sp", st.v[:, 0:KQ, :], src, w=[st.b])
                I(cx, "act", "copy", w.v[:, q * KQ:(q + 1) * KQ, :], st.v[:, 0:KQ, :], r=[st.b], w=[w.b])

    emit_load(0)
    cur_s0 = None
    for idx, (s0, c0) in enumerate(items):
        pp = idx % 2
        if s0 != cur_s0:
            cur_s0 = s0
            for i in range(3):
                dma(cx, "sp", xb[i].v, oT_list[i][:, s0:s0 + SB].rearrange("(a p) s -> p a s", p=128), w=[xb[i].b])
        if idx + 1 < len(items):
            emit_load(idx + 1)
        for lo in range(0, PW, 128):
            col = c0 + lo
            tts = []
            for i in range(3):
                bank = (nb % 2) * 3 + i
                ps = cx.psum[bank][:, 0:SB]
                w = wp[i][pp]
                for kc in range(KCs[i]):
                    I(cx, "pe", "matmul", ps, w.v[:, kc, lo:lo + 128], xb[i].v[:, kc, :],
                      start=(kc == 0), stop=(kc == KCs[i] - 1), r=[w.b, xb[i].b], w=[cx.psb[bank]])
                g = gt[i].next()
                dma(cx, "sp", g.v, gT_d[i * D + col:i * D + col + 128, s0:s0 + SB], w=[g.b])
                t = tt[i].next()
                I(cx, "dve", "tensor_tensor", t.v, ps, g.v, ALU.mult, r=[cx.psb[bank], g.b], w=[t.b])
                tts.append(t)
            nb += 1
            I(cx, "pool", "tensor_tensor", tts[0].v, tts[0].v, tts[1].v, ALU.add, r=[tts[0].b, tts[1].b], w=[tts[0].b])
            y = yo.next()
            I(cx, "pool", "tensor_tensor", y.v, tts[0].v, tts[2].v, ALU.add, r=[tts[0].b, tts[2].b], w=[y.b])
            dma(cx, "pool", yT_d[col:col + 128, s0:s0 + SB], y.v, r=[y.b])
    cx.s.barrier()
    cx.arena.release()


def phase_resid_proj(cx, c, K, xT_d, Kdim, W_d, h_d):
    cx.arena.mark()
    ht = Rot(cx, 3, [128, 256], F32, "rh")
    tiles = [(256 * j, 256, 256 * j) for j in range(c.D // 256)]

    def epi(cx, tag, s0, ns, ps, psb):
        t = ht.next()
        dma(cx, "sp", t.v, h_d[s0:s0 + 128, tag:tag + 256], w=[t.b])
        I(cx, "dve", "tensor_tensor", t.v, ps, t.v, ALU.add, r=[psb, t.b], w=[t.b])
        dma(cx, "pool", h_d[s0:s0 + 128, tag:tag + 256], t.v, r=[t.b])
    phase_proj(cx, c, xT_d, Kdim, W_d, tiles, "N", epi)
    cx.arena.release()


def phase_moe(cx, c, K, T, cin, h_d, g_d, wg_d, bg_d, we_d, be_d, wgate_d, wup_d, wdown_d):
    S, D, NT, NE = c.S, c.D, c.NT, c.NE
    NC = D // 128
    NGRP = max(1, S // c.MG)
    TPG = NT // NGRP
    NSL = NGRP * c.CAP
    FC = c.FF // 128
    hn2_d = T["hn2"]
    cx.arena.mark()
    A_f = Tile(cx, [128, NT, NE], F32, "A_f")
    A_b = Tile(cx, [128, NT, NE], BF16, "A_b")
    Wt = Tile(cx, [128, NT, NE], F32, "Wt")
    pos = Tile(cx, [128, NT, NE], F32, "pos")
    iota = Tile(cx, [128, 128], F32, "iota")
    dma(cx, "sp", iota.v, cin["c_iota"], w=[iota.b])
    tri = Tile(cx, [128, 128], BF16, "tri")
    dma(cx, "sp", tri.v, cin["c_tri"], w=[tri.b])
    cx.arena.mark()
    gb = Tile(cx, [128, D], F32, "m_gbc")
    dma(cx, "sp", gb.v, g_d.partition_broadcast(128), w=[gb.b])
    wr = Tile(cx, [128, NC, 40], F32, "wr")
    dma(cx, "sp", wr.v[:, :, 0:8], wg_d.rearrange("(a p) n -> p a n", p=128), w=[wr.b])
    dma(cx, "sp", wr.v[:, :, 8:40], we_d.rearrange("(a p) n -> p a n", p=128), w=[wr.b])
    bias = Tile(cx, [128, 40], F32, "rbias")
    dma(cx, "sp", bias.v[:, 0:8], bg_d.partition_broadcast(128), w=[bias.b])
    dma(cx, "sp", bias.v[:, 8:40], be_d.partition_broadcast(128), w=[bias.b])
    xt = Rot(cx, 2, [128, D], F32, "m_x")
    xn = Rot(cx, 1, [128, D], F32, "m_xn")
    xnb = Rot(cx, 2, [128, D], BF16, "m_xnb")
    junk = Tile(cx, [128, D], BF16, "m_junk")
    xT = Rot(cx, 1, [128, NC, 128], F32, "m_xT")
    sc = Rot(cx, 2, [128, 16], F32, "m_sc")
    lg = Rot(cx, 2, [128, 40], F32, "m_lg")
    tmp = Rot(cx, 2, [128, 8, 4], F32, "m_tmp")
    small = Rot(cx, 2, [128, 32], F32, "m_small")
    nb = 0
    for t in range(NT):
        x = xt.next(); n = xn.next(); nbf = xnb.next(); s = sc.next(); l = lg.next(); tp = tmp.next(); sm = small.next()
        xTt = xT.next()
        dma(cx, "sp", x.v, h_d[t * 128:(t + 1) * 128, :], w=[x.b])
        I(cx, "act", "activation", out=junk.v, in_=x.v, func=AF.Square, accum_out=s.v[:, 0:1], r=[x.b], w=[junk.b, s.b])
        I(cx, "dve", "tensor_scalar", s.v[:, 1:2], s.v[:, 0:1], 1.0 / D, c.EPS, ALU.mult, ALU.add, r=[s.b], w=[s.b])
        I(cx, "act", "activation", out=s.v[:, 1:2], in_=s.v[:, 1:2], func=AF.Sqrt, r=[s.b], w=[s.b])
        I(cx, "dve", "reciprocal", s.v[:, 1:2], s.v[:, 1:2], r=[s.b], w=[s.b])
        I(cx, "dve", "scalar_tensor_tensor", n.v, x.v, s.v[:, 1:2], gb.v, ALU.mult, ALU.mult, r=[x.b, s.b, gb.b], w=[n.b])
        I(cx, "pool", "tensor_copy", nbf.v, n.v, r=[n.b], w=[nbf.b])
        dma(cx, "act", hn2_d[t * 128:(t + 1) * 128, :], nbf.v, r=[nbf.b])
        for g4 in range(0, NC, 4):
            bank = nb % 2
            nb += 1
            pt = cx.psum[bank]
            for j in range(4):
                cc = g4 + j
                I(cx, "pe", "transpose", pt[:, j * 128:(j + 1) * 128], n.v[:, cc * 128:(cc + 1) * 128], iota_ident(cx, K),
                  r=[n.b, K.identf.b], w=[cx.psb[bank]])
            src = pt[:, 0:512].rearrange("p (a b) -> p a b", b=128)
            if nb % 2:
                I(cx, "act", "copy", xTt.v[:, g4:g4 + 4, :], src, r=[cx.psb[bank]], w=[xTt.b])
            else:
                I(cx, "dve", "tensor_copy", xTt.v[:, g4:g4 + 4, :], src, r=[cx.psb[bank]], w=[xTt.b])
        lb = 2
        lp = cx.psum[lb][:, 0:40]
        for kc in range(NC):
            I(cx, "pe", "matmul", lp, xTt.v[:, kc, :], wr.v[:, kc, :], start=(kc == 0), stop=(kc == NC - 1),
              r=[xTt.b, wr.b], w=[cx.psb[lb]])
        I(cx, "dve", "tensor_tensor", l.v, lp, bias.v, ALU.add, r=[cx.psb[lb], bias.b], w=[l.b])
        lgv = l.v[:, 0:8]
        le3 = l.v[:, 8:40].rearrange("p (g e) -> p g e", e=4)
        m = s.v[:, 2:3]; negm = s.v[:, 3:4]; se = s.v[:, 4:5]; m1 = s.v[:, 5:6]; nm1 = s.v[:, 6:7]; m2 = s.v[:, 7:8]
        rr = s.v[:, 8:9]; w1 = s.v[:, 9:10]; w2 = s.v[:, 10:11]
        G1 = sm.v[:, 0:8]; esel = sm.v[:, 8:12]; e1 = sm.v[:, 12:16]; es2 = sm.v[:, 16:20]; e2 = sm.v[:, 20:24]
        asel = sm.v[:, 24:28]; wsel = sm.v[:, 28:32]
        R_ = [l.b, s.b, sm.b, tp.b]
        def D_(meth, *a, **k):
            I(cx, "dve", meth, *a, r=R_, w=R_, **k)
        D_("tensor_reduce", m, lgv, AX.X, ALU.max)
        D_("tensor_scalar", G1, lgv, m, None, ALU.is_equal)
        D_("tensor_scalar", negm, m, -1.0, None, ALU.mult)
        I(cx, "act", "activation", out=sm.v[:, 12:20], in_=lgv, func=AF.Exp, bias=negm, accum_out=se, r=R_, w=R_)
        D_("tensor_tensor", tp.v, le3, G1.unsqueeze(2).broadcast_to([128, 8, 4]), ALU.mult)
        D_("tensor_reduce", esel, tp.v.rearrange("p g e -> p e g"), AX.X, ALU.add)
        D_("tensor_reduce", m1, esel, AX.X, ALU.max)
        D_("tensor_scalar", e1, esel, m1, None, ALU.is_equal)
        D_("scalar_tensor_tensor", es2, e1, -1e30, esel, ALU.mult, ALU.add)
        D_("tensor_reduce", m2, es2, AX.X, ALU.max)
        D_("tensor_scalar", e2, es2, m2, None, ALU.is_equal)
        D_("tensor_scalar", nm1, m1, -1.0, None, ALU.mult)
        I(cx, "act", "activation", out=rr, in_=m2, func=AF.Exp, bias=nm1, r=R_, w=R_)
        D_("tensor_scalar", w1, rr, 1.0, None, ALU.add)
        D_("tensor_tensor", w1, w1, se, ALU.mult)
        D_("reciprocal", w1, w1)
        D_("tensor_tensor", w2, w1, rr, ALU.mult)
        D_("tensor_tensor", asel, e1, e2, ALU.add)
        D_("tensor_scalar", wsel, e1, w1, None, ALU.mult)
        D_("scalar_tensor_tensor", wsel, e2, w2, wsel, ALU.mult, ALU.add)
        A3 = A_f.v[:, t, :].rearrange("p (g e) -> p g e", e=4)
        W3 = Wt.v[:, t, :].rearrange("p (g e) -> p g e", e=4)
        g1b = G1.unsqueeze(2).broadcast_to([128, 8, 4])
        I(cx, "dve", "tensor_tensor", A3, g1b, asel.unsqueeze(1).broadcast_to([128, 8, 4]), ALU.mult, r=R_, w=[A_f.b])
        I(cx, "dve", "tensor_tensor", W3, g1b, wsel.unsqueeze(1).broadcast_to([128, 8, 4]), ALU.mult, r=R_, w=[Wt.b])
    I(cx, "dve", "tensor_copy", A_b.v, A_f.v, r=[A_f.b], w=[A_b.b])
    for t in range(NT):
        g0 = (t // TPG) * TPG
        bank = 3 + t % 2
        pp = cx.psum[bank][:, 0:NE]
        for j in range(g0, t + 1):
            l_ = K.ones.v if j < t else tri.v
            I(cx, "pe", "matmul", pp, l_, A_b.v[:, j, :], start=(j == g0), stop=(j == t),
              r=[K.ones.b, tri.b, A_b.b], w=[cx.psb[bank]])
        I(cx, "act", "copy", pos.v[:, t, :], pp, r=[cx.psb[bank]], w=[pos.b])
    cx.s.barrier()
    cx.arena.release()
    cx.arena.mark()
    hg = Tile(cx, [128, TPG, D], BF16, "hg")
    sel = Rot(cx, 2, [128, TPG, 128], BF16, "sel")
    xg = Rot(cx, 2, [128, NC, 128], BF16, "xg")
    nb = 0
    for gi in range(NGRP):
        dma(cx, "sp", hg.v, hn2_d[gi * c.MG:gi * c.MG + TPG * 128, :].rearrange("(a p) d -> p a d", p=128), w=[hg.b])
        for e in range(NE):
            sl = sel.next()
            for i in range(TPG):
                t = gi * TPG + i
                I(cx, "dve" if i % 2 else "pool", "tensor_scalar", sl.v[:, i, :], iota.v, pos.v[:, t, e:e + 1],
                  A_f.v[:, t, e:e + 1], ALU.is_equal, ALU.mult, r=[iota.b, pos.b, A_f.b], w=[sl.b])
            x = xg.next()
            for c4 in range(0, NC, 4):
                bank = nb % 3
                nb += 1
                for j in range(4):
                    cc = c4 + j
                    for i in range(TPG):
                        I(cx, "pe", "matmul", cx.psum[bank][:, j * 128:(j + 1) * 128], hg.v[:, i, cc * 128:(cc + 1) * 128],
                          sl.v[:, i, :], start=(i == 0), stop=(i == TPG - 1), r=[hg.b, sl.b], w=[cx.psb[bank]])
                src = cx.psum[bank][:, 0:512].rearrange("p (a b) -> p a b", b=128)
                if nb % 2:
                    I(cx, "act", "copy", x.v[:, c4:c4 + 4, :], src, r=[cx.psb[bank]], w=[x.b])
                else:
                    I(cx, "dve", "tensor_copy", x.v[:, c4:c4 + 4, :], src, r=[cx.psb[bank]], w=[x.b])
            dma(cx, "pool", T["xg"][e * D:(e + 1) * D, gi * 128:(gi + 1) * 128].rearrange("(a p) s -> p a s", p=128),
                x.v, r=[x.b])
    cx.s.barrier()
    cx.arena.release()
    cs = Cfg.__new__(Cfg)
    cs.__dict__.update(c.__dict__)
    cs.S = NSL; cs.SB = NSL
    cx.arena.mark()
    gs = Tile(cx, [128, FC, NSL], BF16, "gs")
    ao = Rot(cx, 2, [128, NSL], BF16, "ao")
    yo = Rot(cx, 3, [128, 256], BF16, "yo")
    for e in range(NE):
        xTe = T["xg"][e * D:(e + 1) * D, :]
        tiles = ([(128 * f, 128, ("g", f), 0) for f in range(FC)]
                 + [(128 * f, 128, ("u", f), 1) for f in range(FC)])

        def epi_gu(cx, tag, s0, ns, ps, psb):
            kind, f = tag
            if kind == "g":
                I(cx, "act", "activation", out=gs.v[:, f, s0:s0 + ns], in_=ps, func=AF.Silu, r=[psb], w=[gs.b])
            else:
                a = ao.next()
                I(cx, "dve", "tensor_tensor", a.v[:, 0:ns], ps, gs.v[:, f, s0:s0 + ns], ALU.mult, r=[psb, gs.b], w=[a.b])
                dma(cx, "pool", T["aT"][f * 128:(f + 1) * 128, s0:s0 + ns], a.v[:, 0:ns], r=[a.b])
        phase_proj(cx, cs, xTe, D, [wgate_d[e], wup_d[e]], tiles, "T", epi_gu)
        tiles_d = [(256 * j, 256, 256 * j) for j in range(D // 256)]

        def epi_d(cx, tag, s0, ns, ps, psb, e=e):
            y = yo.next()
            I(cx, "act" if (s0 // 128) % 2 else "dve", "copy" if (s0 // 128) % 2 else "tensor_copy", y.v, ps, r=[psb], w=[y.b])
            dma(cx, "pool", T["yexp"][e * NSL + s0:e * NSL + s0 + 128, tag:tag + 256], y.v, r=[y.b])
        phase_proj(cx, cs, T["aT"], c.FF, wdown_d[e], tiles_d, "N", epi_d)
    cx.arena.release()
    cx.arena.mark()
    swT = Tile(cx, [128, NE, TPG * 128], BF16, "swT")
    selw = Rot(cx, 3, [128, 128], BF16, "selw")
    yb = Rot(cx, 2, [128, NE, 512], BF16, "yb")
    ht = Rot(cx, 3, [128, 512], F32, "mh")
    nb = 0
    CW5 = 512 if D >= 512 else D
    for gi in range(NGRP):
        for e in range(NE):
            for i in range(TPG):
                t = gi * TPG + i
                s_ = selw.next()
                I(cx, "dve" if i % 2 else "pool", "tensor_scalar", s_.v, iota.v, pos.v[:, t, e:e + 1], Wt.v[:, t, e:e + 1],
                  ALU.is_equal, ALU.mult, r=[iota.b, pos.b, Wt.b], w=[s_.b])
                bank = 6 + nb % 2
                nb += 1
                pt = psum_bf16(cx, bank)
                I(cx, "pe", "transpose", pt[:, 0:128], s_.v, K.ident.v, r=[s_.b, K.ident.b], w=[cx.psb[bank]])
                I(cx, "act", "copy", swT.v[:, e, i * 128:(i + 1) * 128], pt[:, 0:128], r=[cx.psb[bank]], w=[swT.b])
        chunks = list(range(0, D, CW5))

        def load_y(ci, gi=gi):
            y = yb.next()
            c0 = chunks[ci]
            src = T["yexp"].rearrange("(e s) d -> s e d", e=NE)[gi * 128:(gi + 1) * 128, :, c0:c0 + CW5]
            dma(cx, "sp", y.v[:, :, 0:CW5], src, w=[y.b])
            return y
        ynext = load_y(0)
        for ci, c0 in enumerate(chunks):
            y = ynext
            if ci + 1 < len(chunks):
                ynext = load_y(ci + 1)
            for i in range(TPG):
                t = gi * TPG + i
                bank = nb % 3
                nb += 1
                ps = cx.psum[bank][:, 0:CW5]
                for e in range(NE):
                    I(cx, "pe", "matmul", ps, swT.v[:, e, i * 128:(i + 1) * 128], y.v[:, e, 0:CW5],
                      start=(e == 0), stop=(e == NE - 1), r=[swT.b, y.b], w=[cx.psb[bank]])
                h = ht.next()
                dma(cx, "sp", h.v[:, 0:CW5], h_d[t * 128:(t + 1) * 128, c0:c0 + CW5], w=[h.b])
                I(cx, "dve", "tensor_tensor", h.v[:, 0:CW5], ps, h.v[:, 0:CW5], ALU.add, r=[cx.psb[bank], h.b], w=[h.b])
                dma(cx, "pool", h_d[t * 128:(t + 1) * 128, c0:c0 + CW5], h.v[:, 0:CW5], r=[h.b])
    cx.s.barrier()
    cx.arena.release()
    cx.arena.release()


def iota_ident(cx, K):
    return K.identf.v


def phase_final_norm(cx, c, K, h_d, g_d, out_d):
    D, S = c.D, c.S
    cx.arena.mark()
    gb = Tile(cx, [128, D], F32, "f_gbc")
    dma(cx, "sp", gb.v, g_d.partition_broadcast(128), w=[gb.b])
    xt = Rot(cx, 2, [128, D], F32, "f_x")
    xo = Rot(cx, 2, [128, D], F32, "f_o")
    junk = Tile(cx, [128, D], BF16, "f_junk")
    sc = Rot(cx, 2, [128, 2], F32, "f_sc")
    for t in range(S // 128):
        x = xt.next(); o = xo.next(); s = sc.next()
        dma(cx, "sp", x.v, h_d[t * 128:(t + 1) * 128, :], w=[x.b])
        I(cx, "act", "activation", out=junk.v, in_=x.v, func=AF.Square, accum_out=s.v[:, 0:1], r=[x.b], w=[junk.b, s.b])
        I(cx, "dve", "tensor_scalar", s.v[:, 1:2], s.v[:, 0:1], 1.0 / D, c.EPS, ALU.mult, ALU.add, r=[s.b], w=[s.b])
        I(cx, "act", "activation", out=s.v[:, 1:2], in_=s.v[:, 1:2], func=AF.Sqrt, r=[s.b], w=[s.b])
        I(cx, "dve", "reciprocal", s.v[:, 1:2], s.v[:, 1:2], r=[s.b], w=[s.b])
        I(cx, "dve", "scalar_tensor_tensor", o.v, x.v, s.v[:, 1:2], gb.v, ALU.mult, ALU.mult, r=[x.b, s.b, gb.b], w=[o.b])
        dma(cx, "act", out_d[t * 128:(t + 1) * 128, :], o.v, r=[o.b], out=True)
    cx.s.barrier()
    cx.arena.release()


WEIGHT_SHAPES = lambda c: {
    "attn_norm_g": [c.DEPTH, c.D], "w_in": [c.DEPTH, c.D, c.NIN], "q_norm_g": [c.DEPTH, c.CQ],
    "kv_norm_g": [c.DEPTH, c.CKV], "wq_b": [c.DEPTH, c.CQ, c.CH * (c.CN + c.CR)],
    "wkv_b": [c.DEPTH, c.CKV, c.CH * (c.CN + c.CV)], "sinks": [c.DEPTH, c.BH],
    "w_out_a": [c.DEPTH, c.AW, c.D], "w_out_b": [c.DEPTH, c.BW, c.D], "w_out_c": [c.DEPTH, c.CW, c.D],
    "w_o": [c.DEPTH, c.D, c.D], "ffn_norm_g": [c.DEPTH, c.D], "w_group": [c.DEPTH, c.D, c.NG],
    "b_group": [c.DEPTH, c.NG], "w_expert": [c.DEPTH, c.D, c.NE], "b_expert": [c.DEPTH, c.NE],
    "w_gate": [c.DEPTH, c.NE, c.D, c.FF], "w_up": [c.DEPTH, c.NE, c.D, c.FF],
    "w_down": [c.DEPTH, c.NE, c.FF, c.D], "final_norm_g": [c.D],
}


def build_forward(c, NB, debug=(), stop_after=None):
    cnp = const_inputs(c)

    def body(cx):
        nc = cx.nc
        cx.debug = set(debug)
        S, D = c.S, c.D
        x_d = nc.dram_tensor("x", [NB * S, D], F32, kind="ExternalInput").ap()
        pos_d = nc.dram_tensor("positions", [NB * S], I32, kind="ExternalInput").ap()
        Wd = {k: nc.dram_tensor(k, shp, F32, kind="ExternalInput").ap() for k, shp in WEIGHT_SHAPES(c).items()}
        cin = {k: nc.dram_tensor(k, list(v.shape), CONST_DT[k], kind="ExternalInput").ap() for k, v in cnp.items()}
        out_d = nc.dram_tensor("out", [NB * S, D], F32, kind="ExternalOutput").ap()
        K = load_consts(cx, c, cin)
        K.identf = Tile(cx, [128, 128], F32, "identf")
        dma(cx, "sp", K.identf.v, cin["c_identf"], w=[K.identf.b])
        T = {}
        NGRP = max(1, S // c.MG)
        NSL = NGRP * c.CAP
        spec = {"h": ([S, D], F32), "hnT": ([D, S], BF16), "qaT": ([c.AW, S], BF16), "kaT": ([c.AW, S], BF16),
                "va": ([S, c.AW], BF16), "qbT": ([c.BW, S], BF16), "kbT": ([c.BKW, S], BF16), "vb": ([S, c.BKW], BF16),
                "cqT": ([c.CQ, S], BF16), "ckvT": ([c.CKV, S], BF16), "kpeT": ([64, S], BF16),
                "gT": ([3 * D, S], BF16), "qnT": ([c.CH * 128, S], BF16), "qpeT": ([c.CH * 64, S], BF16),
                "knT": ([c.CH * 128, S], BF16), "vc": ([S, c.CH * 128], BF16),
                "oaT": ([c.AW, S], BF16), "obT": ([c.BW, S], BF16), "ocT": ([c.CW, S], BF16), "yT": ([D, S], BF16),
                "hn2": ([S, D], BF16), "xg": ([c.NE * D, NSL], BF16), "aT": ([c.FF, NSL], BF16),
                "yexp": ([c.NE * NSL, D], BF16),
                "cosA": ([128, S], F32), "sinA": ([128, S], F32), "cosB": ([128, S], F32), "sinB": ([128, S], F32)}
        for k, (shp, dt) in spec.items():
            T[k] = dram(cx, k, shp, dt)
        E = make_epilogues(cx, c, K, None)
        cx.s.barrier()
        for b in range(NB):
            xb_d = x_d[b * S:(b + 1) * S, :]
            cx.arena.mark()
            ct = Tile(cx, [128, S], F32, "ropec"); st = Tile(cx, [128, S], F32, "ropes")
            for which, (cn, sn) in enumerate((("cosA", "sinA"), ("cosB", "sinB"))):
                build_rope(cx, c, K, pos_d[b * S:(b + 1) * S], which, ct, st)
                dma(cx, "sp", T[cn], ct.v, r=[ct.b])
                dma(cx, "sp", T[sn], st.v, r=[st.b])
                cx.s.barrier()
            cx.arena.release()
            for t in range(S // 128):
                dma(cx, "sp" if t % 2 else "act", T["h"][t * 128:(t + 1) * 128, :], xb_d[t * 128:(t + 1) * 128, :])
            cx.s.barrier()
            R = T
            for l in range(c.DEPTH):
                phase_rmsnorm_T(cx, c, K, T["h"], Wd["attn_norm_g"][l], T["hnT"])
                if stop_after == "norm": break
                phase_A(cx, c, K, E, R, T, T["hnT"], Wd["w_in"][l])
                if stop_after == "A": break
                phase_mla_up(cx, c, K, E, R, T, Wd["q_norm_g"][l], Wd["kv_norm_g"][l], Wd["wq_b"][l], Wd["wkv_b"][l])
                if stop_after == "mla_up": break
                phase_attn_causal(cx, c, K, c.AH, [(T["qaT"], T["kaT"], 128)], T["va"], 128, c.AD ** -0.5, T["oaT"],
                                  moba=True, cin=cin)
                if stop_after == "moba": break
                phase_swa(cx, c, K, T["qbT"], T["kbT"], T["vb"], Wd["sinks"][l], T["obT"], cin)
                if stop_after == "swa": break
                phase_attn_causal(cx, c, K, c.CH, [(T["qnT"], T["knT"], 128), (T["qpeT"], T["kpeT"], 64)], T["vc"], 128,
                                  (c.CN + c.CR) ** -0.5, T["ocT"], mla_shared_k=True)
                if stop_after == "mla": break
                phase_out_gate(cx, c, K, [T["oaT"], T["obT"], T["ocT"]],
                               [Wd["w_out_a"][l], Wd["w_out_b"][l], Wd["w_out_c"][l]], T["gT"], T["yT"])
                phase_resid_proj(cx, c, K, T["yT"], D, Wd["w_o"][l], T["h"])
                if stop_after == "mix": break
                phase_moe(cx, c, K, T, cin, T["h"], Wd["ffn_norm_g"][l], Wd["w_group"][l], Wd["b_group"][l],
                          Wd["w_expert"][l], Wd["b_expert"][l], Wd["w_gate"][l], Wd["w_up"][l], Wd["w_down"][l])
                if stop_after == "moe": break
            phase_final_norm(cx, c, K, T["h"], Wd["final_norm_g"], out_d[b * S:(b + 1) * S, :])
    nc = build_program(body)
    return nc, cnp


_CACHE = {}


def kernel(**inputs):
    c = Cfg()
    if "nc" not in _CACHE:
        _CACHE["nc"] = build_forward(c, 1)
    nc, cnp = _CACHE["nc"]
    B = inputs["x"].shape[0]
    in_maps = []
    for b in range(B):
        m = {"x": np.ascontiguousarray(np.asarray(inputs["x"][b], dtype=np.float32)),
             "positions": np.ascontiguousarray(np.asarray(inputs["positions"][b]).astype(np.int32))}
        for k in WEIGHT_SHAPES(c):
            m[k] = np.asarray(inputs[k], dtype=np.float32)
        m.update(cnp)
        in_maps.append(m)
    res = run_bass_kernel_spmd(nc, in_maps, core_ids=list(range(B)))
    out = np.stack([np.asarray(res.results[b]["out"]) for b in range(B)], axis=0)
    return out.astype(np.float32)
```
